# Optimizing a Trainium2 kernel written in Bass

```python
import math
import jax, jax.numpy as jnp
from jax import lax
import numpy as np

D_MODEL = 2048
BATCH = 2
SEQ = 16384
DEPTH = 2

CHUNK = 64
D_CONV = 1024
CONV_K = 3
N_HEADS = 8
HEAD_DIM = 128
D_ATTN = N_HEADS * HEAD_DIM
Q_BLOCK = 128
N_BRANCH = 2
P_TOTAL = 3 * D_CONV + 3 * D_ATTN + N_HEADS + N_BRANCH * D_MODEL
D_FF = 5632
N_EXPERTS = 8
TOP_K = 2
D_FF_EXPERT = 7168
MOE_GROUP = 256
N_DENSE = (DEPTH + 1) // 2
N_MOE = DEPTH // 2
RMS_EPS = 1e-6

kernel_name = "hybrid_conv_forgetting_attn_moe_trunk"


def rmsnorm(x, g):
    xf = x.astype(jnp.float32)
    y = xf * lax.rsqrt(jnp.mean(xf * xf, axis=-1, keepdims=True) + RMS_EPS)
    return (y * g.astype(jnp.float32)).astype(x.dtype)


def causal_dwconv(u, w):
    S = u.shape[1]
    up = jnp.pad(u, ((0, 0), (CONV_K - 1, 0), (0, 0)))
    y = up[:, 0:S] * w[0]
    for j in range(1, CONV_K):
        y = y + up[:, j:j + S] * w[j]
    return y


def forgetting_attention(q, k, v, log_f):
    Bsz, S, H, Dh = q.shape
    nb = S // Q_BLOCK
    scale = 1.0 / math.sqrt(Dh)
    c = jnp.cumsum(log_f, axis=1)
    qb = q.reshape(Bsz, nb, Q_BLOCK, H, Dh).transpose(1, 0, 3, 2, 4)
    cq = c.reshape(Bsz, nb, Q_BLOCK, H).transpose(1, 0, 3, 2)
    kT = k.transpose(0, 2, 1, 3)
    vT = v.transpose(0, 2, 1, 3)
    ck = c.transpose(0, 2, 1)
    key_pos = jnp.arange(S)

    def block(args):
        qi, cqi, bi = args
        s = jnp.einsum('bhqd,bhkd->bhqk', qi, kT, preferred_element_type=jnp.float32) * scale
        s = s + cqi[..., None] - ck[:, :, None, :]
        q_pos = bi * Q_BLOCK + jnp.arange(Q_BLOCK)
        mask = key_pos[None, :] <= q_pos[:, None]
        s = jnp.where(mask[None, None], s, -jnp.inf)
        p = jax.nn.softmax(s, axis=-1)
        return jnp.einsum('bhqk,bhkd->bhqd', p.astype(vT.dtype), vT)

    o = lax.map(block, (qb, cq, jnp.arange(nb)))
    return o.transpose(1, 0, 3, 2, 4).reshape(Bsz, S, H * Dh)


def hybrid_mixer(a, w_in, b_forget, conv_w, w_conv_out, w_attn_out, w_o):
    Bsz, S, _ = a.shape
    proj = a @ w_in
    sizes = [D_CONV, D_CONV, D_CONV, D_ATTN, D_ATTN, D_ATTN, N_HEADS]
    offs = [int(o) for o in np.cumsum(sizes)]
    b_gate, c_gate, u, q, k, v, f_logit, gates = jnp.split(proj, offs, axis=-1)
    y_conv = (b_gate * causal_dwconv(c_gate * u, conv_w)) @ w_conv_out
    log_f = jax.nn.log_sigmoid(f_logit.astype(jnp.float32) + b_forget.astype(jnp.float32))
    o = forgetting_attention(q.reshape(Bsz, S, N_HEADS, HEAD_DIM),
                             k.reshape(Bsz, S, N_HEADS, HEAD_DIM),
                             v.reshape(Bsz, S, N_HEADS, HEAD_DIM), log_f)
    y_attn = o @ w_attn_out
    g = jax.nn.sigmoid(gates)
    g_conv, g_attn = g[..., :D_MODEL], g[..., D_MODEL:]
    return (g_conv * y_conv + g_attn * y_attn) @ w_o


def swiglu(a, w_gate, w_up, w_down):
    return (jax.nn.silu(a @ w_gate) * (a @ w_up)) @ w_down


def moe_swiglu(a, router_w, router_b, w_gate, w_up, w_down):
    Bsz, S, D = a.shape
    af = a.reshape(-1, D)
    N = af.shape[0]
    logits = (af @ router_w).astype(jnp.float32) + router_b.astype(jnp.float32)
    top_logit, top_idx = lax.top_k(logits, TOP_K)
    top_w = jax.nn.softmax(top_logit, axis=-1)
    NK = N * TOP_K
    flat_e = top_idx.reshape(NK).astype(jnp.int32)
    slot = jnp.arange(NK, dtype=jnp.int32)
    se, order = lax.sort((flat_e, slot), num_keys=1, is_stable=True)
    stok = order // TOP_K
    sw = top_w.reshape(NK)[order]
    counts = jnp.bincount(flat_e, length=N_EXPERTS)
    padded = (counts + MOE_GROUP - 1) // MOE_GROUP * MOE_GROUP
    pad_end = jnp.cumsum(padded)
    pad_start = pad_end - padded
    start = jnp.cumsum(counts) - counts
    dest = pad_start[se] + slot - start[se]
    n_blocks = -(-NK // MOE_GROUP) + N_EXPERTS
    R = n_blocks * MOE_GROUP
    row_tok = jnp.zeros((R,), jnp.int32).at[dest].set(stok)
    row_w = jnp.zeros((R,), jnp.float32).at[dest].set(sw)
    block_start = jnp.arange(n_blocks) * MOE_GROUP
    block_e = jnp.minimum(jnp.searchsorted(pad_end, block_start, side='right'), N_EXPERTS - 1)
    xb = af[row_tok].reshape(n_blocks, MOE_GROUP, D)

    def expert_block(args):
        xg, e = args
        hdn = jax.nn.silu(xg @ w_gate[e]) * (xg @ w_up[e])
        return hdn @ w_down[e]

    yb = lax.map(expert_block, (xb, block_e)).reshape(R, D)
    y = jax.ops.segment_sum(yb.astype(jnp.float32) * row_w[:, None], row_tok, num_segments=N)
    return y.astype(a.dtype).reshape(Bsz, S, D)


def setup_inputs(seed: int = 0) -> dict:
    key = jax.random.key(seed)
    ks = jax.random.split(key, 20)
    f32 = jnp.float32

    def nrm(k, shape, fan_in):
        return jax.random.normal(k, shape, f32) * (fan_in ** -0.5)

    return {
        "x": jax.random.normal(ks[0], (BATCH, SEQ, D_MODEL), f32),
        "mix_norm": 1.0 + 0.02 * jax.random.normal(ks[1], (DEPTH, D_MODEL), f32),
        "w_in": nrm(ks[2], (DEPTH, D_MODEL, P_TOTAL), D_MODEL),
        "b_forget": 4.0 + 0.5 * jax.random.normal(ks[3], (DEPTH, N_HEADS), f32),
        "conv_w": nrm(ks[4], (DEPTH, CONV_K, D_CONV), CONV_K),
        "w_conv_out": nrm(ks[5], (DEPTH, D_CONV, D_MODEL), D_CONV),
        "w_attn_out": nrm(ks[6], (DEPTH, D_ATTN, D_MODEL), D_ATTN),
        "w_o": nrm(ks[7], (DEPTH, D_MODEL, D_MODEL), D_MODEL),
        "ffn_norm": 1.0 + 0.02 * jax.random.normal(ks[8], (DEPTH, D_MODEL), f32),
        "dense_w_gate": nrm(ks[9], (N_DENSE, D_MODEL, D_FF), D_MODEL),
        "dense_w_up": nrm(ks[10], (N_DENSE, D_MODEL, D_FF), D_MODEL),
        "dense_w_down": nrm(ks[11], (N_DENSE, D_FF, D_MODEL), D_FF),
        "router_w": nrm(ks[12], (N_MOE, D_MODEL, N_EXPERTS), D_MODEL),
        "router_b": 0.01 * jax.random.normal(ks[13], (N_MOE, N_EXPERTS), f32),
        "moe_w_gate": nrm(ks[14], (N_MOE, N_EXPERTS, D_MODEL, D_FF_EXPERT), D_MODEL),
        "moe_w_up": nrm(ks[15], (N_MOE, N_EXPERTS, D_MODEL, D_FF_EXPERT), D_MODEL),
        "moe_w_down": nrm(ks[16], (N_MOE, N_EXPERTS, D_FF_EXPERT, D_MODEL), D_FF_EXPERT),
        "final_norm": 1.0 + 0.02 * jax.random.normal(ks[17], (D_MODEL,), f32),
    }


def reference(x, mix_norm, w_in, b_forget, conv_w, w_conv_out, w_attn_out, w_o, ffn_norm,
              dense_w_gate, dense_w_up, dense_w_down, router_w, router_b,
              moe_w_gate, moe_w_up, moe_w_down, final_norm):
    h = x
    for layer in range(DEPTH):
        a = rmsnorm(h, mix_norm[layer])
        h = h + hybrid_mixer(a, w_in[layer], b_forget[layer], conv_w[layer],
                             w_conv_out[layer], w_attn_out[layer], w_o[layer])
        a = rmsnorm(h, ffn_norm[layer])
        if layer % 2 == 0:
            i = layer // 2
            h = h + swiglu(a, dense_w_gate[i], dense_w_up[i], dense_w_down[i])
        else:
            i = layer // 2
            h = h + moe_swiglu(a, router_w[i], router_b[i], moe_w_gate[i], moe_w_up[i], moe_w_down[i])
    return rmsnorm(h, final_norm)
```

```python
import numpy as np
import ml_dtypes
from contextlib import ExitStack
import concourse.bass as bass
import concourse.mybir as mybir
from concourse.bass_utils import run_bass_kernel_spmd

F32 = mybir.dt.float32
BF16 = mybir.dt.bfloat16
AF = mybir.ActivationFunctionType
ALU = mybir.AluOpType

D = 2048
NCORE = 8
DC = 1024
DA = 1024
NH = 8
HD = 128
PTOT = 3 * DC + 3 * DA + NH + 2 * D
DFF = 5632
NE = 8
DFE = 7168
EPS = 1e-6
TT = 512


class Sem:
    def __init__(self, kb, name):
        self.h = kb.es.enter_context(kb.nc.semaphore(name))
        self.v = 0


class Tile:
    def __init__(self, kb, ap, name):
        self.kb = kb
        self.ap = ap
        self.name = name
        self.w = []
        self.r = []
        self.pr = []
        self.dsem = None

    def dma_sem(self):
        if self.dsem is None:
            self.dsem = self.kb.new_dma_sem(self.name)
        return self.dsem

    def __getitem__(self, idx):
        return self.ap[idx]


class KB:
    def __init__(self):
        self.nc = bass.Bass("TRN2", target_bir_lowering=False)
        self.es = ExitStack()
        self.waited = {}
        self.esem = {}
        self.dma_sems = []
        self.free_dma_sems = []
        self.n_ins = 0
        nc = self.nc
        self.engs = {"pe": nc.tensor, "act": nc.scalar, "dve": nc.vector, "pool": nc.gpsimd, "sp": nc.sync}
        for k in self.engs:
            self.esem[k] = Sem(self, "e_" + k)
        self.pending = {k: False for k in self.engs}

    def new_dma_sem(self, name):
        s = Sem(self, "d_" + name)
        self.dma_sems.append(s)
        return s

    def dram(self, name, shape, dt, kind="Internal"):
        return self.nc.dram_tensor(name, list(shape), dt, kind=kind).ap()

    def sbuf(self, name, shape, dt):
        t = self.es.enter_context(self.nc.sbuf_tensor(name, list(shape), dt))
        return Tile(self, t, name)

    def psum(self, name, shape, dt=F32):
        t = self.es.enter_context(self.nc.psum_tensor(name, list(shape), dt))
        return Tile(self, t, name)

    def _wait(self, ek, sem, val):
        key = (ek, id(sem))
        if self.waited.get(key, 0) >= val:
            return
        self.engs[ek].wait_ge(sem.h, val)
        self.waited[key] = val

    def _deps(self, ek, reads, writes):
        need = {}

        def add(s, v):
            k = id(s)
            if k not in need or need[k][1] < v:
                need[k] = (s, v)
        for t in reads:
            for (s, v, e) in t.w:
                add(s, v)
        for t in writes:
            if t.r:
                t.pr = t.r
                t.r = []
                t.w = []
            for (s, v, e) in t.pr:
                if e != ek:
                    add(s, v)
        for (s, v) in need.values():
            self._wait(ek, s, v)

    def op(self, ek, fn, reads=(), writes=(), signal=True):
        self._deps(ek, reads, writes)
        ins = fn()
        self.n_ins += 1
        es = self.esem[ek]
        if signal:
            ins.then_inc(es.h, 1)
            es.v += 1
            val = es.v
            self.pending[ek] = False
        else:
            val = es.v + 1
            self.pending[ek] = True
        for t in reads:
            t.r.append((es, val, ek))
        for t in writes:
            t.w.append((es, val, ek))
        return ins

    def dma(self, qk, out_ap, in_ap, sb_tile, load, extra_reads=(), **kw):
        if load:
            self._deps(qk, list(extra_reads), [sb_tile])
        else:
            self._deps(qk, [sb_tile] + list(extra_reads), [])
        s = sb_tile.dma_sem()
        ins = self.engs[qk].dma_start(out=out_ap, in_=in_ap, **kw)
        ins.then_inc(s.h, 16)
        s.v += 16
        self.n_ins += 1
        if load:
            sb_tile.w.append((s, s.v, "dma"))
        else:
            sb_tile.r.append((s, s.v, "dma"))
        return ins

    def dram_dma(self, qk, out_ap, in_ap, sem, **kw):
        ins = self.engs[qk].dma_start(out=out_ap, in_=in_ap, **kw)
        ins.then_inc(sem.h, 16)
        sem.v += 16
        self.n_ins += 1
        return ins

    def barrier(self, extra_sems=()):
        for ek in self.engs:
            assert not self.pending[ek], ek
        allsems = list(self.esem.values()) + self.dma_sems + list(extra_sems)
        for ek in self.engs:
            for s in allsems:
                if s.v > 0:
                    self._wait(ek, s, s.v)

    def finish(self, extra_sems=()):
        self.barrier(extra_sems)
        self.es.close()
        return self.nc


def mm_group(kb, out_tile, out_ap, pairs, reads):
    n = len(pairs)
    for i, (l, r) in enumerate(pairs):
        kb.op("pe", lambda l=l, r=r, i=i: kb.nc.tensor.matmul(out_ap, l, r, start=(i == 0), stop=(i == n - 1)),
              reads=reads if i == 0 else (), writes=[out_tile] if i == 0 else (), signal=(i == n - 1))
    es = kb.esem["pe"]
    out_tile.w = [(es, es.v, "pe")]
    for t in reads:
        t.r.append((es, es.v, "pe"))


def cast_rows(kb, sem, src, dst, rows, cols, row_step=128):
    for r0 in range(0, rows, row_step):
        r1 = min(rows, r0 + row_step)
        kb.dram_dma("pool", dst[r0:r1, :], src[r0:r1, :], sem, max_dma_last_dim=4096)


def build_cast(specs):
    kb = KB()
    sem = Sem(kb, "cast")
    for (name, rows, cols) in specs:
        src = kb.dram(name, [rows, cols], F32, kind="ExternalInput")
        dst = kb.dram(name + "_b", [rows, cols], BF16, kind="ExternalOutput")
        cast_rows(kb, sem, src, dst, rows, cols)
    return kb.finish([sem])


class Consts:
    pass


def norm_transpose(kb, cs, h_dram, tok0, aT, hin_ring, ablk_ring, tp_ring, junk, ss_ring, gcol, ident, ctr, after_block=None):
    nc = kb.nc
    for blk in range(TT // 128):
        i = ctr["nb"]
        ctr["nb"] += 1
        hin = hin_ring[i % len(hin_ring)]
        ab = ablk_ring[i % len(ablk_ring)]
        ss = ss_ring[i % len(ss_ring)]
        r0 = tok0 + blk * 128
        kb.dma("sp", hin.ap[:, :], h_dram[r0:r0 + 128, :], hin, load=True)
        kb.op("act", lambda: nc.scalar.activation(out=ab.ap[:, :], in_=hin.ap[:, :], func=AF.Square,
                                                  accum_out=ss.ap[:, 0:1]), reads=[hin], writes=[ab, ss])
        kb.op("act", lambda: nc.scalar.activation(out=ss.ap[:, 1:2], in_=ss.ap[:, 0:1], func=AF.Sqrt,
                                                  bias=cs.eps.ap[:, 0:1], scale=1.0 / D), reads=[ss, cs.eps], writes=[ss])
        kb.op("dve", lambda: nc.vector.reciprocal(out=ss.ap[:, 2:3], in_=ss.ap[:, 1:2]), reads=[ss], writes=[ss])
        kb.op("dve", lambda: nc.vector.tensor_scalar(out=ab.ap[:, :], in0=hin.ap[:, :], scalar1=ss.ap[:, 2:3],
                                                     scalar2=None, op0=ALU.mult), reads=[hin, ss], writes=[ab])
        for half in range(2):
            tp = tp_ring[ctr["tp"] % len(tp_ring)]
            ctr["tp"] += 1
            for j in range(8):
                c = half * 8 + j
                kb.op("pe", lambda c=c, j=j: nc.tensor.transpose(tp.ap[:, j, :], ab.ap[:, c * 128:(c + 1) * 128], ident.ap[:, :]),
                      reads=[ab, ident] if j == 0 else (), writes=[tp] if j == 0 else (), signal=(j == 7))
            es = kb.esem["pe"]
            tp.w = [(es, es.v, "pe")]
            ab.r.append((es, es.v, "pe"))
            for j in range(8):
                c = half * 8 + j
                if True:
                    kb.op("dve", lambda c=c, j=j: nc.vector.tensor_scalar(out=aT.ap[:, c, blk * 128:(blk + 1) * 128], in0=tp.ap[:, j, :],
                                                                          scalar1=gcol.ap[:, c:c + 1], scalar2=None, op0=ALU.mult),
                          reads=[tp, gcol], writes=[aT])
                else:
                    kb.op("act", lambda c=c, j=j: nc.scalar.activation(out=aT.ap[:, c, blk * 128:(blk + 1) * 128], in_=tp.ap[:, j, :],
                                                                       func=AF.Copy, scale=gcol.ap[:, c:c + 1]),
                          reads=[tp, gcol], writes=[aT])
        if after_block is not None:
            after_block(blk)


def load_w(kb, wring, ctr, w_dram, k0, kc, c0, ncols):
    wt = wring[ctr["w"] % len(wring)]
    ctr["w"] += 1
    src = w_dram[k0 * 128:(k0 + kc) * 128, c0:c0 + ncols].rearrange("(c p) n -> p c n", p=128)
    kb.dma("sp", wt.ap[:, 0:kc, 0:ncols], src, wt, load=True)
    return wt


def build_p1(TC):
    kb = KB()
    nc = kb.nc
    NT = TC // TT
    h = kb.dram("h", [TC, D], F32, kind="ExternalInput")
    w_in = kb.dram("w_in_b", [D, PTOT], BF16, kind="ExternalInput")
    gcol_d = kb.dram("gcol", [128, 16], F32, kind="ExternalInput")
    convw_d = kb.dram("convw", [128, 8, 3], F32, kind="ExternalInput")
    negb_d = kb.dram("bf", [8, 1], F32, kind="ExternalInput")
    ident_d = kb.dram("ident", [128, 128], BF16, kind="ExternalInput")
    zT = kb.dram("zT", [DC, TC], BF16, kind="ExternalOutput")
    cu_tail = kb.dram("cu_tail", [DC, 2], F32, kind="ExternalOutput")
    b_head = kb.dram("b_head", [DC, 2], F32, kind="ExternalOutput")
    qT = kb.dram("qT", [DA, TC], BF16, kind="ExternalOutput")
    kT = kb.dram("kT", [DA, TC], BF16, kind="ExternalOutput")
    vv = kb.dram("v", [TC, DA], BF16, kind="ExternalOutput")
    lf = kb.dram("lf", [NH, TC], F32, kind="ExternalOutput")
    gT = kb.dram("gT", [2 * D, TC], BF16, kind="ExternalOutput")

    cs = Consts()
    cs.eps = kb.sbuf("eps", [128, 1], F32)
    gcol = kb.sbuf("gcol_s", [128, 16], F32)
    convw = kb.sbuf("convw_s", [128, 8, 3], F32)
    bfs = kb.sbuf("bf_s", [8, 2], F32)
    ident = kb.sbuf("ident_s", [128, 128], BF16)
    kb.op("dve", lambda: nc.vector.memset(cs.eps.ap[:, :], EPS), writes=[cs.eps])
    kb.dma("sp", gcol.ap[:, :], gcol_d[:, :], gcol, load=True)
    kb.dma("sp", convw.ap[:, :, :], convw_d[:, :, :], convw, load=True)
    kb.dma("sp", bfs.ap[:, 0:1], negb_d[:, :], bfs, load=True)
    kb.dma("sp", ident.ap[:, :], ident_d[:, :], ident, load=True)
    kb.op("dve", lambda: nc.vector.tensor_scalar(out=bfs.ap[:, 1:2], in0=bfs.ap[:, 0:1], scalar1=-1.0, scalar2=None, op0=ALU.mult),
          reads=[bfs], writes=[bfs])

    hin_ring = [kb.sbuf(f"hin{i}", [128, D], F32) for i in range(2)]
    ablk_ring = [kb.sbuf(f"ablk{i}", [128, D], BF16) for i in range(2)]
    ss_ring = [kb.sbuf(f"ss{i}", [128, 4], F32) for i in range(4)]
    junk = None
    aT_ring = [kb.sbuf(f"aT{i}", [128, 16, TT], BF16) for i in range(2)]
    wring = [kb.sbuf(f"w{i}", [128, 16, 512], BF16) for i in range(3)]
    tp_ring = [kb.psum(f"tp{i}", [128, 8, 128], BF16) for i in range(2)]
    ps_ring = [kb.psum(f"ps{i}", [128, 512], F32) for i in range(6)]
    bcu = [kb.sbuf(f"bcu{i}", [128, 8, TT], BF16) for i in range(3)]
    cuext = [kb.sbuf(f"cuext{i}", [128, TT + 2], F32) for i in range(2)]
    carry = kb.sbuf("carry", [128, 8, 2], F32)
    ycv = [kb.sbuf(f"ycv{i}", [128, TT], F32) for i in range(2)]
    zst = [kb.sbuf(f"zst{i}", [128, 8, TT], BF16) for i in range(1)]
    qst = [kb.sbuf(f"qst{i}", [128, 4, TT], BF16) for i in range(3)]
    vst = [kb.sbuf(f"vst{i}", [128, 512], BF16) for i in range(2)]
    lfst = [kb.sbuf(f"lfst{i}", [8, 3, TT], F32) for i in range(2)]
    bh = kb.sbuf("bh", [128, 8, 2], F32)
    kb.op("pool", lambda: nc.gpsimd.memset(carry.ap[:, :, :], 0.0), writes=[carry])

    ctr = {"nb": 0, "tp": 0, "w": 0, "ps": 0, "q": 0, "v": 0, "ev": 0}

    def next_ps():
        p = ps_ring[ctr["ps"] % len(ps_ring)]
        ctr["ps"] += 1
        return p

    def evac_copy(out_tile, out_ap, ps):
        kb.op("dve", lambda: nc.vector.tensor_copy(out=out_ap, in_=ps.ap[:, :]), reads=[ps], writes=[out_tile])

    blocks = []
    for s in range(3):
        for cb in range(2):
            blocks.append(("bcu", s, cb, s * DC + cb * 512))
    for s in range(2):
        for cb in range(2):
            blocks.append(("qk", s, cb, 3 * DC + s * DA + cb * 512))
    for cb in range(2):
        blocks.append(("v", 0, cb, 3 * DC + 2 * DA + cb * 512))
    blocks.append(("f", 0, 0, 3 * DC + 3 * DA))
    for cb in range(8):
        blocks.append(("g", 0, cb, 3 * DC + 3 * DA + NH + cb * 512))

    import os
    SKIP = os.environ.get('P1_SKIP', '').split(',')
    blocks = [b for b in blocks if b[0] not in SKIP]
    norm_transpose(kb, cs, h, 0, aT_ring[0], hin_ring, ablk_ring, tp_ring, junk, ss_ring, gcol, ident, ctr)
    for t in range(NT):
        aT = aT_ring[t % 2]
        tok0 = t * TT
        if not blocks:
            break
        wt_next = load_w(kb, wring, ctr, w_in, 0, 16, blocks[0][3], 512)
        for bi, (kind, s, cb, c0) in enumerate(blocks):
            wt = wt_next
            if bi + 1 < len(blocks):
                nk = blocks[bi + 1]
                wt_next = load_w(kb, wring, ctr, w_in, 0, 16, nk[3], 8 if nk[0] == "f" else 512)
            if bi == min(6, len(blocks) - 1) and t + 1 < NT:
                norm_transpose(kb, cs, h, (t + 1) * TT, aT_ring[(t + 1) % 2], hin_ring, ablk_ring, tp_ring, junk, ss_ring, gcol, ident, ctr)
            if kind in ("bcu", "qk", "g"):
                if kind == "qk" or kind == "g":
                    st = qst[ctr["q"] % len(qst)]
                    ctr["q"] += 1
                for c in range(4):
                    ps = next_ps()
                    mm_group(kb, ps, ps.ap[:, :], [(wt.ap[:, k, c * 128:(c + 1) * 128], aT.ap[:, k, :]) for k in range(16)], [wt, aT])
                    if kind == "bcu":
                        evac_copy(bcu[s], bcu[s].ap[:, cb * 4 + c, :], ps)
                    elif kind == "qk":
                        evac_copy(st, st.ap[:, c, :], ps)
                    else:
                        kb.op("act", lambda c=c, ps=ps, st=st: nc.scalar.activation(out=st.ap[:, c, :], in_=ps.ap[:, :], func=AF.Sigmoid),
                              reads=[ps], writes=[st])
                if kind == "qk":
                    dst = (qT if s == 0 else kT)[cb * 512:(cb + 1) * 512, tok0:tok0 + TT].rearrange("(c p) t -> p c t", p=128)
                    kb.dma("sp", dst, st.ap[:, :, :], st, load=False)
                elif kind == "g":
                    dst = gT[cb * 512:(cb + 1) * 512, tok0:tok0 + TT].rearrange("(c p) t -> p c t", p=128)
                    kb.dma("sp", dst, st.ap[:, :, :], st, load=False)
                elif s == 2 and cb == 1 and 'conv' not in SKIP:
                    zs = zst[0]
                    for c in range(8):
                        ce = cuext[c % 2]
                        yc = ycv[c % 2]
                        kb.op("pool", lambda c=c, ce=ce: nc.gpsimd.tensor_copy(out=ce.ap[:, 0:2], in_=carry.ap[:, c, :]), reads=[carry], writes=[ce])
                        kb.op("pool", lambda c=c, ce=ce: nc.gpsimd.tensor_tensor(out=ce.ap[:, 2:TT + 2], in0=bcu[1].ap[:, c, :], in1=bcu[2].ap[:, c, :], op=ALU.mult),
                              reads=[bcu[1], bcu[2]], writes=[ce])
                        kb.op("pool", lambda c=c, ce=ce: nc.gpsimd.tensor_copy(out=carry.ap[:, c, :], in_=ce.ap[:, TT:TT + 2]), reads=[ce], writes=[carry])
                        kb.op("pool", lambda c=c, ce=ce, yc=yc: nc.gpsimd.tensor_scalar(out=yc.ap[:, :], in0=ce.ap[:, 0:TT], scalar1=convw.ap[:, c, 0:1], scalar2=None, op0=ALU.mult),
                              reads=[ce, convw], writes=[yc])
                        kb.op("dve", lambda c=c, ce=ce, yc=yc: nc.vector.scalar_tensor_tensor(out=yc.ap[:, :], in0=ce.ap[:, 1:TT + 1], scalar=convw.ap[:, c, 1:2], in1=yc.ap[:, :], op0=ALU.mult, op1=ALU.add),
                              reads=[ce, convw, yc], writes=[yc])
                        kb.op("dve", lambda c=c, ce=ce, yc=yc: nc.vector.scalar_tensor_tensor(out=yc.ap[:, :], in0=ce.ap[:, 2:TT + 2], scalar=convw.ap[:, c, 2:3], in1=yc.ap[:, :], op0=ALU.mult, op1=ALU.add),
                              reads=[ce, convw, yc], writes=[yc])
                        kb.op("pool", lambda c=c, yc=yc, zs=zs: nc.gpsimd.tensor_tensor(out=zs.ap[:, c, :], in0=yc.ap[:, :], in1=bcu[0].ap[:, c, :], op=ALU.mult),
                              reads=[yc, bcu[0]], writes=[zs])
                    dst = zT[:, tok0:tok0 + TT].rearrange("(c p) t -> p c t", p=128)
                    kb.dma("sp", dst, zs.ap[:, :, :], zs, load=False)
                    if t == 0:
                        kb.op("pool", lambda: nc.gpsimd.tensor_copy(out=bh.ap[:, :, :], in_=bcu[0].ap[:, :, 0:2]), reads=[bcu[0]], writes=[bh])
                        kb.dma("sp", b_head.rearrange("(c p) t -> p c t", p=128), bh.ap[:, :, :], bh, load=False)
                    if t == NT - 1:
                        kb.dma("sp", cu_tail.rearrange("(c p) t -> p c t", p=128), carry.ap[:, :, :], carry, load=False)
            elif kind == "v":
                for tb in range(TT // 128):
                    ps = next_ps()
                    mm_group(kb, ps, ps.ap[:, :], [(aT.ap[:, k, tb * 128:(tb + 1) * 128], wt.ap[:, k, :]) for k in range(16)], [wt, aT])
                    st = vst[ctr["v"] % 2]
                    ctr["v"] += 1
                    evac_copy(st, st.ap[:, :], ps)
                    kb.dma("sp", vv[tok0 + tb * 128: tok0 + (tb + 1) * 128, cb * 512:(cb + 1) * 512], st.ap[:, :], st, load=False)
            elif kind == "f":
                ps = next_ps()
                mm_group(kb, ps, ps.ap[0:8, :], [(wt.ap[:, k, 0:8], aT.ap[:, k, :]) for k in range(16)], [wt, aT])
                st = lfst[t % 2]
                kb.op("act", lambda ps=ps, st=st: nc.scalar.activation(out=st.ap[:, 0, :], in_=ps.ap[0:8, :], func=AF.Exp, bias=bfs.ap[:, 1:2], scale=-1.0),
                      reads=[ps, bfs], writes=[st])
                kb.op("act", lambda st=st: nc.scalar.activation(out=st.ap[:, 1, :], in_=st.ap[:, 0, :], func=AF.Ln, bias=1.0, scale=1.0),
                      reads=[st], writes=[st])
                kb.op("dve", lambda st=st: nc.vector.tensor_scalar(out=st.ap[:, 2, :], in0=st.ap[:, 1, :], scalar1=-1.0, scalar2=None, op0=ALU.mult),
                      reads=[st], writes=[st])
                kb.dma("sp", lf[:, tok0:tok0 + TT], st.ap[:, 2, :], st, load=False)
    return kb.finish()


def build_p2(S, NSEQ):
    kb = KB()
    nc = kb.nc
    NTOK = S * NSEQ
    NBS = S // 128
    NQ = S // TT
    qT = kb.dram("qT", [128, NTOK], BF16, kind="ExternalInput")
    kT = kb.dram("kT", [128, NTOK], BF16, kind="ExternalInput")
    vv = kb.dram("v", [NTOK, 128], BF16, kind="ExternalInput")
    lfc_d = kb.dram("lfcol", [128, NTOK // 128], F32, kind="ExternalInput")
    tri_d = kb.dram("tri", [128, 128], F32, kind="ExternalInput")
    sel_d = kb.dram("sel", [128, 128], F32, kind="ExternalInput")
    ones_d = kb.dram("onesb", [128, 128], BF16, kind="ExternalInput")
    mask_d = kb.dram("masks", [128, 4, TT], BF16, kind="ExternalInput")
    oT = kb.dram("oT", [128, NTOK], BF16, kind="ExternalOutput")

    tri = kb.sbuf("tri_s", [128, 128], F32)
    sel = kb.sbuf("sel_s", [128, 128], F32)
    onesb = kb.sbuf("ones_s", [128, 128], BF16)
    masks = kb.sbuf("mask_s", [128, 4, TT], BF16)
    onesf = kb.sbuf("onesf", [128, NBS], F32)
    lfc = kb.sbuf("lfc", [128, NTOK // 128], F32)
    for (t, d) in ((tri, tri_d), (sel, sel_d), (onesb, ones_d), (lfc, lfc_d)):
        kb.dma("sp", t.ap[:, :], d[:, :], t, load=True)
    kb.dma("sp", masks.ap[:, :, :], mask_d[:, :, :], masks, load=True)
    kb.op("dve", lambda: nc.vector.memset(onesf.ap[:, :], 1.0), writes=[onesf])

    kTs = kb.sbuf("kTs", [128, S], BF16)
    qTs = kb.sbuf("qTs", [128, S], BF16)
    vs = kb.sbuf("vs", [128, NBS, 128], BF16)
    within = kb.sbuf("within", [128, NBS], F32)
    totb = kb.sbuf("totb", [128, NBS], F32)
    incl = kb.sbuf("incl", [128, NBS], F32)
    ccol = kb.sbuf("ccol", [128, NBS], F32)
    crefb = kb.sbuf("crefb", [128, NBS], F32)
    biasq = [kb.sbuf(f"biasq{i}", [128, NBS], F32) for i in range(2)]
    pt = [kb.sbuf(f"pt{i}", [128, TT], BF16) for i in range(3)]
    rs = [kb.sbuf(f"rs{i}", [128, TT], F32) for i in range(2)]
    ost = [kb.sbuf(f"ost{i}", [128, TT], BF16) for i in range(2)]
    ps_s = [kb.psum(f"pss{i}", [128, TT], F32) for i in range(3)]
    acc_o = [kb.psum(f"acco{i}", [128, TT], F32) for i in range(2)]
    acc_s = [kb.psum(f"accs{i}", [128, TT], F32) for i in range(2)]
    psc = kb.psum("psc", [128, 512], F32)
    scale = 1.0 / float(np.sqrt(HD))

    for s in range(NSEQ):
        t0 = s * S
        nch = 4
        for i in range(nch):
            a, b = i * S // nch, (i + 1) * S // nch
            kb.dma("sp", kTs.ap[:, a:b], kT[:, t0 + a:t0 + b], kTs, load=True)
            kb.dma("sp", qTs.ap[:, a:b], qT[:, t0 + a:t0 + b], qTs, load=True)
            kb.dma("sp", vs.ap[:, a // 128:b // 128, :], vv[t0 + a:t0 + b, :].rearrange("(n p) d -> p n d", p=128), vs, load=True)
        kb.op("pe", lambda: nc.tensor.matmul(psc.ap[:, 0:NBS], tri.ap[:, :], lfc.ap[:, s * NBS:(s + 1) * NBS], start=True, stop=True),
              reads=[tri, lfc], writes=[psc])
        kb.op("dve", lambda: nc.vector.tensor_copy(out=within.ap[:, :], in_=psc.ap[:, 0:NBS]), reads=[psc], writes=[within])
        kb.op("pe", lambda: nc.tensor.matmul(psc.ap[:, 0:NBS], sel.ap[:, :], within.ap[:, :], start=True, stop=True),
              reads=[sel, within], writes=[psc])
        kb.op("dve", lambda: nc.vector.tensor_copy(out=totb.ap[:, :], in_=psc.ap[:, 0:NBS]), reads=[psc], writes=[totb])
        kb.op("dve", lambda: nc.vector.tensor_tensor_scan(out=incl.ap[:, :], data0=onesf.ap[:, :], data1=totb.ap[:, :], initial=0.0,
                                                          op0=ALU.mult, op1=ALU.add), reads=[onesf, totb], writes=[incl])
        kb.op("dve", lambda: nc.vector.tensor_tensor(out=ccol.ap[:, :], in0=within.ap[:, :], in1=incl.ap[:, :], op=ALU.add),
              reads=[within, incl], writes=[ccol])
        kb.op("dve", lambda: nc.vector.tensor_tensor(out=ccol.ap[:, :], in0=ccol.ap[:, :], in1=totb.ap[:, :], op=ALU.subtract),
              reads=[ccol, totb], writes=[ccol])
        kb.op("pe", lambda: nc.tensor.matmul(psc.ap[:, 0:NBS], sel.ap[:, :], ccol.ap[:, :], start=True, stop=True),
              reads=[sel, ccol], writes=[psc])
        kb.op("dve", lambda: nc.vector.tensor_copy(out=crefb.ap[:, :], in_=psc.ap[:, 0:NBS]), reads=[psc], writes=[crefb])

        steps = [(qt, kbk) for qt in range(NQ) for kbk in range(4 * qt + 4)]

        def emit_s(i):
            qt, kbk = steps[i]
            p = ps_s[i % 3]
            kb.op("pe", lambda: nc.tensor.matmul(p.ap[:, :], kTs.ap[:, kbk * 128:(kbk + 1) * 128], qTs.ap[:, qt * TT:(qt + 1) * TT], start=True, stop=True),
                  reads=[kTs, qTs], writes=[p])

        emit_s(0)
        for i, (qt, kbk) in enumerate(steps):
            nk = 4 * qt + 4
            bq = biasq[qt % 2]
            if kbk == 0:
                kb.op("dve", lambda: nc.vector.tensor_scalar(out=bq.ap[:, 0:nk], in0=ccol.ap[:, 0:nk], scalar1=-1.0,
                                                             scalar2=crefb.ap[:, 4 * qt + 1:4 * qt + 2], op0=ALU.mult, op1=ALU.add),
                      reads=[ccol, crefb], writes=[bq])
            if i + 1 < len(steps):
                emit_s(i + 1)
            p = ps_s[i % 3]
            ptile = pt[i % 3]
            kb.op("act", lambda: nc.scalar.activation(out=ptile.ap[:, :], in_=p.ap[:, :], func=AF.Exp, bias=bq.ap[:, kbk:kbk + 1], scale=scale),
                  reads=[p, bq], writes=[ptile])
            if kbk >= 4 * qt:
                j = kbk - 4 * qt
                kb.op("pool", lambda: nc.gpsimd.tensor_tensor(out=ptile.ap[:, :], in0=ptile.ap[:, :], in1=masks.ap[:, j, :], op=ALU.mult),
                      reads=[ptile, masks], writes=[ptile])
            ao = acc_o[qt % 2]
            asum = acc_s[qt % 2]
            first, last = (kbk == 0), (kbk == nk - 1)
            kb.op("pe", lambda: nc.tensor.matmul(ao.ap[:, :], vs.ap[:, kbk, :], ptile.ap[:, :], start=first, stop=last),
                  reads=[vs, ptile], writes=[ao] if first else (), signal=False)
            kb.op("pe", lambda: nc.tensor.matmul(asum.ap[:, :], onesb.ap[:, :], ptile.ap[:, :], start=first, stop=last),
                  reads=[onesb, ptile], writes=[asum] if first else (), signal=True)
            if last:
                es = kb.esem["pe"]
                ao.w = [(es, es.v, "pe")]
                asum.w = [(es, es.v, "pe")]
                r = rs[qt % 2]
                o = ost[qt % 2]
                kb.op("dve", lambda: nc.vector.reciprocal(out=r.ap[:, :], in_=asum.ap[:, :]), reads=[asum], writes=[r])
                kb.op("dve", lambda: nc.vector.tensor_tensor(out=o.ap[:, :], in0=ao.ap[:, :], in1=r.ap[:, :], op=ALU.mult), reads=[ao, r], writes=[o])
                kb.dma("sp", oT[:, t0 + qt * TT:t0 + (qt + 1) * TT], o.ap[:, :], o, load=False)
    return kb.finish()


def p2_consts():
    bf = ml_dtypes.bfloat16
    kk = np.arange(128)
    tri = (kk[:, None] <= kk[None, :]).astype(np.float32)
    sel = np.zeros((128, 128), np.float32)
    sel[127, :] = 1.0
    masks = np.zeros((128, 4, TT), np.float32)
    qq = np.arange(TT)
    for j in range(4):
        masks[:, j, :] = ((j * 128 + kk)[:, None] <= qq[None, :])
    return {"tri": tri, "sel": sel, "onesb": np.ones((128, 128), np.float32).astype(bf), "masks": masks.astype(bf)}


def build_p3(TC):
    kb = KB()
    nc = kb.nc
    NT = TC // TT
    zT = kb.dram("zT", [DC, TC], BF16, kind="ExternalInput")
    oT = kb.dram("oT", [DA, TC], BF16, kind="ExternalInput")
    gT = kb.dram("gT", [2 * D, TC], BF16, kind="ExternalInput")
    cup_d = kb.dram("cu_prev", [128, 8, 2], F32, kind="ExternalInput")
    bh_d = kb.dram("b_head", [128, 8, 2], F32, kind="ExternalInput")
    convw_d = kb.dram("convw", [128, 8, 3], F32, kind="ExternalInput")
    wA = kb.dram("wA_b", [DC, D], BF16, kind="ExternalInput")
    wB = kb.dram("wB_b", [DA, D], BF16, kind="ExternalInput")
    wo = kb.dram("wo_b", [D, D], BF16, kind="ExternalInput")
    h = kb.dram("h", [TC, D], F32, kind="ExternalInput")
    h1 = kb.dram("h1", [TC, D], F32, kind="ExternalOutput")

    cup = kb.sbuf("cup", [128, 8, 2], F32)
    bh = kb.sbuf("bh", [128, 8, 2], F32)
    convw = kb.sbuf("convw_s", [128, 8, 3], F32)
    fx = kb.sbuf("fx", [128, 8, 4], F32)
    for (t, d) in ((cup, cup_d), (bh, bh_d), (convw, convw_d)):
        kb.dma("sp", t.ap[:, :, :], d[:, :, :], t, load=True)
    wring = [kb.sbuf(f"w{i}", [128, 16, 512], BF16) for i in range(3)]
    zt_r = [kb.sbuf(f"zt{i}", [128, 8, TT], BF16) for i in range(2)]
    ot_r = [kb.sbuf(f"ot{i}", [128, 8, TT], BF16) for i in range(2)]
    gt = kb.sbuf("gt", [128, 32, TT], BF16)
    mT = kb.sbuf("mT", [128, 16, TT], BF16)
    hall = kb.sbuf("hall", [128, 4, D], F32)
    tmp = [kb.sbuf(f"tmp{i}", [128, TT], F32) for i in range(4)]
    ps_ring = [kb.psum(f"ps{i}", [128, 512], F32) for i in range(8)]
    ctr = {"w": 0, "ps": 0, "tmp": 0}

    def next_ps():
        p = ps_ring[ctr["ps"] % len(ps_ring)]
        ctr["ps"] += 1
        return p

    V = nc.vector
    kb.op("dve", lambda: V.tensor_tensor(out=fx.ap[:, :, 0], in0=cup.ap[:, :, 0], in1=convw.ap[:, :, 0], op=ALU.mult), reads=[cup, convw], writes=[fx])
    kb.op("dve", lambda: V.tensor_tensor(out=fx.ap[:, :, 2], in0=cup.ap[:, :, 1], in1=convw.ap[:, :, 1], op=ALU.mult), reads=[cup, convw], writes=[fx])
    kb.op("dve", lambda: V.tensor_tensor(out=fx.ap[:, :, 0], in0=fx.ap[:, :, 0], in1=fx.ap[:, :, 2], op=ALU.add), reads=[fx], writes=[fx])
    kb.op("dve", lambda: V.tensor_tensor(out=fx.ap[:, :, 0], in0=fx.ap[:, :, 0], in1=bh.ap[:, :, 0], op=ALU.mult), reads=[fx, bh], writes=[fx])
    kb.op("dve", lambda: V.tensor_tensor(out=fx.ap[:, :, 1], in0=cup.ap[:, :, 1], in1=convw.ap[:, :, 0], op=ALU.mult), reads=[cup, convw], writes=[fx])
    kb.op("dve", lambda: V.tensor_tensor(out=fx.ap[:, :, 1], in0=fx.ap[:, :, 1], in1=bh.ap[:, :, 1], op=ALU.mult), reads=[fx, bh], writes=[fx])

    for t in range(NT):
        tok0 = t * TT
        zt, ot = zt_r[t % 2], ot_r[t % 2]
        kb.dma("sp", zt.ap[:, :, :], zT[:, tok0:tok0 + TT].rearrange("(c p) t -> p c t", p=128), zt, load=True)
        kb.dma("sp", ot.ap[:, :, :], oT[:, tok0:tok0 + TT].rearrange("(c p) t -> p c t", p=128), ot, load=True)
        for q4 in range(4):
            kb.dma("sp", gt.ap[:, q4 * 8:(q4 + 1) * 8, :], gT[q4 * 1024:(q4 + 1) * 1024, tok0:tok0 + TT].rearrange("(c p) t -> p c t", p=128), gt, load=True)
        kb.dma("sp", hall.ap[:, :, :], h[tok0:tok0 + TT, :].rearrange("(n p) d -> p n d", p=128), hall, load=True)
        if t == 0:
            kb.op("dve", lambda: V.tensor_tensor(out=zt.ap[:, :, 0:2], in0=zt.ap[:, :, 0:2], in1=fx.ap[:, :, 0:2], op=ALU.add), reads=[zt, fx], writes=[zt])
        for jb in range(4):
            wt = wring[ctr["w"] % 3]
            ctr["w"] += 1
            kb.dma("sp", wt.ap[:, 0:8, :], wA[:, jb * 512:(jb + 1) * 512].rearrange("(c p) n -> p c n", p=128), wt, load=True)
            kb.dma("sp", wt.ap[:, 8:16, :], wB[:, jb * 512:(jb + 1) * 512].rearrange("(c p) n -> p c n", p=128), wt, load=True)
            for c in range(4):
                j = jb * 4 + c
                pa = next_ps()
                mm_group(kb, pa, pa.ap[:, :], [(wt.ap[:, k, c * 128:(c + 1) * 128], zt.ap[:, k, :]) for k in range(8)], [wt, zt])
                pb = next_ps()
                mm_group(kb, pb, pb.ap[:, :], [(wt.ap[:, 8 + k, c * 128:(c + 1) * 128], ot.ap[:, k, :]) for k in range(8)], [wt, ot])
                t1 = tmp[ctr["tmp"] % 4]
                t2 = tmp[(ctr["tmp"] + 1) % 4]
                ctr["tmp"] += 2
                kb.op("dve", lambda: V.tensor_tensor(out=t1.ap[:, :], in0=pa.ap[:, :], in1=gt.ap[:, j, :], op=ALU.mult), reads=[pa, gt], writes=[t1])
                kb.op("dve", lambda: V.tensor_tensor(out=t2.ap[:, :], in0=pb.ap[:, :], in1=gt.ap[:, 16 + j, :], op=ALU.mult), reads=[pb, gt], writes=[t2])
                kb.op("pool", lambda: nc.gpsimd.tensor_tensor(out=mT.ap[:, j, :], in0=t1.ap[:, :], in1=t2.ap[:, :], op=ALU.add), reads=[t1, t2], writes=[mT])
        for cb in range(4):
            wt = load_w(kb, wring, ctr, wo, 0, 16, cb * 512, 512)
            for tb in range(4):
                p = next_ps()
                mm_group(kb, p, p.ap[:, :], [(mT.ap[:, k, tb * 128:(tb + 1) * 128], wt.ap[:, k, :]) for k in range(16)], [wt, mT])
                kb.op("dve", lambda: V.tensor_tensor(out=hall.ap[:, tb, cb * 512:(cb + 1) * 512], in0=p.ap[:, :], in1=hall.ap[:, tb, cb * 512:(cb + 1) * 512], op=ALU.add),
                      reads=[p, hall], writes=[hall])
        kb.dma("sp", h1[tok0:tok0 + TT, :].rearrange("(n p) d -> p n d", p=128), hall.ap[:, :, :], hall, load=False)
    return kb.finish()


def build_ffn(TC, moe):
    kb = KB()
    nc = kb.nc
    V = nc.vector
    NT = TC // TT
    DFX = DFE if moe else DFF
    NEX = NE if moe else 1
    NCB = DFX // 512
    KCH = DFX // 128
    h = kb.dram("h", [TC, D], F32, kind="ExternalInput")
    gcol_d = kb.dram("gcol", [128, 16], F32, kind="ExternalInput")
    ident_d = kb.dram("ident", [128, 128], BF16, kind="ExternalInput")
    wg = kb.dram("wg_b", [NEX * D, DFX], BF16, kind="ExternalInput")
    wu = kb.dram("wu_b", [NEX * D, DFX], BF16, kind="ExternalInput")
    wd = kb.dram("wd_b", [NEX * DFX, D], BF16, kind="ExternalInput")
    out = kb.dram("hout", [TC, D], F32, kind="ExternalOutput")

    cs = Consts()
    cs.eps = kb.sbuf("eps", [128, 1], F32)
    gcol = kb.sbuf("gcol_s", [128, 16], F32)
    ident = kb.sbuf("ident_s", [128, 128], BF16)
    kb.op("dve", lambda: V.memset(cs.eps.ap[:, :], EPS), writes=[cs.eps])
    kb.dma("sp", gcol.ap[:, :], gcol_d[:, :], gcol, load=True)
    kb.dma("sp", ident.ap[:, :], ident_d[:, :], ident, load=True)
    hin_ring = [kb.sbuf("hin0", [128, D], F32)]
    ablk_ring = [kb.sbuf("ablk0", [128, D], BF16)]
    ss_ring = [kb.sbuf(f"ss{i}", [128, 4], F32) for i in range(4)]
    aT = kb.sbuf("aT", [128, 16, TT], BF16)
    wring = [kb.sbuf(f"w{i}", [128, 16, 512], BF16) for i in range(3 if not moe else 2)]
    hdnT = kb.sbuf("hdnT", [128, KCH, TT], BF16)
    hall = kb.sbuf("hall", [128, 4, D], F32)
    sg = [kb.sbuf(f"sg{i}", [128, TT], F32) for i in range(2)]
    tp_ring = [kb.psum(f"tp{i}", [128, 8, 128], BF16) for i in range(2)]
    router = None
    if moe:
        rw_d = kb.dram("rw", [128, 16, NE], F32, kind="ExternalInput")
        rb_d = kb.dram("rb", [128, NE], F32, kind="ExternalInput")
        gfin_d = kb.dram("gfin", [128, D], F32, kind="ExternalInput")
        id32_d = kb.dram("ident32", [128, 128], F32, kind="ExternalInput")
        rw = kb.sbuf("rw_s", [128, 16, NE], F32)
        rb = kb.sbuf("rb_s", [128, NE], F32)
        gfin = kb.sbuf("gfin_s", [128, D], F32)
        id32 = kb.sbuf("id32_s", [128, 128], F32)
        kb.dma("sp", rw.ap[:, :, :], rw_d[:, :, :], rw, load=True)
        kb.dma("sp", rb.ap[:, :], rb_d[:, :], rb, load=True)
        kb.dma("sp", gfin.ap[:, :], gfin_d[:, :], gfin, load=True)
        kb.dma("sp", id32.ap[:, :], id32_d[:, :], id32, load=True)
        ab32 = kb.sbuf("ab32", [128, D], F32)
        a32T = [kb.sbuf(f"a32T{i}", [128, 4, 128], F32) for i in range(2)]
        tp32 = kb.psum("tp32", [128, 4, 128], F32)
        psl = kb.psum("psl", [128, 512], F32)
        Wall = kb.sbuf("Wall", [128, 4, NE], F32)
        rt = kb.sbuf("rt", [128, 8, NE], F32)
        router = True
    ps_ring = [kb.psum(f"ps{i}", [128, 512], F32) for i in range(4 if moe else 6)]
    ctr = {"nb": 0, "tp": 0, "w": 0, "ps": 0, "sg": 0, "a32": 0}

    def next_ps():
        p = ps_ring[ctr["ps"] % len(ps_ring)]
        ctr["ps"] += 1
        return p

    def route(blk):
        hin = hin_ring[0]
        ss = ss_ring[(ctr["nb"] - 1) % len(ss_ring)]
        kb.op("dve", lambda: V.tensor_scalar(out=ab32.ap[:, :], in0=hin.ap[:, :], scalar1=ss.ap[:, 2:3], scalar2=None, op0=ALU.mult),
              reads=[hin, ss], writes=[ab32])
        for r in range(4):
            for j in range(4):
                c = r * 4 + j
                kb.op("pe", lambda c=c, j=j: nc.tensor.transpose(tp32.ap[:, j, :], ab32.ap[:, c * 128:(c + 1) * 128], id32.ap[:, :]),
                      reads=[ab32, id32] if j == 0 else (), writes=[tp32] if j == 0 else (), signal=(j == 3))
            es = kb.esem["pe"]
            tp32.w = [(es, es.v, "pe")]
            ab32.r.append((es, es.v, "pe"))
            a3 = a32T[ctr["a32"] % 2]
            ctr["a32"] += 1
            for j in range(4):
                c = r * 4 + j
                kb.op("dve", lambda c=c, j=j: V.tensor_scalar(out=a3.ap[:, j, :], in0=tp32.ap[:, j, :], scalar1=gcol.ap[:, c:c + 1], scalar2=None, op0=ALU.mult),
                      reads=[tp32, gcol], writes=[a3])
            for j in range(4):
                c = r * 4 + j
                kb.op("pe", lambda c=c, j=j: nc.tensor.matmul(psl.ap[:, 0:NE], a3.ap[:, j, :], rw.ap[:, c, :], start=(c == 0), stop=(c == 15)),
                      reads=[a3, rw], writes=[psl] if c == 0 else (), signal=(j == 3))
        es = kb.esem["pe"]
        psl.w = [(es, es.v, "pe")]
        L, m1k, L2, m2k = rt.ap[:, 0, :], rt.ap[:, 1, :], rt.ap[:, 2, :], rt.ap[:, 3, :]
        sc = rt.ap[:, 4, :]
        AX = mybir.AxisListType.X
        kb.op("dve", lambda: V.tensor_tensor(out=L, in0=psl.ap[:, 0:NE], in1=rb.ap[:, :], op=ALU.add), reads=[psl, rb], writes=[rt])
        kb.op("dve", lambda: V.tensor_reduce(out=sc[:, 0:1], in_=L, axis=AX, op=ALU.max), reads=[rt], writes=[rt])
        kb.op("dve", lambda: V.tensor_scalar(out=m1k, in0=L, scalar1=sc[:, 0:1], scalar2=None, op0=ALU.is_equal), reads=[rt], writes=[rt])
        kb.op("dve", lambda: V.scalar_tensor_tensor(out=L2, in0=m1k, scalar=-1e30, in1=L, op0=ALU.mult, op1=ALU.add), reads=[rt], writes=[rt])
        kb.op("dve", lambda: V.tensor_reduce(out=sc[:, 1:2], in_=L2, axis=AX, op=ALU.max), reads=[rt], writes=[rt])
        kb.op("dve", lambda: V.tensor_scalar(out=m2k, in0=L2, scalar1=sc[:, 1:2], scalar2=None, op0=ALU.is_equal), reads=[rt], writes=[rt])
        kb.op("dve", lambda: V.tensor_tensor(out=sc[:, 2:3], in0=sc[:, 1:2], in1=sc[:, 0:1], op=ALU.subtract), reads=[rt], writes=[rt])
        kb.op("act", lambda: nc.scalar.activation(out=sc[:, 3:4], in_=sc[:, 2:3], func=AF.Sigmoid), reads=[rt], writes=[rt])
        kb.op("dve", lambda: V.tensor_scalar(out=sc[:, 4:5], in0=sc[:, 3:4], scalar1=-1.0, scalar2=1.0, op0=ALU.mult, op1=ALU.add), reads=[rt], writes=[rt])
        kb.op("dve", lambda: V.tensor_scalar(out=Wall.ap[:, blk, :], in0=m1k, scalar1=sc[:, 4:5], scalar2=None, op0=ALU.mult), reads=[rt], writes=[Wall])
        kb.op("dve", lambda: V.scalar_tensor_tensor(out=Wall.ap[:, blk, :], in0=m2k, scalar=sc[:, 3:4], in1=Wall.ap[:, blk, :], op0=ALU.mult, op1=ALU.add),
              reads=[rt, Wall], writes=[Wall])

    for t in range(NT):
        tok0 = t * TT
        norm_transpose(kb, cs, h, tok0, aT, hin_ring, ablk_ring, tp_ring, None, ss_ring, gcol, ident, ctr, after_block=(route if moe else None))
        kb.dma("sp", hall.ap[:, :, :], h[tok0:tok0 + TT, :].rearrange("(n p) d -> p n d", p=128), hall, load=True)
        for e in range(NEX):
            for cbk in range(NCB):
                wtg = load_w(kb, wring, ctr, wg, e * 16, 16, cbk * 512, 512)
                wtu = load_w(kb, wring, ctr, wu, e * 16, 16, cbk * 512, 512)
                for c in range(4):
                    pg = next_ps()
                    mm_group(kb, pg, pg.ap[:, :], [(wtg.ap[:, k, c * 128:(c + 1) * 128], aT.ap[:, k, :]) for k in range(16)], [wtg, aT])
                    pu = next_ps()
                    mm_group(kb, pu, pu.ap[:, :], [(wtu.ap[:, k, c * 128:(c + 1) * 128], aT.ap[:, k, :]) for k in range(16)], [wtu, aT])
                    s_ = sg[ctr["sg"] % 2]
                    ctr["sg"] += 1
                    kb.op("act", lambda: nc.scalar.activation(out=s_.ap[:, :], in_=pg.ap[:, :], func=AF.Silu), reads=[pg], writes=[s_])
                    kb.op("dve", lambda: V.tensor_tensor(out=hdnT.ap[:, cbk * 4 + c, :], in0=pu.ap[:, :], in1=s_.ap[:, :], op=ALU.mult), reads=[pu, s_], writes=[hdnT])
            kgs = [(k0, min(16, KCH - k0)) for k0 in range(0, KCH, 16)]
            for cb in range(4):
                pss = [next_ps() for _ in range(4)]
                for gi, (k0, kc) in enumerate(kgs):
                    wt = load_w(kb, wring, ctr, wd, e * KCH + k0, kc, cb * 512, 512)
                    for tb in range(4):
                        p = pss[tb]
                        for k in range(kc):
                            first = (gi == 0 and k == 0)
                            last = (gi == len(kgs) - 1 and k == kc - 1)
                            kb.op("pe", lambda k=k, p=p, tb=tb: nc.tensor.matmul(p.ap[:, :], hdnT.ap[:, k0 + k, tb * 128:(tb + 1) * 128], wt.ap[:, k, :], start=first, stop=last),
                                  reads=[hdnT, wt] if k == 0 else (), writes=[p] if first else (), signal=(k == kc - 1))
                        es = kb.esem["pe"]
                        wt.r.append((es, es.v, "pe"))
                        hdnT.r.append((es, es.v, "pe"))
                        if gi == len(kgs) - 1:
                            p.w = [(es, es.v, "pe")]
                for tb in range(4):
                    p = pss[tb]
                    dst = hall.ap[:, tb, cb * 512:(cb + 1) * 512]
                    if moe:
                        kb.op("dve", lambda p=p, dst=dst, tb=tb: V.scalar_tensor_tensor(out=dst, in0=p.ap[:, :], scalar=Wall.ap[:, tb, e:e + 1], in1=dst, op0=ALU.mult, op1=ALU.add),
                              reads=[p, hall, Wall], writes=[hall])
                    else:
                        kb.op("dve", lambda p=p, dst=dst: V.tensor_tensor(out=dst, in0=p.ap[:, :], in1=dst, op=ALU.add), reads=[p, hall], writes=[hall])
        if moe:
            hin = hin_ring[0]
            for tb in range(4):
                ss = ss_ring[tb]
                kb.op("act", lambda: nc.scalar.activation(out=hin.ap[:, :], in_=hall.ap[:, tb, :], func=AF.Square, accum_out=ss.ap[:, 0:1]), reads=[hall], writes=[hin, ss])
                kb.op("act", lambda: nc.scalar.activation(out=ss.ap[:, 1:2], in_=ss.ap[:, 0:1], func=AF.Sqrt, bias=cs.eps.ap[:, 0:1], scale=1.0 / D), reads=[ss, cs.eps], writes=[ss])
                kb.op("dve", lambda: V.reciprocal(out=ss.ap[:, 2:3], in_=ss.ap[:, 1:2]), reads=[ss], writes=[ss])
                kb.op("dve", lambda: V.scalar_tensor_tensor(out=hall.ap[:, tb, :], in0=hall.ap[:, tb, :], scalar=ss.ap[:, 2:3], in1=gfin.ap[:, :], op0=ALU.mult, op1=ALU.mult),
                      reads=[hall, ss, gfin], writes=[hall])
        kb.dma("sp", out[tok0:tok0 + TT, :].rearrange("(n p) d -> p n d", p=128), hall.ap[:, :, :], hall, load=False)
    return kb.finish()


_PROG = {}


def _prog(key, fn):
    if key not in _PROG:
        _PROG[key] = fn()
    return _PROG[key]


def _run(nc, in_maps):
    res = run_bass_kernel_spmd(nc, in_maps, core_ids=list(range(NCORE)))
    return res.results


def _col(a):
    return np.ascontiguousarray(a.reshape(8, 128, a.shape[1]).transpose(1, 0, 2))


def kernel(x, mix_norm, w_in, b_forget, conv_w, w_conv_out, w_attn_out, w_o, ffn_norm,
           dense_w_gate, dense_w_up, dense_w_down, router_w, router_b,
           moe_w_gate, moe_w_up, moe_w_down, final_norm):
    f32 = np.float32
    bf = ml_dtypes.bfloat16
    B, S, _ = x.shape
    NTOK = B * S
    TC = NTOK // NCORE
    depth = w_in.shape[0]
    A = lambda a: np.ascontiguousarray(np.asarray(a, dtype=f32))

    wsrc = {
        "w_in": A(w_in).reshape(-1, PTOT), "wA": A(w_conv_out).reshape(-1, D), "wB": A(w_attn_out).reshape(-1, D),
        "wo": A(w_o).reshape(-1, D), "dg": A(dense_w_gate).reshape(-1, DFF), "du": A(dense_w_up).reshape(-1, DFF),
        "dd": A(dense_w_down).reshape(-1, D), "mg": A(moe_w_gate).reshape(-1, DFE), "mu": A(moe_w_up).reshape(-1, DFE),
        "md": A(moe_w_down).reshape(-1, D),
    }
    specs = [(n, a.shape[0] // NCORE, a.shape[1]) for n, a in wsrc.items()]
    nc0 = _prog("cast", lambda: build_cast(specs))
    in_maps = [{n: a[c * (a.shape[0] // NCORE):(c + 1) * (a.shape[0] // NCORE)] for n, a in wsrc.items()} for c in range(NCORE)]
    r0 = _run(nc0, in_maps)
    wb = {n: np.concatenate([np.asarray(r0[c][n + "_b"]) for c in range(NCORE)], axis=0) for n in wsrc}
    del wsrc, in_maps, r0

    ident = np.eye(128, dtype=f32).astype(bf)
    consts2 = p2_consts()
    h = [A(x).reshape(NTOK, D)[c * TC:(c + 1) * TC] for c in range(NCORE)]
    nc1 = _prog("p1", lambda: build_p1(TC))
    nc2 = _prog("p2", lambda: build_p2(S, B))
    nc3 = _prog("p3", lambda: build_p3(TC))
    cores_per_seq = NCORE // B
    out = None
    for l in range(depth):
        gcol = np.ascontiguousarray(A(mix_norm[l]).reshape(16, 128).T)
        convw = _col(np.ascontiguousarray(A(conv_w[l]).T))
        bfg = A(b_forget[l]).reshape(NH, 1)
        w_in_l = wb["w_in"][l * D:(l + 1) * D]
        r1 = _run(nc1, [{"h": h[c], "w_in_b": w_in_l, "gcol": gcol, "convw": convw, "bf": bfg, "ident": ident} for c in range(NCORE)])
        in2 = []
        for hd in range(NH):
            sl = slice(hd * HD, (hd + 1) * HD)
            lf_h = np.concatenate([np.asarray(r1[c]["lf"])[hd] for c in range(NCORE)])
            m = {"qT": np.concatenate([np.asarray(r1[c]["qT"])[sl] for c in range(NCORE)], axis=1),
                 "kT": np.concatenate([np.asarray(r1[c]["kT"])[sl] for c in range(NCORE)], axis=1),
                 "v": np.ascontiguousarray(np.concatenate([np.asarray(r1[c]["v"])[:, sl] for c in range(NCORE)], axis=0)),
                 "lfcol": np.ascontiguousarray(lf_h.reshape(NTOK // 128, 128).T)}
            m.update(consts2)
            in2.append(m)
        r2 = _run(nc2, in2)
        del in2
        in3 = []
        for c in range(NCORE):
            oT_c = np.concatenate([np.asarray(r2[hd]["oT"])[:, c * TC:(c + 1) * TC] for hd in range(NH)], axis=0)
            cu_prev = np.zeros((DC, 2), f32) if c % cores_per_seq == 0 else np.asarray(r1[c - 1]["cu_tail"])
            in3.append({"zT": np.asarray(r1[c]["zT"]), "oT": np.ascontiguousarray(oT_c), "gT": np.asarray(r1[c]["gT"]),
                        "cu_prev": _col(cu_prev), "b_head": _col(np.asarray(r1[c]["b_head"])), "convw": convw,
                        "wA_b": wb["wA"][l * DC:(l + 1) * DC], "wB_b": wb["wB"][l * DA:(l + 1) * DA], "wo_b": wb["wo"][l * D:(l + 1) * D],
                        "h": h[c]})
        r3 = _run(nc3, in3)
        del in3, r1, r2
        h1 = [np.asarray(r3[c]["h1"]) for c in range(NCORE)]
        gcol2 = np.ascontiguousarray(A(ffn_norm[l]).reshape(16, 128).T)
        i = l // 2
        if l % 2 == 0:
            nc4 = _prog("ffn_dense", lambda: build_ffn(TC, False))
            r4 = _run(nc4, [{"h": h1[c], "gcol": gcol2, "ident": ident, "wg_b": wb["dg"][i * D:(i + 1) * D],
                             "wu_b": wb["du"][i * D:(i + 1) * D], "wd_b": wb["dd"][i * DFF:(i + 1) * DFF]} for c in range(NCORE)])
            h = [np.asarray(r4[c]["hout"]) for c in range(NCORE)]
        else:
            nc5 = _prog("ffn_moe", lambda: build_ffn(TC, True))
            rw = np.ascontiguousarray(A(router_w[i]).reshape(16, 128, NE).transpose(1, 0, 2))
            rb = np.ascontiguousarray(np.broadcast_to(A(router_b[i]), (128, NE)))
            gfin = np.ascontiguousarray(np.broadcast_to(A(final_norm), (128, D)))
            r5 = _run(nc5, [{"h": h1[c], "gcol": gcol2, "ident": ident, "wg_b": wb["mg"][i * NE * D:(i + 1) * NE * D],
                             "wu_b": wb["mu"][i * NE * D:(i + 1) * NE * D], "wd_b": wb["md"][i * NE * DFE:(i + 1) * NE * DFE],
                             "rw": rw, "rb": rb, "gfin": gfin, "ident32": np.eye(128, dtype=f32)} for c in range(NCORE)])
            h = [np.asarray(r5[c]["hout"]) for c in range(NCORE)]
    out = np.concatenate(h, axis=0).reshape(B, S, D).astype(f32)
    return out
```

```python
import numpy as np
import ml_dtypes
from contextlib import ExitStack
import concourse.bass as bass
import concourse.mybir as mybir
from concourse.bass_utils import run_bass_kernel_spmd

F32 = mybir.dt.float32
BF16 = mybir.dt.bfloat16
AF = mybir.ActivationFunctionType
ALU = mybir.AluOpType

D = 2048
NCORE = 8
DC = 1024
DA = 1024
NH = 8
HD = 128
PTOT = 3 * DC + 3 * DA + NH + 2 * D
DFF = 5632
NE = 8
DFE = 7168
EPS = 1e-6
TT = 512


class Sem:
    def __init__(self, kb, name):
        self.h = kb.es.enter_context(kb.nc.semaphore(name))
        self.v = 0


class Tile:
    def __init__(self, kb, ap, name):
        self.kb = kb
        self.ap = ap
        self.name = name
        self.w = []
        self.r = []
        self.pr = []
        self.dsem = None

    def dma_sem(self):
        if self.dsem is None:
            self.dsem = self.kb.new_dma_sem(self.name)
        return self.dsem

    def __getitem__(self, idx):
        return self.ap[idx]


class KB:
    def __init__(self):
        self.nc = bass.Bass("TRN2", target_bir_lowering=False)
        self.es = ExitStack()
        self.waited = {}
        self.esem = {}
        self.dma_sems = []
        self.free_dma_sems = []
        self.n_ins = 0
        nc = self.nc
        self.engs = {"pe": nc.tensor, "act": nc.scalar, "dve": nc.vector, "pool": nc.gpsimd, "sp": nc.sync}
        for k in self.engs:
            self.esem[k] = Sem(self, "e_" + k)
        self.pending = {k: False for k in self.engs}
        self.es_phase = None
        self.sem_pool = []
        self.phase_sems = []
        self.nuid = 0

    def phase_begin(self):
        self.es_phase = ExitStack()
        self.phase_sems = []

    def phase_end(self, extra_sems=()):
        self.barrier(extra_sems)
        self.es_phase.close()
        self.es_phase = None
        self.sem_pool.extend(self.phase_sems)
        self.phase_sems = []

    def new_dma_sem(self, name):
        if self.sem_pool:
            s = self.sem_pool.pop()
        else:
            self.nuid += 1
            s = Sem(self, "d%d_" % self.nuid + name)
            self.dma_sems.append(s)
        self.phase_sems.append(s)
        return s

    def dram(self, name, shape, dt, kind="Internal"):
        return self.nc.dram_tensor(name, list(shape), dt, kind=kind).ap()

    def sbuf(self, name, shape, dt):
        self.nuid += 1
        es = self.es_phase if self.es_phase is not None else self.es
        t = es.enter_context(self.nc.sbuf_tensor("%s_%d" % (name, self.nuid), list(shape), dt))
        return Tile(self, t, name)

    def psum(self, name, shape, dt=F32):
        self.nuid += 1
        es = self.es_phase if self.es_phase is not None else self.es
        t = es.enter_context(self.nc.psum_tensor("%s_%d" % (name, self.nuid), list(shape), dt))
        return Tile(self, t, name)

    def dr(self, T, name, shape, dt, kind):
        if T is not None:
            return T.get(name)
        return self.dram(name, shape, dt, kind=kind)

    def _wait(self, ek, sem, val):
        key = (ek, id(sem))
        if self.waited.get(key, 0) >= val:
            return
        self.engs[ek].wait_ge(sem.h, val)
        self.waited[key] = val

    def _deps(self, ek, reads, writes):
        need = {}

        def add(s, v):
            k = id(s)
            if k not in need or need[k][1] < v:
                need[k] = (s, v)
        for t in reads:
            for (s, v, e) in t.w:
                add(s, v)
        for t in writes:
            if t.r:
                t.pr = t.r
                t.r = []
                t.w = []
            for (s, v, e) in t.pr:
                if e != ek:
                    add(s, v)
        for (s, v) in need.values():
            self._wait(ek, s, v)

    def op(self, ek, fn, reads=(), writes=(), signal=True):
        self._deps(ek, reads, writes)
        ins = fn()
        self.n_ins += 1
        es = self.esem[ek]
        if signal:
            ins.then_inc(es.h, 1)
            es.v += 1
            val = es.v
            self.pending[ek] = False
        else:
            val = es.v + 1
            self.pending[ek] = True
        for t in reads:
            t.r.append((es, val, ek))
        for t in writes:
            t.w.append((es, val, ek))
        return ins

    def dma(self, qk, out_ap, in_ap, sb_tile, load, extra_reads=(), **kw):
        if load:
            self._deps(qk, list(extra_reads), [sb_tile])
        else:
            self._deps(qk, [sb_tile] + list(extra_reads), [])
        s = sb_tile.dma_sem()
        ins = self.engs[qk].dma_start(out=out_ap, in_=in_ap, **kw)
        ins.then_inc(s.h, 16)
        s.v += 16
        self.n_ins += 1
        if load:
            sb_tile.w.append((s, s.v, "dma"))
        else:
            sb_tile.r.append((s, s.v, "dma"))
        return ins

    def dram_dma(self, qk, out_ap, in_ap, sem, **kw):
        ins = self.engs[qk].dma_start(out=out_ap, in_=in_ap, **kw)
        ins.then_inc(sem.h, 16)
        sem.v += 16
        self.n_ins += 1
        return ins

    def barrier(self, extra_sems=()):
        for ek in self.engs:
            assert not self.pending[ek], ek
        allsems = list(self.esem.values()) + self.dma_sems + list(extra_sems)
        for ek in self.engs:
            for s in allsems:
                if s.v > 0:
                    self._wait(ek, s, s.v)

    def finish(self, extra_sems=()):
        self.barrier(extra_sems)
        self.es.close()
        return self.nc


def mm_group(kb, out_tile, out_ap, pairs, reads):
    n = len(pairs)
    for i, (l, r) in enumerate(pairs):
        kb.op("pe", lambda l=l, r=r, i=i: kb.nc.tensor.matmul(out_ap, l, r, start=(i == 0), stop=(i == n - 1)),
              reads=reads if i == 0 else (), writes=[out_tile] if i == 0 else (), signal=(i == n - 1))
    es = kb.esem["pe"]
    out_tile.w = [(es, es.v, "pe")]
    for t in reads:
        t.r.append((es, es.v, "pe"))


def cast_rows(kb, sem, src, dst, rows, cols, row_step=128):
    for r0 in range(0, rows, row_step):
        r1 = min(rows, r0 + row_step)
        kb.dram_dma("pool", dst[r0:r1, :], src[r0:r1, :], sem, max_dma_last_dim=4096)


def build_cast(specs):
    kb = KB()
    sem = Sem(kb, "cast")
    for (name, rows, cols) in specs:
        src = kb.dram(name, [rows, cols], F32, kind="ExternalInput")
        dst = kb.dram(name + "_b", [rows, cols], BF16, kind="ExternalOutput")
        cast_rows(kb, sem, src, dst, rows, cols)
    return kb.finish([sem])


class Consts:
    pass


def norm_transpose(kb, cs, h_dram, tok0, aT, hin_ring, ablk_ring, tp_ring, junk, ss_ring, gcol, ident, ctr, after_block=None):
    nc = kb.nc
    for blk in range(TT // 128):
        i = ctr["nb"]
        ctr["nb"] += 1
        hin = hin_ring[i % len(hin_ring)]
        ab = ablk_ring[i % len(ablk_ring)]
        ss = ss_ring[i % len(ss_ring)]
        r0 = tok0 + blk * 128
        kb.dma("sp", hin.ap[:, :], h_dram[r0:r0 + 128, :], hin, load=True)
        kb.op("act", lambda: nc.scalar.activation(out=ab.ap[:, :], in_=hin.ap[:, :], func=AF.Square,
                                                  accum_out=ss.ap[:, 0:1]), reads=[hin], writes=[ab, ss])
        kb.op("act", lambda: nc.scalar.activation(out=ss.ap[:, 1:2], in_=ss.ap[:, 0:1], func=AF.Sqrt,
                                                  bias=cs.eps.ap[:, 0:1], scale=1.0 / D), reads=[ss, cs.eps], writes=[ss])
        kb.op("dve", lambda: nc.vector.reciprocal(out=ss.ap[:, 2:3], in_=ss.ap[:, 1:2]), reads=[ss], writes=[ss])
        kb.op("dve", lambda: nc.vector.tensor_scalar(out=ab.ap[:, :], in0=hin.ap[:, :], scalar1=ss.ap[:, 2:3],
                                                     scalar2=None, op0=ALU.mult), reads=[hin, ss], writes=[ab])
        for half in range(2):
            tp = tp_ring[ctr["tp"] % len(tp_ring)]
            ctr["tp"] += 1
            for j in range(8):
                c = half * 8 + j
                kb.op("pe", lambda c=c, j=j: nc.tensor.transpose(tp.ap[:, j, :], ab.ap[:, c * 128:(c + 1) * 128], ident.ap[:, :]),
                      reads=[ab, ident] if j == 0 else (), writes=[tp] if j == 0 else (), signal=(j == 7))
            es = kb.esem["pe"]
            tp.w = [(es, es.v, "pe")]
            ab.r.append((es, es.v, "pe"))
            for j in range(8):
                c = half * 8 + j
                if True:
                    kb.op("dve", lambda c=c, j=j: nc.vector.tensor_scalar(out=aT.ap[:, c, blk * 128:(blk + 1) * 128], in0=tp.ap[:, j, :],
                                                                          scalar1=gcol.ap[:, c:c + 1], scalar2=None, op0=ALU.mult),
                          reads=[tp, gcol], writes=[aT])
                else:
                    kb.op("act", lambda c=c, j=j: nc.scalar.activation(out=aT.ap[:, c, blk * 128:(blk + 1) * 128], in_=tp.ap[:, j, :],
                                                                       func=AF.Copy, scale=gcol.ap[:, c:c + 1]),
                          reads=[tp, gcol], writes=[aT])
        if after_block is not None:
            after_block(blk)


def load_w(kb, wring, ctr, w_dram, k0, kc, c0, ncols):
    wt = wring[ctr["w"] % len(wring)]
    ctr["w"] += 1
    src = w_dram[k0 * 128:(k0 + kc) * 128, c0:c0 + ncols].rearrange("(c p) n -> p c n", p=128)
    kb.dma("sp", wt.ap[:, 0:kc, 0:ncols], src, wt, load=True)
    return wt


def build_p1(TC, kb=None, T=None):
    own = kb is None
    kb = kb or KB()
    if not own:
        kb.phase_begin()
    nc = kb.nc
    NT = TC // TT
    h = kb.dr(T, "h", [TC, D], F32, kind="ExternalInput")
    w_in = kb.dr(T, "w_in_b", [D, PTOT], BF16, kind="ExternalInput")
    gcol_d = kb.dr(T, "gcol", [128, 16], F32, kind="ExternalInput")
    convw_d = kb.dr(T, "convw", [128, 8, 3], F32, kind="ExternalInput")
    negb_d = kb.dr(T, "bf", [8, 1], F32, kind="ExternalInput")
    ident_d = kb.dr(T, "ident", [128, 128], BF16, kind="ExternalInput")
    zT = kb.dr(T, "zT", [DC, TC], BF16, kind="ExternalOutput")
    cu_tail = kb.dr(T, "cu_tail", [DC, 2], F32, kind="ExternalOutput")
    b_head = kb.dr(T, "b_head", [DC, 2], F32, kind="ExternalOutput")
    qT = kb.dr(T, "qT", [DA, TC], BF16, kind="ExternalOutput")
    kT = kb.dr(T, "kT", [DA, TC], BF16, kind="ExternalOutput")
    vv = kb.dr(T, "v", [TC, DA], BF16, kind="ExternalOutput")
    lf = kb.dr(T, "lf", [NH, TC], F32, kind="ExternalOutput")
    gT = kb.dr(T, "gT", [2 * D, TC], BF16, kind="ExternalOutput")

    cs = Consts()
    cs.eps = kb.sbuf("eps", [128, 1], F32)
    gcol = kb.sbuf("gcol_s", [128, 16], F32)
    convw = kb.sbuf("convw_s", [128, 8, 3], F32)
    bfs = kb.sbuf("bf_s", [8, 2], F32)
    ident = kb.sbuf("ident_s", [128, 128], BF16)
    kb.op("dve", lambda: nc.vector.memset(cs.eps.ap[:, :], EPS), writes=[cs.eps])
    kb.dma("sp", gcol.ap[:, :], gcol_d[:, :], gcol, load=True)
    kb.dma("sp", convw.ap[:, :, :], convw_d[:, :, :], convw, load=True)
    kb.dma("sp", bfs.ap[:, 0:1], negb_d[:, :], bfs, load=True)
    kb.dma("sp", ident.ap[:, :], ident_d[:, :], ident, load=True)
    kb.op("dve", lambda: nc.vector.tensor_scalar(out=bfs.ap[:, 1:2], in0=bfs.ap[:, 0:1], scalar1=-1.0, scalar2=None, op0=ALU.mult),
          reads=[bfs], writes=[bfs])

    hin_ring = [kb.sbuf(f"hin{i}", [128, D], F32) for i in range(2)]
    ablk_ring = [kb.sbuf(f"ablk{i}", [128, D], BF16) for i in range(2)]
    ss_ring = [kb.sbuf(f"ss{i}", [128, 4], F32) for i in range(4)]
    junk = None
    aT_ring = [kb.sbuf(f"aT{i}", [128, 16, TT], BF16) for i in range(2)]
    wring = [kb.sbuf(f"w{i}", [128, 16, 512], BF16) for i in range(3)]
    tp_ring = [kb.psum(f"tp{i}", [128, 8, 128], BF16) for i in range(2)]
    ps_ring = [kb.psum(f"ps{i}", [128, 512], F32) for i in range(6)]
    bcu = [kb.sbuf(f"bcu{i}", [128, 8, TT], BF16) for i in range(3)]
    cuext = [kb.sbuf(f"cuext{i}", [128, TT + 2], F32) for i in range(2)]
    carry = kb.sbuf("carry", [128, 8, 2], F32)
    ycv = [kb.sbuf(f"ycv{i}", [128, TT], F32) for i in range(2)]
    zst = [kb.sbuf(f"zst{i}", [128, 8, TT], BF16) for i in range(1)]
    qst = [kb.sbuf(f"qst{i}", [128, 4, TT], BF16) for i in range(3)]
    vst = [kb.sbuf(f"vst{i}", [128, 512], BF16) for i in range(2)]
    lfst = [kb.sbuf(f"lfst{i}", [8, 3, TT], F32) for i in range(2)]
    bh = kb.sbuf("bh", [128, 8, 2], F32)
    kb.op("pool", lambda: nc.gpsimd.memset(carry.ap[:, :, :], 0.0), writes=[carry])

    ctr = {"nb": 0, "tp": 0, "w": 0, "ps": 0, "q": 0, "v": 0, "ev": 0}

    def next_ps():
        p = ps_ring[ctr["ps"] % len(ps_ring)]
        ctr["ps"] += 1
        return p

    def evac_copy(out_tile, out_ap, ps):
        kb.op("dve", lambda: nc.vector.tensor_copy(out=out_ap, in_=ps.ap[:, :]), reads=[ps], writes=[out_tile])

    blocks = []
    for s in range(3):
        for cb in range(2):
            blocks.append(("bcu", s, cb, s * DC + cb * 512))
    for s in range(2):
        for cb in range(2):
            blocks.append(("qk", s, cb, 3 * DC + s * DA + cb * 512))
    for cb in range(2):
        blocks.append(("v", 0, cb, 3 * DC + 2 * DA + cb * 512))
    blocks.append(("f", 0, 0, 3 * DC + 3 * DA))
    for cb in range(8):
        blocks.append(("g", 0, cb, 3 * DC + 3 * DA + NH + cb * 512))

    import os
    SKIP = os.environ.get('P1_SKIP', '').split(',')
    blocks = [b for b in blocks if b[0] not in SKIP]
    norm_transpose(kb, cs, h, 0, aT_ring[0], hin_ring, ablk_ring, tp_ring, junk, ss_ring, gcol, ident, ctr)
    for t in range(NT):
        aT = aT_ring[t % 2]
        tok0 = t * TT
        if not blocks:
            break
        wt_next = load_w(kb, wring, ctr, w_in, 0, 16, blocks[0][3], 512)
        for bi, (kind, s, cb, c0) in enumerate(blocks):
            wt = wt_next
            if bi + 1 < len(blocks):
                nk = blocks[bi + 1]
                wt_next = load_w(kb, wring, ctr, w_in, 0, 16, nk[3], 8 if nk[0] == "f" else 512)
            if bi == min(6, len(blocks) - 1) and t + 1 < NT:
                norm_transpose(kb, cs, h, (t + 1) * TT, aT_ring[(t + 1) % 2], hin_ring, ablk_ring, tp_ring, junk, ss_ring, gcol, ident, ctr)
            if kind in ("bcu", "qk", "g"):
                if kind == "qk" or kind == "g":
                    st = qst[ctr["q"] % len(qst)]
                    ctr["q"] += 1
                for c in range(4):
                    ps = next_ps()
                    mm_group(kb, ps, ps.ap[:, :], [(wt.ap[:, k, c * 128:(c + 1) * 128], aT.ap[:, k, :]) for k in range(16)], [wt, aT])
                    if kind == "bcu":
                        evac_copy(bcu[s], bcu[s].ap[:, cb * 4 + c, :], ps)
                    elif kind == "qk":
                        evac_copy(st, st.ap[:, c, :], ps)
                    else:
                        kb.op("act", lambda c=c, ps=ps, st=st: nc.scalar.activation(out=st.ap[:, c, :], in_=ps.ap[:, :], func=AF.Sigmoid),
                              reads=[ps], writes=[st])
                if kind == "qk":
                    dst = (qT if s == 0 else kT)[cb * 512:(cb + 1) * 512, tok0:tok0 + TT].rearrange("(c p) t -> p c t", p=128)
                    kb.dma("sp", dst, st.ap[:, :, :], st, load=False)
                elif kind == "g":
                    dst = gT[cb * 512:(cb + 1) * 512, tok0:tok0 + TT].rearrange("(c p) t -> p c t", p=128)
                    kb.dma("sp", dst, st.ap[:, :, :], st, load=False)
                elif s == 2 and cb == 1 and 'conv' not in SKIP:
                    zs = zst[0]
                    for c in range(8):
                        ce = cuext[c % 2]
                        yc = ycv[c % 2]
                        kb.op("pool", lambda c=c, ce=ce: nc.gpsimd.tensor_copy(out=ce.ap[:, 0:2], in_=carry.ap[:, c, :]), reads=[carry], writes=[ce])
                        kb.op("pool", lambda c=c, ce=ce: nc.gpsimd.tensor_tensor(out=ce.ap[:, 2:TT + 2], in0=bcu[1].ap[:, c, :], in1=bcu[2].ap[:, c, :], op=ALU.mult),
                              reads=[bcu[1], bcu[2]], writes=[ce])
                        kb.op("pool", lambda c=c, ce=ce: nc.gpsimd.tensor_copy(out=carry.ap[:, c, :], in_=ce.ap[:, TT:TT + 2]), reads=[ce], writes=[carry])
                        kb.op("pool", lambda c=c, ce=ce, yc=yc: nc.gpsimd.tensor_scalar(out=yc.ap[:, :], in0=ce.ap[:, 0:TT], scalar1=convw.ap[:, c, 0:1], scalar2=None, op0=ALU.mult),
                              reads=[ce, convw], writes=[yc])
                        kb.op("dve", lambda c=c, ce=ce, yc=yc: nc.vector.scalar_tensor_tensor(out=yc.ap[:, :], in0=ce.ap[:, 1:TT + 1], scalar=convw.ap[:, c, 1:2], in1=yc.ap[:, :], op0=ALU.mult, op1=ALU.add),
                              reads=[ce, convw, yc], writes=[yc])
                        kb.op("dve", lambda c=c, ce=ce, yc=yc: nc.vector.scalar_tensor_tensor(out=yc.ap[:, :], in0=ce.ap[:, 2:TT + 2], scalar=convw.ap[:, c, 2:3], in1=yc.ap[:, :], op0=ALU.mult, op1=ALU.add),
                              reads=[ce, convw, yc], writes=[yc])
                        kb.op("pool", lambda c=c, yc=yc, zs=zs: nc.gpsimd.tensor_tensor(out=zs.ap[:, c, :], in0=yc.ap[:, :], in1=bcu[0].ap[:, c, :], op=ALU.mult),
                              reads=[yc, bcu[0]], writes=[zs])
                    dst = zT[:, tok0:tok0 + TT].rearrange("(c p) t -> p c t", p=128)
                    kb.dma("sp", dst, zs.ap[:, :, :], zs, load=False)
                    if t == 0:
                        kb.op("pool", lambda: nc.gpsimd.tensor_copy(out=bh.ap[:, :, :], in_=bcu[0].ap[:, :, 0:2]), reads=[bcu[0]], writes=[bh])
                        kb.dma("sp", b_head.rearrange("(c p) t -> p c t", p=128), bh.ap[:, :, :], bh, load=False)
                    if t == NT - 1:
                        kb.dma("sp", cu_tail.rearrange("(c p) t -> p c t", p=128), carry.ap[:, :, :], carry, load=False)
            elif kind == "v":
                for tb in range(TT // 128):
                    ps = next_ps()
                    mm_group(kb, ps, ps.ap[:, :], [(aT.ap[:, k, tb * 128:(tb + 1) * 128], wt.ap[:, k, :]) for k in range(16)], [wt, aT])
                    st = vst[ctr["v"] % 2]
                    ctr["v"] += 1
                    evac_copy(st, st.ap[:, :], ps)
                    kb.dma("sp", vv[tok0 + tb * 128: tok0 + (tb + 1) * 128, cb * 512:(cb + 1) * 512], st.ap[:, :], st, load=False)
            elif kind == "f":
                ps = next_ps()
                mm_group(kb, ps, ps.ap[0:8, :], [(wt.ap[:, k, 0:8], aT.ap[:, k, :]) for k in range(16)], [wt, aT])
                st = lfst[t % 2]
                kb.op("act", lambda ps=ps, st=st: nc.scalar.activation(out=st.ap[:, 0, :], in_=ps.ap[0:8, :], func=AF.Exp, bias=bfs.ap[:, 1:2], scale=-1.0),
                      reads=[ps, bfs], writes=[st])
                kb.op("act", lambda st=st: nc.scalar.activation(out=st.ap[:, 1, :], in_=st.ap[:, 0, :], func=AF.Ln, bias=1.0, scale=1.0),
                      reads=[st], writes=[st])
                kb.op("dve", lambda st=st: nc.vector.tensor_scalar(out=st.ap[:, 2, :], in0=st.ap[:, 1, :], scalar1=-1.0, scalar2=None, op0=ALU.mult),
                      reads=[st], writes=[st])
                kb.dma("sp", lf[:, tok0:tok0 + TT], st.ap[:, 2, :], st, load=False)
    if own:
        return kb.finish()
    kb.phase_end()


def build_p2(S, NSEQ, kb=None, T=None):
    own = kb is None
    kb = kb or KB()
    if not own:
        kb.phase_begin()
    nc = kb.nc
    NTOK = S * NSEQ
    NBS = S // 128
    NQ = S // TT
    qT = kb.dr(T, "qT", [128, NTOK], BF16, kind="ExternalInput")
    kT = kb.dr(T, "kT", [128, NTOK], BF16, kind="ExternalInput")
    vv = kb.dr(T, "v", [NTOK, 128], BF16, kind="ExternalInput")
    lfc_d = kb.dr(T, "lfcol", [128, NTOK // 128], F32, kind="ExternalInput")
    tri_d = kb.dr(T, "tri", [128, 128], F32, kind="ExternalInput")
    sel_d = kb.dr(T, "sel", [128, 128], F32, kind="ExternalInput")
    ones_d = kb.dr(T, "onesb", [128, 128], BF16, kind="ExternalInput")
    mask_d = kb.dr(T, "masks", [128, 4, TT], BF16, kind="ExternalInput")
    oT = kb.dr(T, "oT", [128, NTOK], BF16, kind="ExternalOutput")

    tri = kb.sbuf("tri_s", [128, 128], F32)
    sel = kb.sbuf("sel_s", [128, 128], F32)
    onesb = kb.sbuf("ones_s", [128, 128], BF16)
    masks = kb.sbuf("mask_s", [128, 4, TT], BF16)
    onesf = kb.sbuf("onesf", [128, NBS], F32)
    lfc = kb.sbuf("lfc", [128, NTOK // 128], F32)
    fused = T is not None and "qT_all" in T
    for (t, d) in ((tri, tri_d), (sel, sel_d), (onesb, ones_d)) + (() if fused else ((lfc, lfc_d),)):
        kb.dma("sp", t.ap[:, :], d[:, :], t, load=True)
    if fused:
        TCc = NTOK // NCORE
        CH = min(2048, TCc)
        VG = 8 * CH // 1024
        BPR = TCc // 128
        hmask = kb.sbuf("hmask", [128, 8], F32)
        id32 = kb.sbuf("id32", [128, 128], F32)
        X = kb.sbuf("X", [128, 8, CH], BF16)
        Xv = X.ap[:, :, :].rearrange("p a b -> p (a b)").rearrange("p (n f) -> p n f", f=1024)
        Xlf = kb.sbuf("Xlf", [128, 8, 128], F32)
        lfrow = kb.sbuf("lfrow", [128, 128], F32)
        kb.dma("sp", hmask.ap[:, :], T["hmask"][:, :], hmask, load=True)
        kb.dma("sp", id32.ap[:, :], T["ident32"][:, :], id32, load=True)
        kb.op("dve", lambda: nc.vector.memset(Xlf.ap[:, :, :], 0.0), writes=[Xlf])

        def select(out_tile, out_ap, src_tile, in_fn, mask=None):
            mk = mask or hmask
            kb.op("dve", lambda: nc.vector.tensor_scalar(out=out_ap, in0=in_fn(0), scalar1=mk.ap[:, 0:1], scalar2=None, op0=ALU.mult),
                  reads=[src_tile, mk], writes=[out_tile])
            for hh in range(1, 8):
                kb.op("dve", lambda hh=hh: nc.vector.scalar_tensor_tensor(out=out_ap, in0=in_fn(hh), scalar=mk.ap[:, hh:hh + 1], in1=out_ap, op0=ALU.mult, op1=ALU.add),
                      reads=[src_tile, mk, out_tile], writes=[out_tile])
    kb.dma("sp", masks.ap[:, :, :], mask_d[:, :, :], masks, load=True)
    kb.op("dve", lambda: nc.vector.memset(onesf.ap[:, :], 1.0), writes=[onesf])

    kTs = kb.sbuf("kTs", [128, S], BF16)
    qTs = kb.sbuf("qTs", [128, S], BF16)
    vs = kb.sbuf("vs", [128, NBS, 128], BF16)
    within = kb.sbuf("within", [128, NBS], F32)
    totb = kb.sbuf("totb", [128, NBS], F32)
    incl = kb.sbuf("incl", [128, NBS], F32)
    ccol = kb.sbuf("ccol", [128, NBS], F32)
    crefb = kb.sbuf("crefb", [128, NBS], F32)
    biasq = [kb.sbuf(f"biasq{i}", [128, NBS], F32) for i in range(2)]
    pt = [kb.sbuf(f"pt{i}", [128, TT], BF16) for i in range(3)]
    rs = [kb.sbuf(f"rs{i}", [128, TT], F32) for i in range(2)]
    ost = [kb.sbuf(f"ost{i}", [128, TT], BF16) for i in range(2)]
    ps_s = [kb.psum(f"pss{i}", [128, TT], F32) for i in range(3)]
    acc_o = [kb.psum(f"acco{i}", [128, TT], F32) for i in range(2)]
    acc_s = [kb.psum(f"accs{i}", [128, TT], F32) for i in range(2)]
    psc = kb.psum("psc", [128, 512], F32)
    scale = 1.0 / float(np.sqrt(HD))

    for s in range(NSEQ):
        t0 = s * S
        nch = 4
        if not fused:
            for i in range(nch):
                a, b = i * S // nch, (i + 1) * S // nch
                kb.dma("sp", kTs.ap[:, a:b], kT[:, t0 + a:t0 + b], kTs, load=True)
                kb.dma("sp", qTs.ap[:, a:b], qT[:, t0 + a:t0 + b], qTs, load=True)
                kb.dma("sp", vs.ap[:, a // 128:b // 128, :], vv[t0 + a:t0 + b, :].rearrange("(n p) d -> p n d", p=128), vs, load=True)
        else:
            rps = NCORE // NSEQ
            for rl in range(rps):
                r = s * rps + rl
                for (src_all, dst) in ((T["kT_all"], kTs), (T["qT_all"], qTs)):
                    for hf in range(TCc // CH):
                        kb.dma("sp", X.ap[:, :, :], src_all[r * DA:(r + 1) * DA, hf * CH:(hf + 1) * CH].rearrange("(h d) t -> d h t", d=128), X, load=True)
                        c0 = rl * TCc + hf * CH
                        select(dst, dst.ap[:, c0:c0 + CH], X, lambda hh: X.ap[:, hh, :])
                kb.dma("sp", Xlf.ap[rl * BPR:(rl + 1) * BPR, :, :], T["lf_all"][r * NH:(r + 1) * NH, :].rearrange("h (b p) -> b h p", p=128), Xlf, load=True)
            for g in range(NBS // VG):
                kb.dma("sp", Xv[:, 0:VG, :], T["v_all"][t0 + g * VG * 128:t0 + (g + 1) * VG * 128, :].rearrange("(n p) f -> p n f", p=128), X, load=True)
                select(vs, vs.ap[:, g * VG:(g + 1) * VG, :], X, lambda hh: Xv[:, 0:VG, hh * 128:(hh + 1) * 128])
            select(lfrow, lfrow.ap[:, :], Xlf, lambda hh: Xlf.ap[:, hh, :])
            kb.op("pe", lambda: nc.tensor.transpose(psc.ap[:, 0:128], lfrow.ap[:, :], id32.ap[:, :]), reads=[lfrow, id32], writes=[psc])
            kb.op("dve", lambda: nc.vector.tensor_copy(out=lfc.ap[:, s * NBS:(s + 1) * NBS], in_=psc.ap[:, 0:NBS]), reads=[psc], writes=[lfc])
        kb.op("pe", lambda: nc.tensor.matmul(psc.ap[:, 0:NBS], tri.ap[:, :], lfc.ap[:, s * NBS:(s + 1) * NBS], start=True, stop=True),
              reads=[tri, lfc], writes=[psc])
        kb.op("dve", lambda: nc.vector.tensor_copy(out=within.ap[:, :], in_=psc.ap[:, 0:NBS]), reads=[psc], writes=[within])
        kb.op("pe", lambda: nc.tensor.matmul(psc.ap[:, 0:NBS], sel.ap[:, :], within.ap[:, :], start=True, stop=True),
              reads=[sel, within], writes=[psc])
        kb.op("dve", lambda: nc.vector.tensor_copy(out=totb.ap[:, :], in_=psc.ap[:, 0:NBS]), reads=[psc], writes=[totb])
        kb.op("dve", lambda: nc.vector.tensor_tensor_scan(out=incl.ap[:, :], data0=onesf.ap[:, :], data1=totb.ap[:, :], initial=0.0,
                                                          op0=ALU.mult, op1=ALU.add), reads=[onesf, totb], writes=[incl])
        kb.op("dve", lambda: nc.vector.tensor_tensor(out=ccol.ap[:, :], in0=within.ap[:, :], in1=incl.ap[:, :], op=ALU.add),
              reads=[within, incl], writes=[ccol])
        kb.op("dve", lambda: nc.vector.tensor_tensor(out=ccol.ap[:, :], in0=ccol.ap[:, :], in1=totb.ap[:, :], op=ALU.subtract),
              reads=[ccol, totb], writes=[ccol])
        kb.op("pe", lambda: nc.tensor.matmul(psc.ap[:, 0:NBS], sel.ap[:, :], ccol.ap[:, :], start=True, stop=True),
              reads=[sel, ccol], writes=[psc])
        kb.op("dve", lambda: nc.vector.tensor_copy(out=crefb.ap[:, :], in_=psc.ap[:, 0:NBS]), reads=[psc], writes=[crefb])

        steps = [(qt, kbk) for qt in range(NQ) for kbk in range(4 * qt + 4)]

        def emit_s(i):
            qt, kbk = steps[i]
            p = ps_s[i % 3]
            kb.op("pe", lambda: nc.tensor.matmul(p.ap[:, :], kTs.ap[:, kbk * 128:(kbk + 1) * 128], qTs.ap[:, qt * TT:(qt + 1) * TT], start=True, stop=True),
                  reads=[kTs, qTs], writes=[p])

        emit_s(0)
        for i, (qt, kbk) in enumerate(steps):
            nk = 4 * qt + 4
            bq = biasq[qt % 2]
            if kbk == 0:
                kb.op("dve", lambda: nc.vector.tensor_scalar(out=bq.ap[:, 0:nk], in0=ccol.ap[:, 0:nk], scalar1=-1.0,
                                                             scalar2=crefb.ap[:, 4 * qt + 1:4 * qt + 2], op0=ALU.mult, op1=ALU.add),
                      reads=[ccol, crefb], writes=[bq])
            if i + 1 < len(steps):
                emit_s(i + 1)
            p = ps_s[i % 3]
            ptile = pt[i % 3]
            kb.op("act", lambda: nc.scalar.activation(out=ptile.ap[:, :], in_=p.ap[:, :], func=AF.Exp, bias=bq.ap[:, kbk:kbk + 1], scale=scale),
                  reads=[p, bq], writes=[ptile])
            if kbk >= 4 * qt:
                j = kbk - 4 * qt
                kb.op("pool", lambda: nc.gpsimd.tensor_tensor(out=ptile.ap[:, :], in0=ptile.ap[:, :], in1=masks.ap[:, j, :], op=ALU.mult),
                      reads=[ptile, masks], writes=[ptile])
            ao = acc_o[qt % 2]
            asum = acc_s[qt % 2]
            first, last = (kbk == 0), (kbk == nk - 1)
            kb.op("pe", lambda: nc.tensor.matmul(ao.ap[:, :], vs.ap[:, kbk, :], ptile.ap[:, :], start=first, stop=last),
                  reads=[vs, ptile], writes=[ao] if first else (), signal=False)
            kb.op("pe", lambda: nc.tensor.matmul(asum.ap[:, :], onesb.ap[:, :], ptile.ap[:, :], start=first, stop=last),
                  reads=[onesb, ptile], writes=[asum] if first else (), signal=True)
            if last:
                es = kb.esem["pe"]
                ao.w = [(es, es.v, "pe")]
                asum.w = [(es, es.v, "pe")]
                r = rs[qt % 2]
                o = ost[qt % 2]
                kb.op("dve", lambda: nc.vector.reciprocal(out=r.ap[:, :], in_=asum.ap[:, :]), reads=[asum], writes=[r])
                kb.op("dve", lambda: nc.vector.tensor_tensor(out=o.ap[:, :], in0=ao.ap[:, :], in1=r.ap[:, :], op=ALU.mult), reads=[ao, r], writes=[o])
                kb.dma("sp", oT[:, t0 + qt * TT:t0 + (qt + 1) * TT], o.ap[:, :], o, load=False)
    if own:
        return kb.finish()
    kb.phase_end()


def p2_consts():
    bf = ml_dtypes.bfloat16
    kk = np.arange(128)
    tri = (kk[:, None] <= kk[None, :]).astype(np.float32)
    sel = np.zeros((128, 128), np.float32)
    sel[127, :] = 1.0
    masks = np.zeros((128, 4, TT), np.float32)
    qq = np.arange(TT)
    for j in range(4):
        masks[:, j, :] = ((j * 128 + kk)[:, None] <= qq[None, :])
    return {"tri": tri, "sel": sel, "onesb": np.ones((128, 128), np.float32).astype(bf), "masks": masks.astype(bf)}


def build_p3(TC, kb=None, T=None):
    own = kb is None
    kb = kb or KB()
    if not own:
        kb.phase_begin()
    nc = kb.nc
    NT = TC // TT
    zT = kb.dr(T, "zT", [DC, TC], BF16, kind="ExternalInput")
    oT = kb.dr(T, "oT", [DA, TC], BF16, kind="ExternalInput")
    gT = kb.dr(T, "gT", [2 * D, TC], BF16, kind="ExternalInput")
    cup_d = kb.dr(T, "cu_prev", [128, 8, 2], F32, kind="ExternalInput")
    bh_d = kb.dr(T, "b_head", [128, 8, 2], F32, kind="ExternalInput")
    convw_d = kb.dr(T, "convw", [128, 8, 3], F32, kind="ExternalInput")
    wA = kb.dr(T, "wA_b", [DC, D], BF16, kind="ExternalInput")
    wB = kb.dr(T, "wB_b", [DA, D], BF16, kind="ExternalInput")
    wo = kb.dr(T, "wo_b", [D, D], BF16, kind="ExternalInput")
    h = kb.dr(T, "h", [TC, D], F32, kind="ExternalInput")
    h1 = kb.dr(T, "h1", [TC, D], F32, kind="ExternalOutput")

    cup = kb.sbuf("cup", [128, 8, 2], F32)
    bh = kb.sbuf("bh", [128, 8, 2], F32)
    convw = kb.sbuf("convw_s", [128, 8, 3], F32)
    fx = kb.sbuf("fx", [128, 8, 4], F32)
    fused = T is not None and "o_all" in T
    for (t, d) in ((bh, bh_d), (convw, convw_d)) + (() if fused else ((cup, cup_d),)):
        kb.dma("sp", t.ap[:, :, :], d[:, :, :], t, load=True)
    if fused:
        hmask = kb.sbuf("hmask", [128, 8], F32)
        pmask = kb.sbuf("pmask", [128, 8], F32)
        kb.dma("sp", hmask.ap[:, :], T["hmask"][:, :], hmask, load=True)
        kb.dma("sp", pmask.ap[:, :], T["pmask"][:, :], pmask, load=True)
        Xc = kb.sbuf("Xc", [128, 8, 8, 2], F32)
        kb.dma("sp", Xc.ap[:, :, :, :], T["cu_all"].rearrange("(r c p) t -> p r c t", p=128, c=8), Xc, load=True)
        Xo = [kb.sbuf(f"Xo{i}", [128, 8, TT], BF16) for i in range(2)]

        def select(out_tile, out_ap, src_tile, in_fn, mk):
            kb.op("dve", lambda: nc.vector.tensor_scalar(out=out_ap, in0=in_fn(0), scalar1=mk.ap[:, 0:1], scalar2=None, op0=ALU.mult),
                  reads=[src_tile, mk], writes=[out_tile])
            for hh in range(1, 8):
                kb.op("dve", lambda hh=hh: nc.vector.scalar_tensor_tensor(out=out_ap, in0=in_fn(hh), scalar=mk.ap[:, hh:hh + 1], in1=out_ap, op0=ALU.mult, op1=ALU.add),
                      reads=[src_tile, mk, out_tile], writes=[out_tile])
        select(cup, cup.ap[:, :, :], Xc, lambda r: Xc.ap[:, r, :, :], pmask)
    wring = [kb.sbuf(f"w{i}", [128, 16, 512], BF16) for i in range(3)]
    zt_r = [kb.sbuf(f"zt{i}", [128, 8, TT], BF16) for i in range(2)]
    ot_r = [kb.sbuf(f"ot{i}", [128, 8, TT], BF16) for i in range(2)]
    gt = kb.sbuf("gt", [128, 32, TT], BF16)
    mT = kb.sbuf("mT", [128, 16, TT], BF16)
    hall = kb.sbuf("hall", [128, 4, D], F32)
    tmp = [kb.sbuf(f"tmp{i}", [128, TT], F32) for i in range(4)]
    ps_ring = [kb.psum(f"ps{i}", [128, 512], F32) for i in range(8)]
    ctr = {"w": 0, "ps": 0, "tmp": 0}

    def next_ps():
        p = ps_ring[ctr["ps"] % len(ps_ring)]
        ctr["ps"] += 1
        return p

    V = nc.vector
    kb.op("dve", lambda: V.tensor_tensor(out=fx.ap[:, :, 0], in0=cup.ap[:, :, 0], in1=convw.ap[:, :, 0], op=ALU.mult), reads=[cup, convw], writes=[fx])
    kb.op("dve", lambda: V.tensor_tensor(out=fx.ap[:, :, 2], in0=cup.ap[:, :, 1], in1=convw.ap[:, :, 1], op=ALU.mult), reads=[cup, convw], writes=[fx])
    kb.op("dve", lambda: V.tensor_tensor(out=fx.ap[:, :, 0], in0=fx.ap[:, :, 0], in1=fx.ap[:, :, 2], op=ALU.add), reads=[fx], writes=[fx])
    kb.op("dve", lambda: V.tensor_tensor(out=fx.ap[:, :, 0], in0=fx.ap[:, :, 0], in1=bh.ap[:, :, 0], op=ALU.mult), reads=[fx, bh], writes=[fx])
    kb.op("dve", lambda: V.tensor_tensor(out=fx.ap[:, :, 1], in0=cup.ap[:, :, 1], in1=convw.ap[:, :, 0], op=ALU.mult), reads=[cup, convw], writes=[fx])
    kb.op("dve", lambda: V.tensor_tensor(out=fx.ap[:, :, 1], in0=fx.ap[:, :, 1], in1=bh.ap[:, :, 1], op=ALU.mult), reads=[fx, bh], writes=[fx])

    for t in range(NT):
        tok0 = t * TT
        zt, ot = zt_r[t % 2], ot_r[t % 2]
        kb.dma("sp", zt.ap[:, :, :], zT[:, tok0:tok0 + TT].rearrange("(c p) t -> p c t", p=128), zt, load=True)
        if not fused:
            kb.dma("sp", ot.ap[:, :, :], oT[:, tok0:tok0 + TT].rearrange("(c p) t -> p c t", p=128), ot, load=True)
        else:
            for hh in range(8):
                xo = Xo[hh % 2]
                kb.dma("sp", xo.ap[:, :, :], T["o_all"][hh * 128:(hh + 1) * 128, :].rearrange("d (c t) -> d c t", c=8)[:, :, tok0:tok0 + TT], xo, load=True)
                select(ot, ot.ap[:, hh, :], xo, lambda c_, xo=xo: xo.ap[:, c_, :], hmask)
        for q4 in range(4):
            kb.dma("sp", gt.ap[:, q4 * 8:(q4 + 1) * 8, :], gT[q4 * 1024:(q4 + 1) * 1024, tok0:tok0 + TT].rearrange("(c p) t -> p c t", p=128), gt, load=True)
        kb.dma("sp", hall.ap[:, :, :], h[tok0:tok0 + TT, :].rearrange("(n p) d -> p n d", p=128), hall, load=True)
        if t == 0:
            kb.op("dve", lambda: V.tensor_tensor(out=zt.ap[:, :, 0:2], in0=zt.ap[:, :, 0:2], in1=fx.ap[:, :, 0:2], op=ALU.add), reads=[zt, fx], writes=[zt])
        for jb in range(4):
            wt = wring[ctr["w"] % len(wring)]
            ctr["w"] += 1
            kb.dma("sp", wt.ap[:, 0:8, :], wA[:, jb * 512:(jb + 1) * 512].rearrange("(c p) n -> p c n", p=128), wt, load=True)
            kb.dma("sp", wt.ap[:, 8:16, :], wB[:, jb * 512:(jb + 1) * 512].rearrange("(c p) n -> p c n", p=128), wt, load=True)
            for c in range(4):
                j = jb * 4 + c
                pa = next_ps()
                mm_group(kb, pa, pa.ap[:, :], [(wt.ap[:, k, c * 128:(c + 1) * 128], zt.ap[:, k, :]) for k in range(8)], [wt, zt])
                pb = next_ps()
                mm_group(kb, pb, pb.ap[:, :], [(wt.ap[:, 8 + k, c * 128:(c + 1) * 128], ot.ap[:, k, :]) for k in range(8)], [wt, ot])
                t1 = tmp[ctr["tmp"] % 4]
                t2 = tmp[(ctr["tmp"] + 1) % 4]
                ctr["tmp"] += 2
                kb.op("dve", lambda: V.tensor_tensor(out=t1.ap[:, :], in0=pa.ap[:, :], in1=gt.ap[:, j, :], op=ALU.mult), reads=[pa, gt], writes=[t1])
                kb.op("dve", lambda: V.tensor_tensor(out=t2.ap[:, :], in0=pb.ap[:, :], in1=gt.ap[:, 16 + j, :], op=ALU.mult), reads=[pb, gt], writes=[t2])
                kb.op("pool", lambda: nc.gpsimd.tensor_tensor(out=mT.ap[:, j, :], in0=t1.ap[:, :], in1=t2.ap[:, :], op=ALU.add), reads=[t1, t2], writes=[mT])
        for cb in range(4):
            wt = load_w(kb, wring, ctr, wo, 0, 16, cb * 512, 512)
            for tb in range(4):
                p = next_ps()
                mm_group(kb, p, p.ap[:, :], [(mT.ap[:, k, tb * 128:(tb + 1) * 128], wt.ap[:, k, :]) for k in range(16)], [wt, mT])
                kb.op("dve", lambda: V.tensor_tensor(out=hall.ap[:, tb, cb * 512:(cb + 1) * 512], in0=p.ap[:, :], in1=hall.ap[:, tb, cb * 512:(cb + 1) * 512], op=ALU.add),
                      reads=[p, hall], writes=[hall])
        kb.dma("sp", h1[tok0:tok0 + TT, :].rearrange("(n p) d -> p n d", p=128), hall.ap[:, :, :], hall, load=False)
    if own:
        return kb.finish()
    kb.phase_end()


def build_ffn(TC, moe, kb=None, T=None):
    own = kb is None
    kb = kb or KB()
    if not own:
        kb.phase_begin()
    nc = kb.nc
    V = nc.vector
    NT = TC // TT
    DFX = DFE if moe else DFF
    NEX = NE if moe else 1
    NHALF = 2 if moe else 1
    NCB = DFX // 512 // NHALF
    KCH = DFX // 128 // NHALF
    h = kb.dr(T, "h", [TC, D], F32, kind="ExternalInput")
    gcol_d = kb.dr(T, "gcol", [128, 16], F32, kind="ExternalInput")
    ident_d = kb.dr(T, "ident", [128, 128], BF16, kind="ExternalInput")
    wg = kb.dr(T, "wg_b", [NEX * D, DFX], BF16, kind="ExternalInput")
    wu = kb.dr(T, "wu_b", [NEX * D, DFX], BF16, kind="ExternalInput")
    wd = kb.dr(T, "wd_b", [NEX * DFX, D], BF16, kind="ExternalInput")
    out = kb.dr(T, "hout", [TC, D], F32, kind="ExternalOutput")

    cs = Consts()
    cs.eps = kb.sbuf("eps", [128, 1], F32)
    gcol = kb.sbuf("gcol_s", [128, 16], F32)
    ident = kb.sbuf("ident_s", [128, 128], BF16)
    kb.op("dve", lambda: V.memset(cs.eps.ap[:, :], EPS), writes=[cs.eps])
    kb.dma("sp", gcol.ap[:, :], gcol_d[:, :], gcol, load=True)
    kb.dma("sp", ident.ap[:, :], ident_d[:, :], ident, load=True)
    hin_ring = [kb.sbuf("hin0", [128, D], F32)]
    ablk_ring = [kb.sbuf("ablk0", [128, D], BF16)]
    ss_ring = [kb.sbuf(f"ss{i}", [128, 4], F32) for i in range(4)]
    aT = kb.sbuf("aT", [128, 16, TT], BF16)
    wring = [kb.sbuf(f"w{i}", [128, 16, 512], BF16) for i in range(3 if not moe else 4)]
    hdnT = kb.sbuf("hdnT", [128, KCH, TT], BF16)
    hall = kb.sbuf("hall", [128, 4, D], F32)
    sg = [kb.sbuf(f"sg{i}", [128, TT], F32) for i in range(2)]
    tp_ring = [kb.psum(f"tp{i}", [128, 8, 128], BF16) for i in range(2)]
    router = None
    if moe:
        rw_d = kb.dr(T, "rw", [128, 16, NE], F32, kind="ExternalInput")
        rb_d = kb.dr(T, "rb", [128, NE], F32, kind="ExternalInput")
        gfin_d = kb.dr(T, "gfin", [128, D], F32, kind="ExternalInput")
        id32_d = kb.dr(T, "ident32", [128, 128], F32, kind="ExternalInput")
        rw = kb.sbuf("rw_s", [128, 16, NE], F32)
        rb = kb.sbuf("rb_s", [128, NE], F32)
        gfin = kb.sbuf("gfin_s", [128, D], F32)
        id32 = kb.sbuf("id32_s", [128, 128], F32)
        kb.dma("sp", rw.ap[:, :, :], rw_d[:, :, :], rw, load=True)
        kb.dma("sp", rb.ap[:, :], rb_d[:, :], rb, load=True)
        kb.dma("sp", gfin.ap[:, :], gfin_d[:, :], gfin, load=True)
        kb.dma("sp", id32.ap[:, :], id32_d[:, :], id32, load=True)
        ab32 = kb.sbuf("ab32", [128, D], F32)
        a32T = [kb.sbuf(f"a32T{i}", [128, 4, 128], F32) for i in range(2)]
        tp32 = kb.psum("tp32", [128, 4, 128], F32)
        psl = kb.psum("psl", [128, 512], F32)
        Wall = kb.sbuf("Wall", [128, 4, NE], F32)
        rt = kb.sbuf("rt", [128, 8, NE], F32)
        router = True
    ps_ring = [kb.psum(f"ps{i}", [128, 512], F32) for i in range(4 if moe else 6)]
    ctr = {"nb": 0, "tp": 0, "w": 0, "ps": 0, "sg": 0, "a32": 0}

    def next_ps():
        p = ps_ring[ctr["ps"] % len(ps_ring)]
        ctr["ps"] += 1
        return p

    def route(blk):
        hin = hin_ring[0]
        ss = ss_ring[(ctr["nb"] - 1) % len(ss_ring)]
        kb.op("dve", lambda: V.tensor_scalar(out=ab32.ap[:, :], in0=hin.ap[:, :], scalar1=ss.ap[:, 2:3], scalar2=None, op0=ALU.mult),
              reads=[hin, ss], writes=[ab32])
        for r in range(4):
            for j in range(4):
                c = r * 4 + j
                kb.op("pe", lambda c=c, j=j: nc.tensor.transpose(tp32.ap[:, j, :], ab32.ap[:, c * 128:(c + 1) * 128], id32.ap[:, :]),
                      reads=[ab32, id32] if j == 0 else (), writes=[tp32] if j == 0 else (), signal=(j == 3))
            es = kb.esem["pe"]
            tp32.w = [(es, es.v, "pe")]
            ab32.r.append((es, es.v, "pe"))
            a3 = a32T[ctr["a32"] % 2]
            ctr["a32"] += 1
            for j in range(4):
                c = r * 4 + j
                kb.op("dve", lambda c=c, j=j: V.tensor_scalar(out=a3.ap[:, j, :], in0=tp32.ap[:, j, :], scalar1=gcol.ap[:, c:c + 1], scalar2=None, op0=ALU.mult),
                      reads=[tp32, gcol], writes=[a3])
            for j in range(4):
                c = r * 4 + j
                kb.op("pe", lambda c=c, j=j: nc.tensor.matmul(psl.ap[:, 0:NE], a3.ap[:, j, :], rw.ap[:, c, :], start=(c == 0), stop=(c == 15)),
                      reads=[a3, rw], writes=[psl] if c == 0 else (), signal=(j == 3))
        es = kb.esem["pe"]
        psl.w = [(es, es.v, "pe")]
        L, m1k, L2, m2k = rt.ap[:, 0, :], rt.ap[:, 1, :], rt.ap[:, 2, :], rt.ap[:, 3, :]
        sc = rt.ap[:, 4, :]
        AX = mybir.AxisListType.X
        kb.op("dve", lambda: V.tensor_tensor(out=L, in0=psl.ap[:, 0:NE], in1=rb.ap[:, :], op=ALU.add), reads=[psl, rb], writes=[rt])
        kb.op("dve", lambda: V.tensor_reduce(out=sc[:, 0:1], in_=L, axis=AX, op=ALU.max), reads=[rt], writes=[rt])
        kb.op("dve", lambda: V.tensor_scalar(out=m1k, in0=L, scalar1=sc[:, 0:1], scalar2=None, op0=ALU.is_equal), reads=[rt], writes=[rt])
        kb.op("dve", lambda: V.scalar_tensor_tensor(out=L2, in0=m1k, scalar=-1e30, in1=L, op0=ALU.mult, op1=ALU.add), reads=[rt], writes=[rt])
        kb.op("dve", lambda: V.tensor_reduce(out=sc[:, 1:2], in_=L2, axis=AX, op=ALU.max), reads=[rt], writes=[rt])
        kb.op("dve", lambda: V.tensor_scalar(out=m2k, in0=L2, scalar1=sc[:, 1:2], scalar2=None, op0=ALU.is_equal), reads=[rt], writes=[rt])
        kb.op("dve", lambda: V.tensor_tensor(out=sc[:, 2:3], in0=sc[:, 1:2], in1=sc[:, 0:1], op=ALU.subtract), reads=[rt], writes=[rt])
        kb.op("act", lambda: nc.scalar.activation(out=sc[:, 3:4], in_=sc[:, 2:3], func=AF.Sigmoid), reads=[rt], writes=[rt])
        kb.op("dve", lambda: V.tensor_scalar(out=sc[:, 4:5], in0=sc[:, 3:4], scalar1=-1.0, scalar2=1.0, op0=ALU.mult, op1=ALU.add), reads=[rt], writes=[rt])
        kb.op("dve", lambda: V.tensor_scalar(out=Wall.ap[:, blk, :], in0=m1k, scalar1=sc[:, 4:5], scalar2=None, op0=ALU.mult), reads=[rt], writes=[Wall])
        kb.op("dve", lambda: V.scalar_tensor_tensor(out=Wall.ap[:, blk, :], in0=m2k, scalar=sc[:, 3:4], in1=Wall.ap[:, blk, :], op0=ALU.mult, op1=ALU.add),
              reads=[rt, Wall], writes=[Wall])

    for t in range(NT):
        tok0 = t * TT
        norm_transpose(kb, cs, h, tok0, aT, hin_ring, ablk_ring, tp_ring, None, ss_ring, gcol, ident, ctr, after_block=(route if moe else None))
        kb.dma("sp", hall.ap[:, :, :], h[tok0:tok0 + TT, :].rearrange("(n p) d -> p n d", p=128), hall, load=True)
        for e, hf in [(e_, h_) for e_ in range(NEX) for h_ in range(NHALF)]:
            for cbk in range(NCB):
                wtg = load_w(kb, wring, ctr, wg, e * 16, 16, (hf * NCB + cbk) * 512, 512)
                wtu = load_w(kb, wring, ctr, wu, e * 16, 16, (hf * NCB + cbk) * 512, 512)
                for c in range(4):
                    pg = next_ps()
                    mm_group(kb, pg, pg.ap[:, :], [(wtg.ap[:, k, c * 128:(c + 1) * 128], aT.ap[:, k, :]) for k in range(16)], [wtg, aT])
                    pu = next_ps()
                    mm_group(kb, pu, pu.ap[:, :], [(wtu.ap[:, k, c * 128:(c + 1) * 128], aT.ap[:, k, :]) for k in range(16)], [wtu, aT])
                    s_ = sg[ctr["sg"] % 2]
                    ctr["sg"] += 1
                    kb.op("act", lambda: nc.scalar.activation(out=s_.ap[:, :], in_=pg.ap[:, :], func=AF.Silu), reads=[pg], writes=[s_])
                    kb.op("dve", lambda: V.tensor_tensor(out=hdnT.ap[:, cbk * 4 + c, :], in0=pu.ap[:, :], in1=s_.ap[:, :], op=ALU.mult), reads=[pu, s_], writes=[hdnT])
            kgs = [(k0, min(16, KCH - k0)) for k0 in range(0, KCH, 16)]
            for cb in range(4):
                pss = [next_ps() for _ in range(4)]
                for gi, (k0, kc) in enumerate(kgs):
                    wt = load_w(kb, wring, ctr, wd, (e * NHALF + hf) * KCH + k0, kc, cb * 512, 512)
                    for tb in range(4):
                        p = pss[tb]
                        for k in range(kc):
                            first = (gi == 0 and k == 0)
                            last = (gi == len(kgs) - 1 and k == kc - 1)
                            kb.op("pe", lambda k=k, p=p, tb=tb: nc.tensor.matmul(p.ap[:, :], hdnT.ap[:, k0 + k, tb * 128:(tb + 1) * 128], wt.ap[:, k, :], start=first, stop=last),
                                  reads=[hdnT, wt] if k == 0 else (), writes=[p] if first else (), signal=(k == kc - 1))
                        es = kb.esem["pe"]
                        wt.r.append((es, es.v, "pe"))
                        hdnT.r.append((es, es.v, "pe"))
                        if gi == len(kgs) - 1:
                            p.w = [(es, es.v, "pe")]
                for tb in range(4):
                    p = pss[tb]
                    dst = hall.ap[:, tb, cb * 512:(cb + 1) * 512]
                    if moe:
                        kb.op("dve", lambda p=p, dst=dst, tb=tb: V.scalar_tensor_tensor(out=dst, in0=p.ap[:, :], scalar=Wall.ap[:, tb, e:e + 1], in1=dst, op0=ALU.mult, op1=ALU.add),
                              reads=[p, hall, Wall], writes=[hall])
                    else:
                        kb.op("dve", lambda p=p, dst=dst: V.tensor_tensor(out=dst, in0=p.ap[:, :], in1=dst, op=ALU.add), reads=[p, hall], writes=[hall])
        if moe:
            hin = hin_ring[0]
            for tb in range(4):
                ss = ss_ring[tb]
                kb.op("act", lambda: nc.scalar.activation(out=hin.ap[:, :], in_=hall.ap[:, tb, :], func=AF.Square, accum_out=ss.ap[:, 0:1]), reads=[hall], writes=[hin, ss])
                kb.op("act", lambda: nc.scalar.activation(out=ss.ap[:, 1:2], in_=ss.ap[:, 0:1], func=AF.Sqrt, bias=cs.eps.ap[:, 0:1], scale=1.0 / D), reads=[ss, cs.eps], writes=[ss])
                kb.op("dve", lambda: V.reciprocal(out=ss.ap[:, 2:3], in_=ss.ap[:, 1:2]), reads=[ss], writes=[ss])
                kb.op("dve", lambda: V.scalar_tensor_tensor(out=hall.ap[:, tb, :], in0=hall.ap[:, tb, :], scalar=ss.ap[:, 2:3], in1=gfin.ap[:, :], op0=ALU.mult, op1=ALU.mult),
                      reads=[hall, ss, gfin], writes=[hall])
        kb.dma("sp", out[tok0:tok0 + TT, :].rearrange("(n p) d -> p n d", p=128), hall.ap[:, :, :], hall, load=False)
    if own:
        return kb.finish()
    kb.phase_end()


_PROG = {}


def _prog(key, fn):
    if key not in _PROG:
        _PROG[key] = fn()
    return _PROG[key]


def _run(nc, in_maps):
    res = run_bass_kernel_spmd(nc, in_maps, core_ids=list(range(NCORE)))
    return res.results


def _col(a):
    return np.ascontiguousarray(a.reshape(8, 128, a.shape[1]).transpose(1, 0, 2))


def kernel_unfused(x, mix_norm, w_in, b_forget, conv_w, w_conv_out, w_attn_out, w_o, ffn_norm,
           dense_w_gate, dense_w_up, dense_w_down, router_w, router_b,
           moe_w_gate, moe_w_up, moe_w_down, final_norm):
    f32 = np.float32
    bf = ml_dtypes.bfloat16
    B, S, _ = x.shape
    NTOK = B * S
    TC = NTOK // NCORE
    depth = w_in.shape[0]
    A = lambda a: np.ascontiguousarray(np.asarray(a, dtype=f32))

    wsrc = {
        "w_in": A(w_in).reshape(-1, PTOT), "wA": A(w_conv_out).reshape(-1, D), "wB": A(w_attn_out).reshape(-1, D),
        "wo": A(w_o).reshape(-1, D), "dg": A(dense_w_gate).reshape(-1, DFF), "du": A(dense_w_up).reshape(-1, DFF),
        "dd": A(dense_w_down).reshape(-1, D), "mg": A(moe_w_gate).reshape(-1, DFE), "mu": A(moe_w_up).reshape(-1, DFE),
        "md": A(moe_w_down).reshape(-1, D),
    }
    specs = [(n, a.shape[0] // NCORE, a.shape[1]) for n, a in wsrc.items()]
    nc0 = _prog("cast", lambda: build_cast(specs))
    in_maps = [{n: a[c * (a.shape[0] // NCORE):(c + 1) * (a.shape[0] // NCORE)] for n, a in wsrc.items()} for c in range(NCORE)]
    r0 = _run(nc0, in_maps)
    wb = {n: np.concatenate([np.asarray(r0[c][n + "_b"]) for c in range(NCORE)], axis=0) for n in wsrc}
    del wsrc, in_maps, r0

    ident = np.eye(128, dtype=f32).astype(bf)
    consts2 = p2_consts()
    h = [A(x).reshape(NTOK, D)[c * TC:(c + 1) * TC] for c in range(NCORE)]
    nc1 = _prog("p1", lambda: build_p1(TC))
    nc2 = _prog("p2", lambda: build_p2(S, B))
    nc3 = _prog("p3", lambda: build_p3(TC))
    cores_per_seq = NCORE // B
    out = None
    for l in range(depth):
        gcol = np.ascontiguousarray(A(mix_norm[l]).reshape(16, 128).T)
        convw = _col(np.ascontiguousarray(A(conv_w[l]).T))
        bfg = A(b_forget[l]).reshape(NH, 1)
        w_in_l = wb["w_in"][l * D:(l + 1) * D]
        r1 = _run(nc1, [{"h": h[c], "w_in_b": w_in_l, "gcol": gcol, "convw": convw, "bf": bfg, "ident": ident} for c in range(NCORE)])
        in2 = []
        for hd in range(NH):
            sl = slice(hd * HD, (hd + 1) * HD)
            lf_h = np.concatenate([np.asarray(r1[c]["lf"])[hd] for c in range(NCORE)])
            m = {"qT": np.concatenate([np.asarray(r1[c]["qT"])[sl] for c in range(NCORE)], axis=1),
                 "kT": np.concatenate([np.asarray(r1[c]["kT"])[sl] for c in range(NCORE)], axis=1),
                 "v": np.ascontiguousarray(np.concatenate([np.asarray(r1[c]["v"])[:, sl] for c in range(NCORE)], axis=0)),
                 "lfcol": np.ascontiguousarray(lf_h.reshape(NTOK // 128, 128).T)}
            m.update(consts2)
            in2.append(m)
        r2 = _run(nc2, in2)
        del in2
        in3 = []
        for c in range(NCORE):
            oT_c = np.concatenate([np.asarray(r2[hd]["oT"])[:, c * TC:(c + 1) * TC] for hd in range(NH)], axis=0)
            cu_prev = np.zeros((DC, 2), f32) if c % cores_per_seq == 0 else np.asarray(r1[c - 1]["cu_tail"])
            in3.append({"zT": np.asarray(r1[c]["zT"]), "oT": np.ascontiguousarray(oT_c), "gT": np.asarray(r1[c]["gT"]),
                        "cu_prev": _col(cu_prev), "b_head": _col(np.asarray(r1[c]["b_head"])), "convw": convw,
                        "wA_b": wb["wA"][l * DC:(l + 1) * DC], "wB_b": wb["wB"][l * DA:(l + 1) * DA], "wo_b": wb["wo"][l * D:(l + 1) * D],
                        "h": h[c]})
        r3 = _run(nc3, in3)
        del in3, r1, r2
        h1 = [np.asarray(r3[c]["h1"]) for c in range(NCORE)]
        gcol2 = np.ascontiguousarray(A(ffn_norm[l]).reshape(16, 128).T)
        i = l // 2
        if l % 2 == 0:
            nc4 = _prog("ffn_dense", lambda: build_ffn(TC, False))
            r4 = _run(nc4, [{"h": h1[c], "gcol": gcol2, "ident": ident, "wg_b": wb["dg"][i * D:(i + 1) * D],
                             "wu_b": wb["du"][i * D:(i + 1) * D], "wd_b": wb["dd"][i * DFF:(i + 1) * DFF]} for c in range(NCORE)])
            h = [np.asarray(r4[c]["hout"]) for c in range(NCORE)]
        else:
            nc5 = _prog("ffn_moe", lambda: build_ffn(TC, True))
            rw = np.ascontiguousarray(A(router_w[i]).reshape(16, 128, NE).transpose(1, 0, 2))
            rb = np.ascontiguousarray(np.broadcast_to(A(router_b[i]), (128, NE)))
            gfin = np.ascontiguousarray(np.broadcast_to(A(final_norm), (128, D)))
            r5 = _run(nc5, [{"h": h1[c], "gcol": gcol2, "ident": ident, "wg_b": wb["mg"][i * NE * D:(i + 1) * NE * D],
                             "wu_b": wb["mu"][i * NE * D:(i + 1) * NE * D], "wd_b": wb["md"][i * NE * DFE:(i + 1) * NE * DFE],
                             "rw": rw, "rb": rb, "gfin": gfin, "ident32": np.eye(128, dtype=f32)} for c in range(NCORE)])
            h = [np.asarray(r5[c]["hout"]) for c in range(NCORE)]
    out = np.concatenate(h, axis=0).reshape(B, S, D).astype(f32)
    return out


WSPECS = [
    ("w_in", 2 * D, PTOT), ("wA", 2 * DC, D), ("wB", 2 * DA, D), ("wo", 2 * D, D),
    ("dg", D, DFF), ("du", D, DFF), ("dd", DFF, D),
    ("mg", NE * D, DFE), ("mu", NE * D, DFE), ("md", NE * DFE, D),
]


def build_fused(S, B):
    kb = KB()
    nc = kb.nc
    NTOK = S * B
    TC = NTOK // NCORE
    I32 = mybir.dt.int32
    ein = lambda n, sh, dt: kb.dram(n, sh, dt, kind="ExternalInput")
    x = ein("x", [TC, D], F32)
    out = kb.dram("out", [TC, D], F32, kind="ExternalOutput")
    P = {}
    for l in range(2):
        P[f"gcol{l}"] = ein(f"gcol{l}", [128, 16], F32)
        P[f"gcolf{l}"] = ein(f"gcolf{l}", [128, 16], F32)
        P[f"convw{l}"] = ein(f"convw{l}", [128, 8, 3], F32)
        P[f"bf{l}"] = ein(f"bf{l}", [8, 1], F32)
    for (n, sh, dt) in (("ident", [128, 128], BF16), ("ident32", [128, 128], F32), ("tri", [128, 128], F32), ("sel", [128, 128], F32),
                        ("onesb", [128, 128], BF16), ("masks", [128, 4, TT], BF16), ("hmask", [128, 8], F32), ("pmask", [128, 8], F32),
                        ("rw", [128, 16, NE], F32), ("rb", [128, NE], F32), ("gfin", [128, D], F32)):
        P[n] = ein(n, sh, dt)
    W = {}
    cast = Sem(kb, "cast")
    ccs = {}
    rg = [list(range(NCORE))]
    for (n, rows, cols) in WSPECS:
        src = ein(n, [rows // NCORE, cols], F32)
        my = kb.dram(n + "_my", [rows // NCORE, cols], BF16)
        full = kb.dram(n + "_full", [rows, cols], BF16)
        cast_rows(kb, cast, src, my, rows // NCORE, cols)
        nc.gpsimd.wait_ge(cast.h, cast.v)
        ccs[n] = Sem(kb, "cc_" + n)
        nc.gpsimd.collective_compute("AllGather", ALU.bypass, replica_groups=rg, ins=[my[:, :]], outs=[full[:, :]]).then_inc(ccs[n].h, 1)
        ccs[n].v += 1
        W[n] = full

    def need(names):
        for ek in kb.engs:
            for n in names:
                kb._wait(ek, ccs[n], ccs[n].v)

    idr = lambda n, sh, dt: kb.dram(n, sh, dt)
    zT = idr("zT_s", [DC, TC], BF16)
    gT = idr("gT_s", [2 * D, TC], BF16)
    b_head = idr("bhead_s", [DC, 2], F32)
    cu_tail = idr("cutail_s", [DC, 2], F32)
    cu_all = idr("cuall_s", [NCORE * DC, 2], F32)
    qT_my = idr("qT_my", [DA, TC], BF16)
    kT_my = idr("kT_my", [DA, TC], BF16)
    v_my = idr("v_my", [TC, DA], BF16)
    lf_my = idr("lf_my", [NH, TC], F32)
    qT_all = idr("qT_all", [NCORE * DA, TC], BF16)
    kT_all = idr("kT_all", [NCORE * DA, TC], BF16)
    v_all = idr("v_all", [NTOK, DA], BF16)
    lf_all = idr("lf_all", [NCORE * NH, TC], F32)
    oT_h = idr("oT_h", [128, NTOK], BF16)
    o_all = idr("o_all", [NCORE * 128, NTOK], BF16)
    h1 = idr("h1_s", [TC, D], F32)
    h2 = idr("h2_s", [TC, D], F32)
    ccx = Sem(kb, "ccx")

    def gather(pairs):
        for (a, b) in pairs:
            nc.gpsimd.collective_compute("AllGather", ALU.bypass, replica_groups=rg, ins=[a[:, :]], outs=[b[:, :]]).then_inc(ccx.h, 1)
            ccx.v += 1
        for ek in kb.engs:
            kb._wait(ek, ccx, ccx.v)

    hcur = x
    for l in range(2):
        need(["w_in"])
        build_p1(TC, kb=kb, T={"h": hcur, "w_in_b": W["w_in"][l * D:(l + 1) * D, :], "gcol": P[f"gcol{l}"], "convw": P[f"convw{l}"], "bf": P[f"bf{l}"],
                               "ident": P["ident"], "zT": zT, "cu_tail": cu_tail, "b_head": b_head, "qT": qT_my, "kT": kT_my, "v": v_my, "lf": lf_my, "gT": gT})
        gather([(qT_my, qT_all), (kT_my, kT_all), (v_my, v_all), (lf_my, lf_all), (cu_tail, cu_all)])
        build_p2(S, B, kb=kb, T={"tri": P["tri"], "sel": P["sel"], "onesb": P["onesb"], "masks": P["masks"], "hmask": P["hmask"], "ident32": P["ident32"],
                                 "qT_all": qT_all, "kT_all": kT_all, "v_all": v_all, "lf_all": lf_all, "oT": oT_h})
        gather([(oT_h, o_all)])
        need(["wA", "wB", "wo"])
        build_p3(TC, kb=kb, T={"zT": zT, "gT": gT, "b_head": b_head.rearrange("(c p) t -> p c t", p=128), "convw": P[f"convw{l}"],
                               "wA_b": W["wA"][l * DC:(l + 1) * DC, :], "wB_b": W["wB"][l * DA:(l + 1) * DA, :], "wo_b": W["wo"][l * D:(l + 1) * D, :],
                               "h": hcur, "h1": h1, "hmask": P["hmask"], "pmask": P["pmask"], "cu_all": cu_all, "o_all": o_all})
        if l % 2 == 0:
            need(["dg", "du", "dd"])
            build_ffn(TC, False, kb=kb, T={"h": h1, "gcol": P[f"gcolf{l}"], "ident": P["ident"], "wg_b": W["dg"], "wu_b": W["du"], "wd_b": W["dd"], "hout": h2})
            hcur = h2
        else:
            need(["mg", "mu", "md"])
            build_ffn(TC, True, kb=kb, T={"h": h1, "gcol": P[f"gcolf{l}"], "ident": P["ident"], "wg_b": W["mg"], "wu_b": W["mu"], "wd_b": W["md"], "hout": out,
                                          "rw": P["rw"], "rb": P["rb"], "gfin": P["gfin"], "ident32": P["ident32"]})
    return kb.finish([cast, ccx] + list(ccs.values()))


def kernel_fused(x, mix_norm, w_in, b_forget, conv_w, w_conv_out, w_attn_out, w_o, ffn_norm,
           dense_w_gate, dense_w_up, dense_w_down, router_w, router_b,
           moe_w_gate, moe_w_up, moe_w_down, final_norm):
    f32 = np.float32
    bf = ml_dtypes.bfloat16
    B, S, _ = x.shape
    NTOK = B * S
    TC = NTOK // NCORE
    A = lambda a: np.ascontiguousarray(np.asarray(a, dtype=f32))
    wsrc = {
        "w_in": A(w_in).reshape(-1, PTOT), "wA": A(w_conv_out).reshape(-1, D), "wB": A(w_attn_out).reshape(-1, D),
        "wo": A(w_o).reshape(-1, D), "dg": A(dense_w_gate).reshape(-1, DFF), "du": A(dense_w_up).reshape(-1, DFF),
        "dd": A(dense_w_down).reshape(-1, D), "mg": A(moe_w_gate).reshape(-1, DFE), "mu": A(moe_w_up).reshape(-1, DFE),
        "md": A(moe_w_down).reshape(-1, D),
    }
    common = {"ident": np.eye(128, dtype=f32).astype(bf), "ident32": np.eye(128, dtype=f32)}
    common.update(p2_consts())
    for l in range(2):
        common[f"gcol{l}"] = np.ascontiguousarray(A(mix_norm[l]).reshape(16, 128).T)
        common[f"gcolf{l}"] = np.ascontiguousarray(A(ffn_norm[l]).reshape(16, 128).T)
        common[f"convw{l}"] = _col(np.ascontiguousarray(A(conv_w[l]).T))
        common[f"bf{l}"] = A(b_forget[l]).reshape(NH, 1)
    common["rw"] = np.ascontiguousarray(A(router_w[0]).reshape(16, 128, NE).transpose(1, 0, 2))
    common["rb"] = np.ascontiguousarray(np.broadcast_to(A(router_b[0]), (128, NE)))
    common["gfin"] = np.ascontiguousarray(np.broadcast_to(A(final_norm), (128, D)))
    xf = A(x).reshape(NTOK, D)
    cps = NCORE // B
    in_maps = []
    for c in range(NCORE):
        m = dict(common)
        m["x"] = xf[c * TC:(c + 1) * TC]
        hm = np.zeros((128, 8), f32)
        hm[:, c] = 1.0
        pm = np.zeros((128, 8), f32)
        if c % cps != 0:
            pm[:, c - 1] = 1.0
        m["hmask"] = hm
        m["pmask"] = pm
        for n, a in wsrc.items():
            r = a.shape[0] // NCORE
            m[n] = a[c * r:(c + 1) * r]
        in_maps.append(m)
    nc = _prog(("fused", S, B), lambda: build_fused(S, B))
    res = run_bass_kernel_spmd(nc, in_maps, core_ids=list(range(NCORE)))
    return np.concatenate([np.asarray(res.results[c]["out"]) for c in range(NCORE)], axis=0).reshape(B, S, D).astype(f32)


def build_mid(TC, moe, with_p1):
    kb = KB()
    E = lambda n, sh, dt: kb.dram(n, sh, dt, kind="ExternalInput")
    O = lambda n, sh, dt: kb.dram(n, sh, dt, kind="ExternalOutput")
    h1 = kb.dram("h1_i", [TC, D], F32)
    ident = E("ident", [128, 128], BF16)
    T3 = {"zT": E("zT", [DC, TC], BF16), "oT": E("oT", [DA, TC], BF16), "gT": E("gT", [2 * D, TC], BF16),
          "cu_prev": E("cu_prev", [128, 8, 2], F32), "b_head": E("b_head", [128, 8, 2], F32), "convw": E("convw", [128, 8, 3], F32),
          "wA_b": E("wA_b", [DC, D], BF16), "wB_b": E("wB_b", [DA, D], BF16), "wo_b": E("wo_b", [D, D], BF16),
          "h": E("h", [TC, D], F32), "h1": h1}
    build_p3(TC, kb=kb, T=T3)
    DFX = DFE if moe else DFF
    NEX = NE if moe else 1
    hout = O("hout", [TC, D], F32)
    TF = {"h": h1, "gcol": E("gcolf", [128, 16], F32), "ident": ident, "wg_b": E("wg_b", [NEX * D, DFX], BF16),
          "wu_b": E("wu_b", [NEX * D, DFX], BF16), "wd_b": E("wd_b", [NEX * DFX, D], BF16), "hout": hout}
    if moe:
        TF.update({"rw": E("rw", [128, 16, NE], F32), "rb": E("rb", [128, NE], F32), "gfin": E("gfin", [128, D], F32),
                   "ident32": E("ident32", [128, 128], F32)})
    build_ffn(TC, moe, kb=kb, T=TF)
    if with_p1:
        T1 = {"h": hout, "w_in_b": E("w_in_b", [D, PTOT], BF16), "gcol": E("gcol_n", [128, 16], F32), "convw": E("convw_n", [128, 8, 3], F32),
              "bf": E("bf_n", [8, 1], F32), "ident": ident,
              "zT": O("zT_n", [DC, TC], BF16), "cu_tail": O("cu_tail_n", [DC, 2], F32), "b_head": O("b_head_n", [DC, 2], F32),
              "qT": O("qT_n", [DA, TC], BF16), "kT": O("kT_n", [DA, TC], BF16), "v": O("v_n", [TC, DA], BF16),
              "lf": O("lf_n", [NH, TC], F32), "gT": O("gT_n", [2 * D, TC], BF16)}
        build_p1(TC, kb=kb, T=T1)
    return kb.finish()


def kernel_unfused6(x, mix_norm, w_in, b_forget, conv_w, w_conv_out, w_attn_out, w_o, ffn_norm,
                    dense_w_gate, dense_w_up, dense_w_down, router_w, router_b,
                    moe_w_gate, moe_w_up, moe_w_down, final_norm):
    f32 = np.float32
    bf = ml_dtypes.bfloat16
    B, S, _ = x.shape
    NTOK = B * S
    TC = NTOK // NCORE
    A = lambda a: np.ascontiguousarray(np.asarray(a, dtype=f32))
    wsrc = {
        "w_in": A(w_in).reshape(-1, PTOT), "wA": A(w_conv_out).reshape(-1, D), "wB": A(w_attn_out).reshape(-1, D),
        "wo": A(w_o).reshape(-1, D), "dg": A(dense_w_gate).reshape(-1, DFF), "du": A(dense_w_up).reshape(-1, DFF),
        "dd": A(dense_w_down).reshape(-1, D), "mg": A(moe_w_gate).reshape(-1, DFE), "mu": A(moe_w_up).reshape(-1, DFE),
        "md": A(moe_w_down).reshape(-1, D),
    }
    specs = [(n, a.shape[0] // NCORE, a.shape[1]) for n, a in wsrc.items()]
    nc0 = _prog("cast", lambda: build_cast(specs))
    r0 = _run(nc0, [{n: a[c * (a.shape[0] // NCORE):(c + 1) * (a.shape[0] // NCORE)] for n, a in wsrc.items()} for c in range(NCORE)])
    wb = {n: np.concatenate([np.asarray(r0[c][n + "_b"]) for c in range(NCORE)], axis=0) for n in wsrc}
    del wsrc, r0
    ident = np.eye(128, dtype=f32).astype(bf)
    consts2 = p2_consts()
    xs = [A(x).reshape(NTOK, D)[c * TC:(c + 1) * TC] for c in range(NCORE)]
    cps = NCORE // B
    gcol = [np.ascontiguousarray(A(mix_norm[l]).reshape(16, 128).T) for l in range(2)]
    gcolf = [np.ascontiguousarray(A(ffn_norm[l]).reshape(16, 128).T) for l in range(2)]
    convw = [_col(np.ascontiguousarray(A(conv_w[l]).T)) for l in range(2)]
    bfg = [A(b_forget[l]).reshape(NH, 1) for l in range(2)]
    nc1 = _prog("p1", lambda: build_p1(TC))
    nc2 = _prog("p2", lambda: build_p2(S, B))
    r1 = _run(nc1, [{"h": xs[c], "w_in_b": wb["w_in"][0:D], "gcol": gcol[0], "convw": convw[0], "bf": bfg[0], "ident": ident} for c in range(NCORE)])
    p1o = [{k: np.asarray(r1[c][k]) for k in ("zT", "cu_tail", "b_head", "qT", "kT", "v", "lf", "gT")} for c in range(NCORE)]
    hprev = xs
    out = None
    for l in range(2):
        in2 = []
        for hd in range(NH):
            sl = slice(hd * HD, (hd + 1) * HD)
            lf_h = np.concatenate([p1o[c]["lf"][hd] for c in range(NCORE)])
            m = {"qT": np.concatenate([p1o[c]["qT"][sl] for c in range(NCORE)], axis=1),
                 "kT": np.concatenate([p1o[c]["kT"][sl] for c in range(NCORE)], axis=1),
                 "v": np.ascontiguousarray(np.concatenate([p1o[c]["v"][:, sl] for c in range(NCORE)], axis=0)),
                 "lfcol": np.ascontiguousarray(lf_h.reshape(NTOK // 128, 128).T)}
            m.update(consts2)
            in2.append(m)
        r2 = _run(nc2, in2)
        del in2
        moe = (l % 2 == 1)
        with_p1 = (l == 0)
        ncm = _prog(("mid", moe, with_p1), lambda: build_mid(TC, moe, with_p1))
        inm = []
        for c in range(NCORE):
            oT_c = np.concatenate([np.asarray(r2[hd]["oT"])[:, c * TC:(c + 1) * TC] for hd in range(NH)], axis=0)
            cu_prev = np.zeros((DC, 2), f32) if c % cps == 0 else p1o[c - 1]["cu_tail"]
            m = {"zT": p1o[c]["zT"], "oT": np.ascontiguousarray(oT_c), "gT": p1o[c]["gT"], "cu_prev": _col(cu_prev), "b_head": _col(p1o[c]["b_head"]),
                 "convw": convw[l], "wA_b": wb["wA"][l * DC:(l + 1) * DC], "wB_b": wb["wB"][l * DA:(l + 1) * DA], "wo_b": wb["wo"][l * D:(l + 1) * D],
                 "h": hprev[c], "ident": ident, "gcolf": gcolf[l]}
            if not moe:
                m.update({"wg_b": wb["dg"], "wu_b": wb["du"], "wd_b": wb["dd"]})
            else:
                m.update({"wg_b": wb["mg"], "wu_b": wb["mu"], "wd_b": wb["md"],
                          "rw": np.ascontiguousarray(A(router_w[0]).reshape(16, 128, NE).transpose(1, 0, 2)),
                          "rb": np.ascontiguousarray(np.broadcast_to(A(router_b[0]), (128, NE))),
                          "gfin": np.ascontiguousarray(np.broadcast_to(A(final_norm), (128, D))), "ident32": np.eye(128, dtype=f32)})
            if with_p1:
                m.update({"w_in_b": wb["w_in"][(l + 1) * D:(l + 2) * D], "gcol_n": gcol[l + 1], "convw_n": convw[l + 1], "bf_n": bfg[l + 1]})
            inm.append(m)
        rm = _run(ncm, inm)
        del inm, r2
        hprev = [np.asarray(rm[c]["hout"]) for c in range(NCORE)]
        if with_p1:
            p1o = [{k: np.asarray(rm[c][k + "_n"]) for k in ("zT", "cu_tail", "b_head", "qT", "kT", "v", "lf", "gT")} for c in range(NCORE)]
    return np.concatenate(hprev, axis=0).reshape(B, S, D).astype(f32)


kernel = kernel_unfused6
```

```python
import numpy as np
import ml_dtypes
from contextlib import ExitStack
import concourse.bass as bass
import concourse.mybir as mybir
from concourse.bass_utils import run_bass_kernel_spmd

F32 = mybir.dt.float32
BF16 = mybir.dt.bfloat16
AF = mybir.ActivationFunctionType
ALU = mybir.AluOpType

D = 2048
NCORE = 8
DC = 1024
DA = 1024
NH = 8
HD = 128
PTOT = 3 * DC + 3 * DA + NH + 2 * D
DFF = 5632
NE = 8
DFE = 7168
EPS = 1e-6
TT = 512


class Sem:
    def __init__(self, kb, name):
        self.h = kb.es.enter_context(kb.nc.semaphore(name))
        self.v = 0


class Tile:
    def __init__(self, kb, ap, name):
        self.kb = kb
        self.ap = ap
        self.name = name
        self.w = []
        self.r = []
        self.pr = []
        self.dsem = None

    def dma_sem(self):
        if self.dsem is None:
            self.dsem = self.kb.new_dma_sem(self.name)
        return self.dsem

    def __getitem__(self, idx):
        return self.ap[idx]


class KB:
    def __init__(self):
        self.nc = bass.Bass("TRN2", target_bir_lowering=False)
        self.es = ExitStack()
        self.waited = {}
        self.esem = {}
        self.dma_sems = []
        self.free_dma_sems = []
        self.n_ins = 0
        nc = self.nc
        self.engs = {"pe": nc.tensor, "act": nc.scalar, "dve": nc.vector, "pool": nc.gpsimd, "sp": nc.sync}
        for k in self.engs:
            self.esem[k] = Sem(self, "e_" + k)
        self.pending = {k: False for k in self.engs}
        self.es_phase = None
        self.sem_pool = []
        self.phase_sems = []
        self.nuid = 0

    def phase_begin(self):
        self.es_phase = ExitStack()
        self.phase_sems = []

    def phase_end(self, extra_sems=()):
        self.barrier(extra_sems)
        self.es_phase.close()
        self.es_phase = None
        self.sem_pool.extend(self.phase_sems)
        self.phase_sems = []

    def new_dma_sem(self, name):
        if self.sem_pool:
            s = self.sem_pool.pop()
        else:
            self.nuid += 1
            s = Sem(self, "d%d_" % self.nuid + name)
            self.dma_sems.append(s)
        self.phase_sems.append(s)
        return s

    def dram(self, name, shape, dt, kind="Internal"):
        return self.nc.dram_tensor(name, list(shape), dt, kind=kind).ap()

    def sbuf(self, name, shape, dt):
        self.nuid += 1
        es = self.es_phase if self.es_phase is not None else self.es
        t = es.enter_context(self.nc.sbuf_tensor("%s_%d" % (name, self.nuid), list(shape), dt))
        return Tile(self, t, name)

    def psum(self, name, shape, dt=F32):
        self.nuid += 1
        es = self.es_phase if self.es_phase is not None else self.es
        t = es.enter_context(self.nc.psum_tensor("%s_%d" % (name, self.nuid), list(shape), dt))
        return Tile(self, t, name)

    def dr(self, T, name, shape, dt, kind):
        if T is not None:
            return T.get(name)
        return self.dram(name, shape, dt, kind=kind)

    def _wait(self, ek, sem, val):
        key = (ek, id(sem))
        if self.waited.get(key, 0) >= val:
            return
        self.engs[ek].wait_ge(sem.h, val)
        self.waited[key] = val

    def _deps(self, ek, reads, writes):
        need = {}

        def add(s, v):
            k = id(s)
            if k not in need or need[k][1] < v:
                need[k] = (s, v)
        for t in reads:
            for (s, v, e) in t.w:
                add(s, v)
        for t in writes:
            if t.r:
                t.pr = t.r
                t.r = []
                t.w = []
            for (s, v, e) in t.pr:
                if e != ek:
                    add(s, v)
        for (s, v) in need.values():
            self._wait(ek, s, v)

    def op(self, ek, fn, reads=(), writes=(), signal=True):
        self._deps(ek, reads, writes)
        ins = fn()
        self.n_ins += 1
        es = self.esem[ek]
        if signal:
            ins.then_inc(es.h, 1)
            es.v += 1
            val = es.v
            self.pending[ek] = False
        else:
            val = es.v + 1
            self.pending[ek] = True
        for t in reads:
            t.r.append((es, val, ek))
        for t in writes:
            t.w.append((es, val, ek))
        return ins

    def dma(self, qk, out_ap, in_ap, sb_tile, load, extra_reads=(), **kw):
        if load:
            self._deps(qk, list(extra_reads), [sb_tile])
        else:
            self._deps(qk, [sb_tile] + list(extra_reads), [])
        s = sb_tile.dma_sem()
        ins = self.engs[qk].dma_start(out=out_ap, in_=in_ap, **kw)
        ins.then_inc(s.h, 16)
        s.v += 16
        self.n_ins += 1
        if load:
            sb_tile.w.append((s, s.v, "dma"))
        else:
            sb_tile.r.append((s, s.v, "dma"))
        return ins

    def dram_dma(self, qk, out_ap, in_ap, sem, **kw):
        ins = self.engs[qk].dma_start(out=out_ap, in_=in_ap, **kw)
        ins.then_inc(sem.h, 16)
        sem.v += 16
        self.n_ins += 1
        return ins

    def barrier(self, extra_sems=()):
        for ek in self.engs:
            assert not self.pending[ek], ek
        allsems = list(self.esem.values()) + self.dma_sems + list(extra_sems)
        for ek in self.engs:
            for s in allsems:
                if s.v > 0:
                    self._wait(ek, s, s.v)

    def finish(self, extra_sems=()):
        self.barrier(extra_sems)
        self.es.close()
        return self.nc


def mm_group(kb, out_tile, out_ap, pairs, reads):
    n = len(pairs)
    for i, (l, r) in enumerate(pairs):
        kb.op("pe", lambda l=l, r=r, i=i: kb.nc.tensor.matmul(out_ap, l, r, start=(i == 0), stop=(i == n - 1)),
              reads=reads if i == 0 else (), writes=[out_tile] if i == 0 else (), signal=(i == n - 1))
    es = kb.esem["pe"]
    out_tile.w = [(es, es.v, "pe")]
    for t in reads:
        t.r.append((es, es.v, "pe"))


def cast_rows(kb, sem, src, dst, rows, cols, row_step=128):
    for r0 in range(0, rows, row_step):
        r1 = min(rows, r0 + row_step)
        kb.dram_dma("pool", dst[r0:r1, :], src[r0:r1, :], sem, max_dma_last_dim=4096)


def build_cast(specs):
    kb = KB()
    sem = Sem(kb, "cast")
    for (name, rows, cols) in specs:
        src = kb.dram(name, [rows, cols], F32, kind="ExternalInput")
        dst = kb.dram(name + "_b", [rows, cols], BF16, kind="ExternalOutput")
        cast_rows(kb, sem, src, dst, rows, cols)
    return kb.finish([sem])


class Consts:
    pass


def norm_transpose(kb, cs, h_dram, tok0, aT, hin_ring, ablk_ring, tp_ring, junk, ss_ring, gcol, ident, ctr, after_block=None):
    nc = kb.nc
    for blk in range(TT // 128):
        i = ctr["nb"]
        ctr["nb"] += 1
        hin = hin_ring[i % len(hin_ring)]
        ab = ablk_ring[i % len(ablk_ring)]
        ss = ss_ring[i % len(ss_ring)]
        r0 = tok0 + blk * 128
        kb.dma("sp", hin.ap[:, :], h_dram[r0:r0 + 128, :], hin, load=True)
        kb.op("act", lambda: nc.scalar.activation(out=ab.ap[:, :], in_=hin.ap[:, :], func=AF.Square,
                                                  accum_out=ss.ap[:, 0:1]), reads=[hin], writes=[ab, ss])
        kb.op("act", lambda: nc.scalar.activation(out=ss.ap[:, 1:2], in_=ss.ap[:, 0:1], func=AF.Sqrt,
                                                  bias=cs.eps.ap[:, 0:1], scale=1.0 / D), reads=[ss, cs.eps], writes=[ss])
        kb.op("dve", lambda: nc.vector.reciprocal(out=ss.ap[:, 2:3], in_=ss.ap[:, 1:2]), reads=[ss], writes=[ss])
        kb.op("dve", lambda: nc.vector.tensor_scalar(out=ab.ap[:, :], in0=hin.ap[:, :], scalar1=ss.ap[:, 2:3],
                                                     scalar2=None, op0=ALU.mult), reads=[hin, ss], writes=[ab])
        for half in range(2):
            tp = tp_ring[ctr["tp"] % len(tp_ring)]
            ctr["tp"] += 1
            for j in range(8):
                c = half * 8 + j
                kb.op("pe", lambda c=c, j=j: nc.tensor.transpose(tp.ap[:, j, :], ab.ap[:, c * 128:(c + 1) * 128], ident.ap[:, :]),
                      reads=[ab, ident] if j == 0 else (), writes=[tp] if j == 0 else (), signal=(j == 7))
            es = kb.esem["pe"]
            tp.w = [(es, es.v, "pe")]
            ab.r.append((es, es.v, "pe"))
            for j in range(8):
                c = half * 8 + j
                if True:
                    kb.op("dve", lambda c=c, j=j: nc.vector.tensor_scalar(out=aT.ap[:, c, blk * 128:(blk + 1) * 128], in0=tp.ap[:, j, :],
                                                                          scalar1=gcol.ap[:, c:c + 1], scalar2=None, op0=ALU.mult),
                          reads=[tp, gcol], writes=[aT])
                else:
                    kb.op("act", lambda c=c, j=j: nc.scalar.activation(out=aT.ap[:, c, blk * 128:(blk + 1) * 128], in_=tp.ap[:, j, :],
                                                                       func=AF.Copy, scale=gcol.ap[:, c:c + 1]),
                          reads=[tp, gcol], writes=[aT])
        if after_block is not None:
            after_block(blk)


def load_w(kb, wring, ctr, w_dram, k0, kc, c0, ncols):
    wt = wring[ctr["w"] % len(wring)]
    ctr["w"] += 1
    src = w_dram[k0 * 128:(k0 + kc) * 128, c0:c0 + ncols].rearrange("(c p) n -> p c n", p=128)
    kb.dma("sp", wt.ap[:, 0:kc, 0:ncols], src, wt, load=True)
    return wt


def build_p1(TC, kb=None, T=None):
    own = kb is None
    kb = kb or KB()
    if not own:
        kb.phase_begin()
    nc = kb.nc
    NT = TC // TT
    h = kb.dr(T, "h", [TC, D], F32, kind="ExternalInput")
    w_in = kb.dr(T, "w_in_b", [D, PTOT], BF16, kind="ExternalInput")
    gcol_d = kb.dr(T, "gcol", [128, 16], F32, kind="ExternalInput")
    convw_d = kb.dr(T, "convw", [128, 8, 3], F32, kind="ExternalInput")
    negb_d = kb.dr(T, "bf", [8, 1], F32, kind="ExternalInput")
    ident_d = kb.dr(T, "ident", [128, 128], BF16, kind="ExternalInput")
    zT = kb.dr(T, "zT", [DC, TC], BF16, kind="ExternalOutput")
    cu_tail = kb.dr(T, "cu_tail", [DC, 2], F32, kind="ExternalOutput")
    b_head = kb.dr(T, "b_head", [DC, 2], F32, kind="ExternalOutput")
    qT = kb.dr(T, "qT", [DA, TC], BF16, kind="ExternalOutput")
    kT = kb.dr(T, "kT", [DA, TC], BF16, kind="ExternalOutput")
    vv = kb.dr(T, "v", [TC, DA], BF16, kind="ExternalOutput")
    lf = kb.dr(T, "lf", [NH, TC], F32, kind="ExternalOutput")
    gT = kb.dr(T, "gT", [2 * D, TC], BF16, kind="ExternalOutput")

    cs = Consts()
    cs.eps = kb.sbuf("eps", [128, 1], F32)
    gcol = kb.sbuf("gcol_s", [128, 16], F32)
    convw = kb.sbuf("convw_s", [128, 8, 3], F32)
    bfs = kb.sbuf("bf_s", [8, 2], F32)
    ident = kb.sbuf("ident_s", [128, 128], BF16)
    kb.op("dve", lambda: nc.vector.memset(cs.eps.ap[:, :], EPS), writes=[cs.eps])
    kb.dma("sp", gcol.ap[:, :], gcol_d[:, :], gcol, load=True)
    kb.dma("sp", convw.ap[:, :, :], convw_d[:, :, :], convw, load=True)
    kb.dma("sp", bfs.ap[:, 0:1], negb_d[:, :], bfs, load=True)
    kb.dma("sp", ident.ap[:, :], ident_d[:, :], ident, load=True)
    kb.op("dve", lambda: nc.vector.tensor_scalar(out=bfs.ap[:, 1:2], in0=bfs.ap[:, 0:1], scalar1=-1.0, scalar2=None, op0=ALU.mult),
          reads=[bfs], writes=[bfs])

    hin_ring = [kb.sbuf(f"hin{i}", [128, D], F32) for i in range(2)]
    ablk_ring = [kb.sbuf(f"ablk{i}", [128, D], BF16) for i in range(2)]
    ss_ring = [kb.sbuf(f"ss{i}", [128, 4], F32) for i in range(4)]
    junk = None
    aT_ring = [kb.sbuf(f"aT{i}", [128, 16, TT], BF16) for i in range(2)]
    wring = [kb.sbuf(f"w{i}", [128, 16, 512], BF16) for i in range(3)]
    tp_ring = [kb.psum(f"tp{i}", [128, 8, 128], BF16) for i in range(2)]
    ps_ring = [kb.psum(f"ps{i}", [128, 512], F32) for i in range(6)]
    bcu = [kb.sbuf(f"bcu{i}", [128, 8, TT], BF16) for i in range(3)]
    cuext = [kb.sbuf(f"cuext{i}", [128, TT + 2], F32) for i in range(2)]
    carry = kb.sbuf("carry", [128, 8, 2], F32)
    ycv = [kb.sbuf(f"ycv{i}", [128, TT], F32) for i in range(2)]
    zst = [kb.sbuf(f"zst{i}", [128, 8, TT], BF16) for i in range(1)]
    qst = [kb.sbuf(f"qst{i}", [128, 4, TT], BF16) for i in range(3)]
    vst = [kb.sbuf(f"vst{i}", [128, 512], BF16) for i in range(2)]
    lfst = [kb.sbuf(f"lfst{i}", [8, 3, TT], F32) for i in range(2)]
    bh = kb.sbuf("bh", [128, 8, 2], F32)
    kb.op("pool", lambda: nc.gpsimd.memset(carry.ap[:, :, :], 0.0), writes=[carry])

    ctr = {"nb": 0, "tp": 0, "w": 0, "ps": 0, "q": 0, "v": 0, "ev": 0}

    def next_ps():
        p = ps_ring[ctr["ps"] % len(ps_ring)]
        ctr["ps"] += 1
        return p

    def evac_copy(out_tile, out_ap, ps):
        kb.op("dve", lambda: nc.vector.tensor_copy(out=out_ap, in_=ps.ap[:, :]), reads=[ps], writes=[out_tile])

    blocks = []
    for s in range(3):
        for cb in range(2):
            blocks.append(("bcu", s, cb, s * DC + cb * 512))
    for s in range(2):
        for cb in range(2):
            blocks.append(("qk", s, cb, 3 * DC + s * DA + cb * 512))
    for cb in range(2):
        blocks.append(("v", 0, cb, 3 * DC + 2 * DA + cb * 512))
    blocks.append(("f", 0, 0, 3 * DC + 3 * DA))
    for cb in range(8):
        blocks.append(("g", 0, cb, 3 * DC + 3 * DA + NH + cb * 512))

    import os
    SKIP = os.environ.get('P1_SKIP', '').split(',')
    blocks = [b for b in blocks if b[0] not in SKIP]
    norm_transpose(kb, cs, h, 0, aT_ring[0], hin_ring, ablk_ring, tp_ring, junk, ss_ring, gcol, ident, ctr)
    for t in range(NT):
        aT = aT_ring[t % 2]
        tok0 = t * TT
        if not blocks:
            break
        wt_next = load_w(kb, wring, ctr, w_in, 0, 16, blocks[0][3], 512)
        for bi, (kind, s, cb, c0) in enumerate(blocks):
            wt = wt_next
            if bi + 1 < len(blocks):
                nk = blocks[bi + 1]
                wt_next = load_w(kb, wring, ctr, w_in, 0, 16, nk[3], 8 if nk[0] == "f" else 512)
            if bi == min(6, len(blocks) - 1) and t + 1 < NT:
                norm_transpose(kb, cs, h, (t + 1) * TT, aT_ring[(t + 1) % 2], hin_ring, ablk_ring, tp_ring, junk, ss_ring, gcol, ident, ctr)
            if kind in ("bcu", "qk", "g"):
                if kind == "qk" or kind == "g":
                    st = qst[ctr["q"] % len(qst)]
                    ctr["q"] += 1
                for c in range(4):
                    ps = next_ps()
                    mm_group(kb, ps, ps.ap[:, :], [(wt.ap[:, k, c * 128:(c + 1) * 128], aT.ap[:, k, :]) for k in range(16)], [wt, aT])
                    if kind == "bcu":
                        evac_copy(bcu[s], bcu[s].ap[:, cb * 4 + c, :], ps)
                    elif kind == "qk":
                        evac_copy(st, st.ap[:, c, :], ps)
                    else:
                        kb.op("act", lambda c=c, ps=ps, st=st: nc.scalar.activation(out=st.ap[:, c, :], in_=ps.ap[:, :], func=AF.Sigmoid),
                              reads=[ps], writes=[st])
                if kind == "qk":
                    dst = (qT if s == 0 else kT)[cb * 512:(cb + 1) * 512, tok0:tok0 + TT].rearrange("(c p) t -> p c t", p=128)
                    kb.dma("sp", dst, st.ap[:, :, :], st, load=False)
                elif kind == "g":
                    dst = gT[cb * 512:(cb + 1) * 512, tok0:tok0 + TT].rearrange("(c p) t -> p c t", p=128)
                    kb.dma("sp", dst, st.ap[:, :, :], st, load=False)
                elif s == 2 and cb == 1 and 'conv' not in SKIP:
                    zs = zst[0]
                    for c in range(8):
                        ce = cuext[c % 2]
                        yc = ycv[c % 2]
                        kb.op("pool", lambda c=c, ce=ce: nc.gpsimd.tensor_copy(out=ce.ap[:, 0:2], in_=carry.ap[:, c, :]), reads=[carry], writes=[ce])
                        kb.op("pool", lambda c=c, ce=ce: nc.gpsimd.tensor_tensor(out=ce.ap[:, 2:TT + 2], in0=bcu[1].ap[:, c, :], in1=bcu[2].ap[:, c, :], op=ALU.mult),
                              reads=[bcu[1], bcu[2]], writes=[ce])
                        kb.op("pool", lambda c=c, ce=ce: nc.gpsimd.tensor_copy(out=carry.ap[:, c, :], in_=ce.ap[:, TT:TT + 2]), reads=[ce], writes=[carry])
                        kb.op("pool", lambda c=c, ce=ce, yc=yc: nc.gpsimd.tensor_scalar(out=yc.ap[:, :], in0=ce.ap[:, 0:TT], scalar1=convw.ap[:, c, 0:1], scalar2=None, op0=ALU.mult),
                              reads=[ce, convw], writes=[yc])
                        kb.op("dve", lambda c=c, ce=ce, yc=yc: nc.vector.scalar_tensor_tensor(out=yc.ap[:, :], in0=ce.ap[:, 1:TT + 1], scalar=convw.ap[:, c, 1:2], in1=yc.ap[:, :], op0=ALU.mult, op1=ALU.add),
                              reads=[ce, convw, yc], writes=[yc])
                        kb.op("dve", lambda c=c, ce=ce, yc=yc: nc.vector.scalar_tensor_tensor(out=yc.ap[:, :], in0=ce.ap[:, 2:TT + 2], scalar=convw.ap[:, c, 2:3], in1=yc.ap[:, :], op0=ALU.mult, op1=ALU.add),
                              reads=[ce, convw, yc], writes=[yc])
                        kb.op("pool", lambda c=c, yc=yc, zs=zs: nc.gpsimd.tensor_tensor(out=zs.ap[:, c, :], in0=yc.ap[:, :], in1=bcu[0].ap[:, c, :], op=ALU.mult),
                              reads=[yc, bcu[0]], writes=[zs])
                    dst = zT[:, tok0:tok0 + TT].rearrange("(c p) t -> p c t", p=128)
                    kb.dma("sp", dst, zs.ap[:, :, :], zs, load=False)
                    if t == 0:
                        kb.op("pool", lambda: nc.gpsimd.tensor_copy(out=bh.ap[:, :, :], in_=bcu[0].ap[:, :, 0:2]), reads=[bcu[0]], writes=[bh])
                        kb.dma("sp", b_head.rearrange("(c p) t -> p c t", p=128), bh.ap[:, :, :], bh, load=False)
                    if t == NT - 1:
                        kb.dma("sp", cu_tail.rearrange("(c p) t -> p c t", p=128), carry.ap[:, :, :], carry, load=False)
            elif kind == "v":
                for tb in range(TT // 128):
                    ps = next_ps()
                    mm_group(kb, ps, ps.ap[:, :], [(aT.ap[:, k, tb * 128:(tb + 1) * 128], wt.ap[:, k, :]) for k in range(16)], [wt, aT])
                    st = vst[ctr["v"] % 2]
                    ctr["v"] += 1
                    evac_copy(st, st.ap[:, :], ps)
                    kb.dma("sp", vv[tok0 + tb * 128: tok0 + (tb + 1) * 128, cb * 512:(cb + 1) * 512], st.ap[:, :], st, load=False)
            elif kind == "f":
                ps = next_ps()
                mm_group(kb, ps, ps.ap[0:8, :], [(wt.ap[:, k, 0:8], aT.ap[:, k, :]) for k in range(16)], [wt, aT])
                st = lfst[t % 2]
                kb.op("act", lambda ps=ps, st=st: nc.scalar.activation(out=st.ap[:, 0, :], in_=ps.ap[0:8, :], func=AF.Exp, bias=bfs.ap[:, 1:2], scale=-1.0),
                      reads=[ps, bfs], writes=[st])
                kb.op("act", lambda st=st: nc.scalar.activation(out=st.ap[:, 1, :], in_=st.ap[:, 0, :], func=AF.Ln, bias=1.0, scale=1.0),
                      reads=[st], writes=[st])
                kb.op("dve", lambda st=st: nc.vector.tensor_scalar(out=st.ap[:, 2, :], in0=st.ap[:, 1, :], scalar1=-1.0, scalar2=None, op0=ALU.mult),
                      reads=[st], writes=[st])
                kb.dma("sp", lf[:, tok0:tok0 + TT], st.ap[:, 2, :], st, load=False)
    if own:
        return kb.finish()
    kb.phase_end()


def build_p2(S, NSEQ, kb=None, T=None):
    own = kb is None
    kb = kb or KB()
    if not own:
        kb.phase_begin()
    nc = kb.nc
    NTOK = S * NSEQ
    NBS = S // 128
    NQ = S // TT
    qT = kb.dr(T, "qT", [128, NTOK], BF16, kind="ExternalInput")
    kT = kb.dr(T, "kT", [128, NTOK], BF16, kind="ExternalInput")
    vv = kb.dr(T, "v", [NTOK, 128], BF16, kind="ExternalInput")
    lfc_d = kb.dr(T, "lfcol", [128, NTOK // 128], F32, kind="ExternalInput")
    tri_d = kb.dr(T, "tri", [128, 128], F32, kind="ExternalInput")
    sel_d = kb.dr(T, "sel", [128, 128], F32, kind="ExternalInput")
    ones_d = kb.dr(T, "onesb", [128, 128], BF16, kind="ExternalInput")
    mask_d = kb.dr(T, "masks", [128, 4, TT], BF16, kind="ExternalInput")
    oT = kb.dr(T, "oT", [128, NTOK], BF16, kind="ExternalOutput")

    tri = kb.sbuf("tri_s", [128, 128], F32)
    sel = kb.sbuf("sel_s", [128, 128], F32)
    onesb = kb.sbuf("ones_s", [128, 128], BF16)
    masks = kb.sbuf("mask_s", [128, 4, TT], BF16)
    onesf = kb.sbuf("onesf", [128, NBS], F32)
    lfc = kb.sbuf("lfc", [128, NTOK // 128], F32)
    fused = T is not None and "qT_all" in T
    for (t, d) in ((tri, tri_d), (sel, sel_d), (onesb, ones_d)) + (() if fused else ((lfc, lfc_d),)):
        kb.dma("sp", t.ap[:, :], d[:, :], t, load=True)
    if fused:
        TCc = NTOK // NCORE
        CH = min(2048, TCc)
        VG = 8 * CH // 1024
        BPR = TCc // 128
        hmask = kb.sbuf("hmask", [128, 8], F32)
        id32 = kb.sbuf("id32", [128, 128], F32)
        X = kb.sbuf("X", [128, 8, CH], BF16)
        Xv = X.ap[:, :, :].rearrange("p a b -> p (a b)").rearrange("p (n f) -> p n f", f=1024)
        Xlf = kb.sbuf("Xlf", [128, 8, 128], F32)
        lfrow = kb.sbuf("lfrow", [128, 128], F32)
        kb.dma("sp", hmask.ap[:, :], T["hmask"][:, :], hmask, load=True)
        kb.dma("sp", id32.ap[:, :], T["ident32"][:, :], id32, load=True)
        kb.op("dve", lambda: nc.vector.memset(Xlf.ap[:, :, :], 0.0), writes=[Xlf])

        def select(out_tile, out_ap, src_tile, in_fn, mask=None):
            mk = mask or hmask
            kb.op("dve", lambda: nc.vector.tensor_scalar(out=out_ap, in0=in_fn(0), scalar1=mk.ap[:, 0:1], scalar2=None, op0=ALU.mult),
                  reads=[src_tile, mk], writes=[out_tile])
            for hh in range(1, 8):
                kb.op("dve", lambda hh=hh: nc.vector.scalar_tensor_tensor(out=out_ap, in0=in_fn(hh), scalar=mk.ap[:, hh:hh + 1], in1=out_ap, op0=ALU.mult, op1=ALU.add),
                      reads=[src_tile, mk, out_tile], writes=[out_tile])
    kb.dma("sp", masks.ap[:, :, :], mask_d[:, :, :], masks, load=True)
    kb.op("dve", lambda: nc.vector.memset(onesf.ap[:, :], 1.0), writes=[onesf])

    kTs = kb.sbuf("kTs", [128, S], BF16)
    qTs = kb.sbuf("qTs", [128, S], BF16)
    vs = kb.sbuf("vs", [128, NBS, 128], BF16)
    within = kb.sbuf("within", [128, NBS], F32)
    totb = kb.sbuf("totb", [128, NBS], F32)
    incl = kb.sbuf("incl", [128, NBS], F32)
    ccol = kb.sbuf("ccol", [128, NBS], F32)
    crefb = kb.sbuf("crefb", [128, NBS], F32)
    biasq = [kb.sbuf(f"biasq{i}", [128, NBS], F32) for i in range(2)]
    pt = [kb.sbuf(f"pt{i}", [128, TT], BF16) for i in range(3)]
    rs = [kb.sbuf(f"rs{i}", [128, TT], F32) for i in range(2)]
    ost = [kb.sbuf(f"ost{i}", [128, TT], BF16) for i in range(2)]
    ps_s = [kb.psum(f"pss{i}", [128, TT], F32) for i in range(3)]
    acc_o = [kb.psum(f"acco{i}", [128, TT], F32) for i in range(2)]
    acc_s = [kb.psum(f"accs{i}", [128, TT], F32) for i in range(2)]
    psc = kb.psum("psc", [128, 512], F32)
    scale = 1.0 / float(np.sqrt(HD))

    for s in range(NSEQ):
        t0 = s * S
        nch = 4
        if not fused:
            for i in range(nch):
                a, b = i * S // nch, (i + 1) * S // nch
                kb.dma("sp", kTs.ap[:, a:b], kT[:, t0 + a:t0 + b], kTs, load=True)
                kb.dma("sp", qTs.ap[:, a:b], qT[:, t0 + a:t0 + b], qTs, load=True)
                kb.dma("sp", vs.ap[:, a // 128:b // 128, :], vv[t0 + a:t0 + b, :].rearrange("(n p) d -> p n d", p=128), vs, load=True)
        else:
            rps = NCORE // NSEQ
            for rl in range(rps):
                r = s * rps + rl
                for (src_all, dst) in ((T["kT_all"], kTs), (T["qT_all"], qTs)):
                    for hf in range(TCc // CH):
                        kb.dma("sp", X.ap[:, :, :], src_all[r * DA:(r + 1) * DA, hf * CH:(hf + 1) * CH].rearrange("(h d) t -> d h t", d=128), X, load=True)
                        c0 = rl * TCc + hf * CH
                        select(dst, dst.ap[:, c0:c0 + CH], X, lambda hh: X.ap[:, hh, :])
                kb.dma("sp", Xlf.ap[rl * BPR:(rl + 1) * BPR, :, :], T["lf_all"][r * NH:(r + 1) * NH, :].rearrange("h (b p) -> b h p", p=128), Xlf, load=True)
            for g in range(NBS // VG):
                kb.dma("sp", Xv[:, 0:VG, :], T["v_all"][t0 + g * VG * 128:t0 + (g + 1) * VG * 128, :].rearrange("(n p) f -> p n f", p=128), X, load=True)
                select(vs, vs.ap[:, g * VG:(g + 1) * VG, :], X, lambda hh: Xv[:, 0:VG, hh * 128:(hh + 1) * 128])
            select(lfrow, lfrow.ap[:, :], Xlf, lambda hh: Xlf.ap[:, hh, :])
            kb.op("pe", lambda: nc.tensor.transpose(psc.ap[:, 0:128], lfrow.ap[:, :], id32.ap[:, :]), reads=[lfrow, id32], writes=[psc])
            kb.op("dve", lambda: nc.vector.tensor_copy(out=lfc.ap[:, s * NBS:(s + 1) * NBS], in_=psc.ap[:, 0:NBS]), reads=[psc], writes=[lfc])
        kb.op("pe", lambda: nc.tensor.matmul(psc.ap[:, 0:NBS], tri.ap[:, :], lfc.ap[:, s * NBS:(s + 1) * NBS], start=True, stop=True),
              reads=[tri, lfc], writes=[psc])
        kb.op("dve", lambda: nc.vector.tensor_copy(out=within.ap[:, :], in_=psc.ap[:, 0:NBS]), reads=[psc], writes=[within])
        kb.op("pe", lambda: nc.tensor.matmul(psc.ap[:, 0:NBS], sel.ap[:, :], within.ap[:, :], start=True, stop=True),
              reads=[sel, within], writes=[psc])
        kb.op("dve", lambda: nc.vector.tensor_copy(out=totb.ap[:, :], in_=psc.ap[:, 0:NBS]), reads=[psc], writes=[totb])
        kb.op("dve", lambda: nc.vector.tensor_tensor_scan(out=incl.ap[:, :], data0=onesf.ap[:, :], data1=totb.ap[:, :], initial=0.0,
                                                          op0=ALU.mult, op1=ALU.add), reads=[onesf, totb], writes=[incl])
        kb.op("dve", lambda: nc.vector.tensor_tensor(out=ccol.ap[:, :], in0=within.ap[:, :], in1=incl.ap[:, :], op=ALU.add),
              reads=[within, incl], writes=[ccol])
        kb.op("dve", lambda: nc.vector.tensor_tensor(out=ccol.ap[:, :], in0=ccol.ap[:, :], in1=totb.ap[:, :], op=ALU.subtract),
              reads=[ccol, totb], writes=[ccol])
        kb.op("pe", lambda: nc.tensor.matmul(psc.ap[:, 0:NBS], sel.ap[:, :], ccol.ap[:, :], start=True, stop=True),
              reads=[sel, ccol], writes=[psc])
        kb.op("dve", lambda: nc.vector.tensor_copy(out=crefb.ap[:, :], in_=psc.ap[:, 0:NBS]), reads=[psc], writes=[crefb])

        steps = [(qt, kbk) for qt in range(NQ) for kbk in range(4 * qt + 4)]

        def emit_s(i):
            qt, kbk = steps[i]
            p = ps_s[i % 3]
            kb.op("pe", lambda: nc.tensor.matmul(p.ap[:, :], kTs.ap[:, kbk * 128:(kbk + 1) * 128], qTs.ap[:, qt * TT:(qt + 1) * TT], start=True, stop=True),
                  reads=[kTs, qTs], writes=[p])

        emit_s(0)
        if len(steps) > 1:
            emit_s(1)
        for i, (qt, kbk) in enumerate(steps):
            nk = 4 * qt + 4
            bq = biasq[qt % 2]
            if kbk == 0:
                kb.op("dve", lambda: nc.vector.tensor_scalar(out=bq.ap[:, 0:nk], in0=ccol.ap[:, 0:nk], scalar1=-1.0,
                                                             scalar2=crefb.ap[:, 4 * qt + 1:4 * qt + 2], op0=ALU.mult, op1=ALU.add),
                      reads=[ccol, crefb], writes=[bq])
            if i + 2 < len(steps):
                emit_s(i + 2)
            p = ps_s[i % 3]
            ptile = pt[i % 3]
            kb.op("act", lambda: nc.scalar.activation(out=ptile.ap[:, :], in_=p.ap[:, :], func=AF.Exp, bias=bq.ap[:, kbk:kbk + 1], scale=scale),
                  reads=[p, bq], writes=[ptile])
            if kbk >= 4 * qt:
                j = kbk - 4 * qt
                kb.op("pool", lambda: nc.gpsimd.tensor_tensor(out=ptile.ap[:, :], in0=ptile.ap[:, :], in1=masks.ap[:, j, :], op=ALU.mult),
                      reads=[ptile, masks], writes=[ptile])
            ao = acc_o[qt % 2]
            asum = acc_s[qt % 2]
            first, last = (kbk == 0), (kbk == nk - 1)
            kb.op("pe", lambda: nc.tensor.matmul(ao.ap[:, :], vs.ap[:, kbk, :], ptile.ap[:, :], start=first, stop=last),
                  reads=[vs, ptile], writes=[ao] if first else (), signal=False)
            kb.op("pe", lambda: nc.tensor.matmul(asum.ap[:, :], onesb.ap[:, :], ptile.ap[:, :], start=first, stop=last),
                  reads=[onesb, ptile], writes=[asum] if first else (), signal=True)
            if last:
                es = kb.esem["pe"]
                ao.w = [(es, es.v, "pe")]
                asum.w = [(es, es.v, "pe")]
                r = rs[qt % 2]
                o = ost[qt % 2]
                kb.op("dve", lambda: nc.vector.reciprocal(out=r.ap[:, :], in_=asum.ap[:, :]), reads=[asum], writes=[r])
                kb.op("dve", lambda: nc.vector.tensor_tensor(out=o.ap[:, :], in0=ao.ap[:, :], in1=r.ap[:, :], op=ALU.mult), reads=[ao, r], writes=[o])
                kb.dma("sp", oT[:, t0 + qt * TT:t0 + (qt + 1) * TT], o.ap[:, :], o, load=False)
    if own:
        return kb.finish()
    kb.phase_end()


def p2_consts():
    bf = ml_dtypes.bfloat16
    kk = np.arange(128)
    tri = (kk[:, None] <= kk[None, :]).astype(np.float32)
    sel = np.zeros((128, 128), np.float32)
    sel[127, :] = 1.0
    masks = np.zeros((128, 4, TT), np.float32)
    qq = np.arange(TT)
    for j in range(4):
        masks[:, j, :] = ((j * 128 + kk)[:, None] <= qq[None, :])
    return {"tri": tri, "sel": sel, "onesb": np.ones((128, 128), np.float32).astype(bf), "masks": masks.astype(bf)}


def build_p3(TC, kb=None, T=None):
    own = kb is None
    kb = kb or KB()
    if not own:
        kb.phase_begin()
    nc = kb.nc
    NT = TC // TT
    zT = kb.dr(T, "zT", [DC, TC], BF16, kind="ExternalInput")
    oT = kb.dr(T, "oT", [DA, TC], BF16, kind="ExternalInput")
    gT = kb.dr(T, "gT", [2 * D, TC], BF16, kind="ExternalInput")
    cup_d = kb.dr(T, "cu_prev", [128, 8, 2], F32, kind="ExternalInput")
    bh_d = kb.dr(T, "b_head", [128, 8, 2], F32, kind="ExternalInput")
    convw_d = kb.dr(T, "convw", [128, 8, 3], F32, kind="ExternalInput")
    wA = kb.dr(T, "wA_b", [DC, D], BF16, kind="ExternalInput")
    wB = kb.dr(T, "wB_b", [DA, D], BF16, kind="ExternalInput")
    wo = kb.dr(T, "wo_b", [D, D], BF16, kind="ExternalInput")
    h = kb.dr(T, "h", [TC, D], F32, kind="ExternalInput")
    h1 = kb.dr(T, "h1", [TC, D], F32, kind="ExternalOutput")

    cup = kb.sbuf("cup", [128, 8, 2], F32)
    bh = kb.sbuf("bh", [128, 8, 2], F32)
    convw = kb.sbuf("convw_s", [128, 8, 3], F32)
    fx = kb.sbuf("fx", [128, 8, 4], F32)
    fused = T is not None and "o_all" in T
    for (t, d) in ((bh, bh_d), (convw, convw_d)) + (() if fused else ((cup, cup_d),)):
        kb.dma("sp", t.ap[:, :, :], d[:, :, :], t, load=True)
    if fused:
        hmask = kb.sbuf("hmask", [128, 8], F32)
        pmask = kb.sbuf("pmask", [128, 8], F32)
        kb.dma("sp", hmask.ap[:, :], T["hmask"][:, :], hmask, load=True)
        kb.dma("sp", pmask.ap[:, :], T["pmask"][:, :], pmask, load=True)
        Xc = kb.sbuf("Xc", [128, 8, 8, 2], F32)
        kb.dma("sp", Xc.ap[:, :, :, :], T["cu_all"].rearrange("(r c p) t -> p r c t", p=128, c=8), Xc, load=True)
        Xo = [kb.sbuf(f"Xo{i}", [128, 8, TT], BF16) for i in range(2)]

        def select(out_tile, out_ap, src_tile, in_fn, mk):
            kb.op("dve", lambda: nc.vector.tensor_scalar(out=out_ap, in0=in_fn(0), scalar1=mk.ap[:, 0:1], scalar2=None, op0=ALU.mult),
                  reads=[src_tile, mk], writes=[out_tile])
            for hh in range(1, 8):
                kb.op("dve", lambda hh=hh: nc.vector.scalar_tensor_tensor(out=out_ap, in0=in_fn(hh), scalar=mk.ap[:, hh:hh + 1], in1=out_ap, op0=ALU.mult, op1=ALU.add),
                      reads=[src_tile, mk, out_tile], writes=[out_tile])
        select(cup, cup.ap[:, :, :], Xc, lambda r: Xc.ap[:, r, :, :], pmask)
    wring = [kb.sbuf(f"w{i}", [128, 16, 512], BF16) for i in range(3)]
    zt_r = [kb.sbuf(f"zt{i}", [128, 8, TT], BF16) for i in range(2)]
    ot_r = [kb.sbuf(f"ot{i}", [128, 8, TT], BF16) for i in range(2)]
    gt = kb.sbuf("gt", [128, 32, TT], BF16)
    mT = kb.sbuf("mT", [128, 16, TT], BF16)
    hall = kb.sbuf("hall", [128, 4, D], F32)
    tmp = [kb.sbuf(f"tmp{i}", [128, TT], F32) for i in range(4)]
    ps_ring = [kb.psum(f"ps{i}", [128, 512], F32) for i in range(8)]
    ctr = {"w": 0, "ps": 0, "tmp": 0}

    def next_ps():
        p = ps_ring[ctr["ps"] % len(ps_ring)]
        ctr["ps"] += 1
        return p

    V = nc.vector
    kb.op("dve", lambda: V.tensor_tensor(out=fx.ap[:, :, 0], in0=cup.ap[:, :, 0], in1=convw.ap[:, :, 0], op=ALU.mult), reads=[cup, convw], writes=[fx])
    kb.op("dve", lambda: V.tensor_tensor(out=fx.ap[:, :, 2], in0=cup.ap[:, :, 1], in1=convw.ap[:, :, 1], op=ALU.mult), reads=[cup, convw], writes=[fx])
    kb.op("dve", lambda: V.tensor_tensor(out=fx.ap[:, :, 0], in0=fx.ap[:, :, 0], in1=fx.ap[:, :, 2], op=ALU.add), reads=[fx], writes=[fx])
    kb.op("dve", lambda: V.tensor_tensor(out=fx.ap[:, :, 0], in0=fx.ap[:, :, 0], in1=bh.ap[:, :, 0], op=ALU.mult), reads=[fx, bh], writes=[fx])
    kb.op("dve", lambda: V.tensor_tensor(out=fx.ap[:, :, 1], in0=cup.ap[:, :, 1], in1=convw.ap[:, :, 0], op=ALU.mult), reads=[cup, convw], writes=[fx])
    kb.op("dve", lambda: V.tensor_tensor(out=fx.ap[:, :, 1], in0=fx.ap[:, :, 1], in1=bh.ap[:, :, 1], op=ALU.mult), reads=[fx, bh], writes=[fx])

    for t in range(NT):
        tok0 = t * TT
        zt, ot = zt_r[t % 2], ot_r[t % 2]
        kb.dma("sp", zt.ap[:, :, :], zT[:, tok0:tok0 + TT].rearrange("(c p) t -> p c t", p=128), zt, load=True)
        if not fused:
            kb.dma("sp", ot.ap[:, :, :], oT[:, tok0:tok0 + TT].rearrange("(c p) t -> p c t", p=128), ot, load=True)
        else:
            for hh in range(8):
                xo = Xo[hh % 2]
                kb.dma("sp", xo.ap[:, :, :], T["o_all"][hh * 128:(hh + 1) * 128, :].rearrange("d (c t) -> d c t", c=8)[:, :, tok0:tok0 + TT], xo, load=True)
                select(ot, ot.ap[:, hh, :], xo, lambda c_, xo=xo: xo.ap[:, c_, :], hmask)
        for q4 in range(4):
            kb.dma("sp", gt.ap[:, q4 * 8:(q4 + 1) * 8, :], gT[q4 * 1024:(q4 + 1) * 1024, tok0:tok0 + TT].rearrange("(c p) t -> p c t", p=128), gt, load=True)
        kb.dma("sp", hall.ap[:, :, :], h[tok0:tok0 + TT, :].rearrange("(n p) d -> p n d", p=128), hall, load=True)
        if t == 0:
            kb.op("dve", lambda: V.tensor_tensor(out=zt.ap[:, :, 0:2], in0=zt.ap[:, :, 0:2], in1=fx.ap[:, :, 0:2], op=ALU.add), reads=[zt, fx], writes=[zt])
        for jb in range(4):
            wt = wring[ctr["w"] % len(wring)]
            ctr["w"] += 1
            kb.dma("sp", wt.ap[:, 0:8, :], wA[:, jb * 512:(jb + 1) * 512].rearrange("(c p) n -> p c n", p=128), wt, load=True)
            kb.dma("sp", wt.ap[:, 8:16, :], wB[:, jb * 512:(jb + 1) * 512].rearrange("(c p) n -> p c n", p=128), wt, load=True)
            for c in range(4):
                j = jb * 4 + c
                pa = next_ps()
                mm_group(kb, pa, pa.ap[:, :], [(wt.ap[:, k, c * 128:(c + 1) * 128], zt.ap[:, k, :]) for k in range(8)], [wt, zt])
                pb = next_ps()
                mm_group(kb, pb, pb.ap[:, :], [(wt.ap[:, 8 + k, c * 128:(c + 1) * 128], ot.ap[:, k, :]) for k in range(8)], [wt, ot])
                t1 = tmp[ctr["tmp"] % 4]
                t2 = tmp[(ctr["tmp"] + 1) % 4]
                ctr["tmp"] += 2
                kb.op("dve", lambda: V.tensor_tensor(out=t1.ap[:, :], in0=pa.ap[:, :], in1=gt.ap[:, j, :], op=ALU.mult), reads=[pa, gt], writes=[t1])
                kb.op("dve", lambda: V.tensor_tensor(out=t2.ap[:, :], in0=pb.ap[:, :], in1=gt.ap[:, 16 + j, :], op=ALU.mult), reads=[pb, gt], writes=[t2])
                kb.op("pool", lambda: nc.gpsimd.tensor_tensor(out=mT.ap[:, j, :], in0=t1.ap[:, :], in1=t2.ap[:, :], op=ALU.add), reads=[t1, t2], writes=[mT])
        for cb in range(4):
            wt = load_w(kb, wring, ctr, wo, 0, 16, cb * 512, 512)
            for tb in range(4):
                p = next_ps()
                mm_group(kb, p, p.ap[:, :], [(mT.ap[:, k, tb * 128:(tb + 1) * 128], wt.ap[:, k, :]) for k in range(16)], [wt, mT])
                kb.op("dve", lambda: V.tensor_tensor(out=hall.ap[:, tb, cb * 512:(cb + 1) * 512], in0=p.ap[:, :], in1=hall.ap[:, tb, cb * 512:(cb + 1) * 512], op=ALU.add),
                      reads=[p, hall], writes=[hall])
        kb.dma("sp", h1[tok0:tok0 + TT, :].rearrange("(n p) d -> p n d", p=128), hall.ap[:, :, :], hall, load=False)
    if own:
        return kb.finish()
    kb.phase_end()


def build_ffn(TC, moe, kb=None, T=None):
    own = kb is None
    kb = kb or KB()
    if not own:
        kb.phase_begin()
    nc = kb.nc
    V = nc.vector
    NT = TC // TT
    DFX = DFE if moe else DFF
    NEX = NE if moe else 1
    NHALF = 2 if moe else 1
    NCB = DFX // 512 // NHALF
    KCH = DFX // 128 // NHALF
    h = kb.dr(T, "h", [TC, D], F32, kind="ExternalInput")
    gcol_d = kb.dr(T, "gcol", [128, 16], F32, kind="ExternalInput")
    ident_d = kb.dr(T, "ident", [128, 128], BF16, kind="ExternalInput")
    wg = kb.dr(T, "wg_b", [NEX * D, DFX], BF16, kind="ExternalInput")
    wu = kb.dr(T, "wu_b", [NEX * D, DFX], BF16, kind="ExternalInput")
    wd = kb.dr(T, "wd_b", [NEX * DFX, D], BF16, kind="ExternalInput")
    out = kb.dr(T, "hout", [TC, D], F32, kind="ExternalOutput")

    cs = Consts()
    cs.eps = kb.sbuf("eps", [128, 1], F32)
    gcol = kb.sbuf("gcol_s", [128, 16], F32)
    ident = kb.sbuf("ident_s", [128, 128], BF16)
    kb.op("dve", lambda: V.memset(cs.eps.ap[:, :], EPS), writes=[cs.eps])
    kb.dma("sp", gcol.ap[:, :], gcol_d[:, :], gcol, load=True)
    kb.dma("sp", ident.ap[:, :], ident_d[:, :], ident, load=True)
    hin_ring = [kb.sbuf("hin0", [128, D], F32)]
    ablk_ring = [kb.sbuf("ablk0", [128, D], BF16)]
    ss_ring = [kb.sbuf(f"ss{i}", [128, 4], F32) for i in range(4)]
    aT = kb.sbuf("aT", [128, 16, TT], BF16)
    wring = [kb.sbuf(f"w{i}", [128, 16, 512], BF16) for i in range(4)]
    hdnT = kb.sbuf("hdnT", [128, KCH, TT], BF16)
    hall = kb.sbuf("hall", [128, 4, D], F32)
    sg8 = kb.sbuf("sg8", [128, 8, TT], F32 if not moe else BF16)
    tp_ring = [kb.psum(f"tp{i}", [128, 8, 128], BF16) for i in range(2)]
    router = None
    if moe:
        rw_d = kb.dr(T, "rw", [128, 16, NE], F32, kind="ExternalInput")
        rb_d = kb.dr(T, "rb", [128, NE], F32, kind="ExternalInput")
        gfin_d = kb.dr(T, "gfin", [128, D], F32, kind="ExternalInput")
        id32_d = kb.dr(T, "ident32", [128, 128], F32, kind="ExternalInput")
        rw = kb.sbuf("rw_s", [128, 16, NE], F32)
        rb = kb.sbuf("rb_s", [128, NE], F32)
        gfin = kb.sbuf("gfin_s", [128, D], F32)
        id32 = kb.sbuf("id32_s", [128, 128], F32)
        kb.dma("sp", rw.ap[:, :, :], rw_d[:, :, :], rw, load=True)
        kb.dma("sp", rb.ap[:, :], rb_d[:, :], rb, load=True)
        kb.dma("sp", gfin.ap[:, :], gfin_d[:, :], gfin, load=True)
        kb.dma("sp", id32.ap[:, :], id32_d[:, :], id32, load=True)
        ab32 = kb.sbuf("ab32", [128, D], F32)
        a32T = [kb.sbuf(f"a32T{i}", [128, 4, 128], F32) for i in range(2)]
        tp32 = kb.psum("tp32", [128, 4, 128], F32)
        psl = kb.psum("psl", [128, 512], F32)
        Wall = kb.sbuf("Wall", [128, 4, NE], F32)
        rt = kb.sbuf("rt", [128, 8, NE], F32)
        router = True
    ps_ring = [kb.psum(f"ps{i}", [128, 512], F32) for i in range(4 if moe else 6)]
    ctr = {"nb": 0, "tp": 0, "w": 0, "ps": 0, "sg": 0, "a32": 0}

    def next_ps():
        p = ps_ring[ctr["ps"] % len(ps_ring)]
        ctr["ps"] += 1
        return p

    def route(blk):
        hin = hin_ring[0]
        ss = ss_ring[(ctr["nb"] - 1) % len(ss_ring)]
        kb.op("dve", lambda: V.tensor_scalar(out=ab32.ap[:, :], in0=hin.ap[:, :], scalar1=ss.ap[:, 2:3], scalar2=None, op0=ALU.mult),
              reads=[hin, ss], writes=[ab32])
        for r in range(4):
            for j in range(4):
                c = r * 4 + j
                kb.op("pe", lambda c=c, j=j: nc.tensor.transpose(tp32.ap[:, j, :], ab32.ap[:, c * 128:(c + 1) * 128], id32.ap[:, :]),
                      reads=[ab32, id32] if j == 0 else (), writes=[tp32] if j == 0 else (), signal=(j == 3))
            es = kb.esem["pe"]
            tp32.w = [(es, es.v, "pe")]
            ab32.r.append((es, es.v, "pe"))
            a3 = a32T[ctr["a32"] % 2]
            ctr["a32"] += 1
            for j in range(4):
                c = r * 4 + j
                kb.op("dve", lambda c=c, j=j: V.tensor_scalar(out=a3.ap[:, j, :], in0=tp32.ap[:, j, :], scalar1=gcol.ap[:, c:c + 1], scalar2=None, op0=ALU.mult),
                      reads=[tp32, gcol], writes=[a3])
            for j in range(4):
                c = r * 4 + j
                kb.op("pe", lambda c=c, j=j: nc.tensor.matmul(psl.ap[:, 0:NE], a3.ap[:, j, :], rw.ap[:, c, :], start=(c == 0), stop=(c == 15)),
                      reads=[a3, rw], writes=[psl] if c == 0 else (), signal=(j == 3))
        es = kb.esem["pe"]
        psl.w = [(es, es.v, "pe")]
        L, m1k, L2, m2k = rt.ap[:, 0, :], rt.ap[:, 1, :], rt.ap[:, 2, :], rt.ap[:, 3, :]
        sc = rt.ap[:, 4, :]
        AX = mybir.AxisListType.X
        kb.op("dve", lambda: V.tensor_tensor(out=L, in0=psl.ap[:, 0:NE], in1=rb.ap[:, :], op=ALU.add), reads=[psl, rb], writes=[rt])
        kb.op("dve", lambda: V.tensor_reduce(out=sc[:, 0:1], in_=L, axis=AX, op=ALU.max), reads=[rt], writes=[rt])
        kb.op("dve", lambda: V.tensor_scalar(out=m1k, in0=L, scalar1=sc[:, 0:1], scalar2=None, op0=ALU.is_equal), reads=[rt], writes=[rt])
        kb.op("dve", lambda: V.scalar_tensor_tensor(out=L2, in0=m1k, scalar=-1e30, in1=L, op0=ALU.mult, op1=ALU.add), reads=[rt], writes=[rt])
        kb.op("dve", lambda: V.tensor_reduce(out=sc[:, 1:2], in_=L2, axis=AX, op=ALU.max), reads=[rt], writes=[rt])
        kb.op("dve", lambda: V.tensor_scalar(out=m2k, in0=L2, scalar1=sc[:, 1:2], scalar2=None, op0=ALU.is_equal), reads=[rt], writes=[rt])
        kb.op("dve", lambda: V.tensor_tensor(out=sc[:, 2:3], in0=sc[:, 1:2], in1=sc[:, 0:1], op=ALU.subtract), reads=[rt], writes=[rt])
        kb.op("act", lambda: nc.scalar.activation(out=sc[:, 3:4], in_=sc[:, 2:3], func=AF.Sigmoid), reads=[rt], writes=[rt])
        kb.op("dve", lambda: V.tensor_scalar(out=sc[:, 4:5], in0=sc[:, 3:4], scalar1=-1.0, scalar2=1.0, op0=ALU.mult, op1=ALU.add), reads=[rt], writes=[rt])
        kb.op("dve", lambda: V.tensor_scalar(out=Wall.ap[:, blk, :], in0=m1k, scalar1=sc[:, 4:5], scalar2=None, op0=ALU.mult), reads=[rt], writes=[Wall])
        kb.op("dve", lambda: V.scalar_tensor_tensor(out=Wall.ap[:, blk, :], in0=m2k, scalar=sc[:, 3:4], in1=Wall.ap[:, blk, :], op0=ALU.mult, op1=ALU.add),
              reads=[rt, Wall], writes=[Wall])

    for t in range(NT):
        tok0 = t * TT
        norm_transpose(kb, cs, h, tok0, aT, hin_ring, ablk_ring, tp_ring, None, ss_ring, gcol, ident, ctr, after_block=(route if moe else None))
        kb.dma("sp", hall.ap[:, :, :], h[tok0:tok0 + TT, :].rearrange("(n p) d -> p n d", p=128), hall, load=True)
        for e, hf in [(e_, h_) for e_ in range(NEX) for h_ in range(NHALF)]:
            ncols_pass = NCB * 512
            blks = [(c0, min(1024, ncols_pass - c0)) for c0 in range(0, ncols_pass, 1024)]

            def load_w2(wd_, krow0, c0, ncols):
                wt_ = wring[ctr["w"] % len(wring)]
                ctr["w"] += 1
                view = wt_.ap[:, :, :].rearrange("p a b -> p (a b)").rearrange("p (k n) -> p k n", n=1024)
                src = wd_[krow0 * 128:(krow0 + 8) * 128, c0:c0 + ncols].rearrange("(c p) n -> p c n", p=128)
                kb.dma("sp", view[:, 0:8, 0:ncols], src, wt_, load=True)
                return wt_, view

            for (c0, ncols) in blks:
                cabs = hf * ncols_pass + c0
                nchk = ncols // 128
                gA, gAv = load_w2(wg, e * 16, cabs, ncols)
                gB, gBv = load_w2(wg, e * 16 + 8, cabs, ncols)
                uA, uAv = load_w2(wu, e * 16, cabs, ncols)
                uB, uBv = load_w2(wu, e * 16 + 8, cabs, ncols)
                for c in range(nchk):
                    pg = next_ps()
                    mm_group(kb, pg, pg.ap[:, :], [(gAv[:, k, c * 128:(c + 1) * 128], aT.ap[:, k, :]) for k in range(8)]
                             + [(gBv[:, k, c * 128:(c + 1) * 128], aT.ap[:, 8 + k, :]) for k in range(8)], [gA, gB, aT])
                    kb.op("act", lambda c=c, pg=pg: nc.scalar.activation(out=sg8.ap[:, c, :], in_=pg.ap[:, :], func=AF.Silu), reads=[pg], writes=[sg8])
                for c in range(nchk):
                    pu = next_ps()
                    mm_group(kb, pu, pu.ap[:, :], [(uAv[:, k, c * 128:(c + 1) * 128], aT.ap[:, k, :]) for k in range(8)]
                             + [(uBv[:, k, c * 128:(c + 1) * 128], aT.ap[:, 8 + k, :]) for k in range(8)], [uA, uB, aT])
                    kb.op("dve", lambda c=c, pu=pu: V.tensor_tensor(out=hdnT.ap[:, c0 // 128 + c, :], in0=pu.ap[:, :], in1=sg8.ap[:, c, :], op=ALU.mult),
                          reads=[pu, sg8], writes=[hdnT])
            kgs = [(k0, min(16, KCH - k0)) for k0 in range(0, KCH, 16)]
            for cb in range(4):
                pss = [next_ps() for _ in range(4)]
                for gi, (k0, kc) in enumerate(kgs):
                    wt = load_w(kb, wring, ctr, wd, (e * NHALF + hf) * KCH + k0, kc, cb * 512, 512)
                    for tb in range(4):
                        p = pss[tb]
                        for k in range(kc):
                            first = (gi == 0 and k == 0)
                            last = (gi == len(kgs) - 1 and k == kc - 1)
                            kb.op("pe", lambda k=k, p=p, tb=tb: nc.tensor.matmul(p.ap[:, :], hdnT.ap[:, k0 + k, tb * 128:(tb + 1) * 128], wt.ap[:, k, :], start=first, stop=last),
                                  reads=[hdnT, wt] if k == 0 else (), writes=[p] if first else (), signal=(k == kc - 1))
                        es = kb.esem["pe"]
                        wt.r.append((es, es.v, "pe"))
                        hdnT.r.append((es, es.v, "pe"))
                        if gi == len(kgs) - 1:
                            p.w = [(es, es.v, "pe")]
                for tb in range(4):
                    p = pss[tb]
                    dst = hall.ap[:, tb, cb * 512:(cb + 1) * 512]
                    if moe:
                        kb.op("dve", lambda p=p, dst=dst, tb=tb: V.scalar_tensor_tensor(out=dst, in0=p.ap[:, :], scalar=Wall.ap[:, tb, e:e + 1], in1=dst, op0=ALU.mult, op1=ALU.add),
                              reads=[p, hall, Wall], writes=[hall])
                    else:
                        kb.op("dve", lambda p=p, dst=dst: V.tensor_tensor(out=dst, in0=p.ap[:, :], in1=dst, op=ALU.add), reads=[p, hall], writes=[hall])
        if moe:
            hin = hin_ring[0]
            for tb in range(4):
                ss = ss_ring[tb]
                kb.op("act", lambda: nc.scalar.activation(out=hin.ap[:, :], in_=hall.ap[:, tb, :], func=AF.Square, accum_out=ss.ap[:, 0:1]), reads=[hall], writes=[hin, ss])
                kb.op("act", lambda: nc.scalar.activation(out=ss.ap[:, 1:2], in_=ss.ap[:, 0:1], func=AF.Sqrt, bias=cs.eps.ap[:, 0:1], scale=1.0 / D), reads=[ss, cs.eps], writes=[ss])
                kb.op("dve", lambda: V.reciprocal(out=ss.ap[:, 2:3], in_=ss.ap[:, 1:2]), reads=[ss], writes=[ss])
                kb.op("dve", lambda: V.scalar_tensor_tensor(out=hall.ap[:, tb, :], in0=hall.ap[:, tb, :], scalar=ss.ap[:, 2:3], in1=gfin.ap[:, :], op0=ALU.mult, op1=ALU.mult),
                      reads=[hall, ss, gfin], writes=[hall])
        kb.dma("sp", out[tok0:tok0 + TT, :].rearrange("(n p) d -> p n d", p=128), hall.ap[:, :, :], hall, load=False)
    if own:
        return kb.finish()
    kb.phase_end()


_PROG = {}


def _prog(key, fn):
    if key not in _PROG:
        _PROG[key] = fn()
    return _PROG[key]


def _run(nc, in_maps):
    res = run_bass_kernel_spmd(nc, in_maps, core_ids=list(range(NCORE)))
    return res.results


def _col(a):
    return np.ascontiguousarray(a.reshape(8, 128, a.shape[1]).transpose(1, 0, 2))


def kernel_unfused(x, mix_norm, w_in, b_forget, conv_w, w_conv_out, w_attn_out, w_o, ffn_norm,
           dense_w_gate, dense_w_up, dense_w_down, router_w, router_b,
           moe_w_gate, moe_w_up, moe_w_down, final_norm):
    f32 = np.float32
    bf = ml_dtypes.bfloat16
    B, S, _ = x.shape
    NTOK = B * S
    TC = NTOK // NCORE
    depth = w_in.shape[0]
    A = lambda a: np.ascontiguousarray(np.asarray(a, dtype=f32))

    wsrc = {
        "w_in": A(w_in).reshape(-1, PTOT), "wA": A(w_conv_out).reshape(-1, D), "wB": A(w_attn_out).reshape(-1, D),
        "wo": A(w_o).reshape(-1, D), "dg": A(dense_w_gate).reshape(-1, DFF), "du": A(dense_w_up).reshape(-1, DFF),
        "dd": A(dense_w_down).reshape(-1, D), "mg": A(moe_w_gate).reshape(-1, DFE), "mu": A(moe_w_up).reshape(-1, DFE),
        "md": A(moe_w_down).reshape(-1, D),
    }
    specs = [(n, a.shape[0] // NCORE, a.shape[1]) for n, a in wsrc.items()]
    nc0 = _prog("cast", lambda: build_cast(specs))
    in_maps = [{n: a[c * (a.shape[0] // NCORE):(c + 1) * (a.shape[0] // NCORE)] for n, a in wsrc.items()} for c in range(NCORE)]
    r0 = _run(nc0, in_maps)
    wb = {n: np.concatenate([np.asarray(r0[c][n + "_b"]) for c in range(NCORE)], axis=0) for n in wsrc}
    del wsrc, in_maps, r0

    ident = np.eye(128, dtype=f32).astype(bf)
    consts2 = p2_consts()
    h = [A(x).reshape(NTOK, D)[c * TC:(c + 1) * TC] for c in range(NCORE)]
    nc1 = _prog("p1", lambda: build_p1(TC))
    nc2 = _prog("p2", lambda: build_p2(S, B))
    nc3 = _prog("p3", lambda: build_p3(TC))
    cores_per_seq = NCORE // B
    out = None
    for l in range(depth):
        gcol = np.ascontiguousarray(A(mix_norm[l]).reshape(16, 128).T)
        convw = _col(np.ascontiguousarray(A(conv_w[l]).T))
        bfg = A(b_forget[l]).reshape(NH, 1)
        w_in_l = wb["w_in"][l * D:(l + 1) * D]
        r1 = _run(nc1, [{"h": h[c], "w_in_b": w_in_l, "gcol": gcol, "convw": convw, "bf": bfg, "ident": ident} for c in range(NCORE)])
        in2 = []
        for hd in range(NH):
            sl = slice(hd * HD, (hd + 1) * HD)
            lf_h = np.concatenate([np.asarray(r1[c]["lf"])[hd] for c in range(NCORE)])
            m = {"qT": np.concatenate([np.asarray(r1[c]["qT"])[sl] for c in range(NCORE)], axis=1),
                 "kT": np.concatenate([np.asarray(r1[c]["kT"])[sl] for c in range(NCORE)], axis=1),
                 "v": np.ascontiguousarray(np.concatenate([np.asarray(r1[c]["v"])[:, sl] for c in range(NCORE)], axis=0)),
                 "lfcol": np.ascontiguousarray(lf_h.reshape(NTOK // 128, 128).T)}
            m.update(consts2)
            in2.append(m)
        r2 = _run(nc2, in2)
        del in2
        in3 = []
        for c in range(NCORE):
            oT_c = np.concatenate([np.asarray(r2[hd]["oT"])[:, c * TC:(c + 1) * TC] for hd in range(NH)], axis=0)
            cu_prev = np.zeros((DC, 2), f32) if c % cores_per_seq == 0 else np.asarray(r1[c - 1]["cu_tail"])
            in3.append({"zT": np.asarray(r1[c]["zT"]), "oT": np.ascontiguousarray(oT_c), "gT": np.asarray(r1[c]["gT"]),
                        "cu_prev": _col(cu_prev), "b_head": _col(np.asarray(r1[c]["b_head"])), "convw": convw,
                        "wA_b": wb["wA"][l * DC:(l + 1) * DC], "wB_b": wb["wB"][l * DA:(l + 1) * DA], "wo_b": wb["wo"][l * D:(l + 1) * D],
                        "h": h[c]})
        r3 = _run(nc3, in3)
        del in3, r1, r2
        h1 = [np.asarray(r3[c]["h1"]) for c in range(NCORE)]
        gcol2 = np.ascontiguousarray(A(ffn_norm[l]).reshape(16, 128).T)
        i = l // 2
        if l % 2 == 0:
            nc4 = _prog("ffn_dense", lambda: build_ffn(TC, False))
            r4 = _run(nc4, [{"h": h1[c], "gcol": gcol2, "ident": ident, "wg_b": wb["dg"][i * D:(i + 1) * D],
                             "wu_b": wb["du"][i * D:(i + 1) * D], "wd_b": wb["dd"][i * DFF:(i + 1) * DFF]} for c in range(NCORE)])
            h = [np.asarray(r4[c]["hout"]) for c in range(NCORE)]
        else:
            nc5 = _prog("ffn_moe", lambda: build_ffn(TC, True))
            rw = np.ascontiguousarray(A(router_w[i]).reshape(16, 128, NE).transpose(1, 0, 2))
            rb = np.ascontiguousarray(np.broadcast_to(A(router_b[i]), (128, NE)))
            gfin = np.ascontiguousarray(np.broadcast_to(A(final_norm), (128, D)))
            r5 = _run(nc5, [{"h": h1[c], "gcol": gcol2, "ident": ident, "wg_b": wb["mg"][i * NE * D:(i + 1) * NE * D],
                             "wu_b": wb["mu"][i * NE * D:(i + 1) * NE * D], "wd_b": wb["md"][i * NE * DFE:(i + 1) * NE * DFE],
                             "rw": rw, "rb": rb, "gfin": gfin, "ident32": np.eye(128, dtype=f32)} for c in range(NCORE)])
            h = [np.asarray(r5[c]["hout"]) for c in range(NCORE)]
    out = np.concatenate(h, axis=0).reshape(B, S, D).astype(f32)
    return out


WSPECS = [
    ("w_in", 2 * D, PTOT), ("wA", 2 * DC, D), ("wB", 2 * DA, D), ("wo", 2 * D, D),
    ("dg", D, DFF), ("du", D, DFF), ("dd", DFF, D),
    ("mg", NE * D, DFE), ("mu", NE * D, DFE), ("md", NE * DFE, D),
]


def build_fused(S, B):
    kb = KB()
    nc = kb.nc
    NTOK = S * B
    TC = NTOK // NCORE
    I32 = mybir.dt.int32
    ein = lambda n, sh, dt: kb.dram(n, sh, dt, kind="ExternalInput")
    x = ein("x", [TC, D], F32)
    out = kb.dram("out", [TC, D], F32, kind="ExternalOutput")
    P = {}
    for l in range(2):
        P[f"gcol{l}"] = ein(f"gcol{l}", [128, 16], F32)
        P[f"gcolf{l}"] = ein(f"gcolf{l}", [128, 16], F32)
        P[f"convw{l}"] = ein(f"convw{l}", [128, 8, 3], F32)
        P[f"bf{l}"] = ein(f"bf{l}", [8, 1], F32)
    for (n, sh, dt) in (("ident", [128, 128], BF16), ("ident32", [128, 128], F32), ("tri", [128, 128], F32), ("sel", [128, 128], F32),
                        ("onesb", [128, 128], BF16), ("masks", [128, 4, TT], BF16), ("hmask", [128, 8], F32), ("pmask", [128, 8], F32),
                        ("rw", [128, 16, NE], F32), ("rb", [128, NE], F32), ("gfin", [128, D], F32)):
        P[n] = ein(n, sh, dt)
    W = {}
    cast = Sem(kb, "cast")
    ccs = {}
    rg = [list(range(NCORE))]
    for (n, rows, cols) in WSPECS:
        src = ein(n, [rows // NCORE, cols], F32)
        my = kb.dram(n + "_my", [rows // NCORE, cols], BF16)
        full = kb.dram(n + "_full", [rows, cols], BF16)
        cast_rows(kb, cast, src, my, rows // NCORE, cols)
        nc.gpsimd.wait_ge(cast.h, cast.v)
        ccs[n] = Sem(kb, "cc_" + n)
        nc.gpsimd.collective_compute("AllGather", ALU.bypass, replica_groups=rg, ins=[my[:, :]], outs=[full[:, :]]).then_inc(ccs[n].h, 1)
        ccs[n].v += 1
        W[n] = full

    def need(names):
        for ek in kb.engs:
            for n in names:
                kb._wait(ek, ccs[n], ccs[n].v)

    idr = lambda n, sh, dt: kb.dram(n, sh, dt)
    zT = idr("zT_s", [DC, TC], BF16)
    gT = idr("gT_s", [2 * D, TC], BF16)
    b_head = idr("bhead_s", [DC, 2], F32)
    cu_tail = idr("cutail_s", [DC, 2], F32)
    cu_all = idr("cuall_s", [NCORE * DC, 2], F32)
    qT_my = idr("qT_my", [DA, TC], BF16)
    kT_my = idr("kT_my", [DA, TC], BF16)
    v_my = idr("v_my", [TC, DA], BF16)
    lf_my = idr("lf_my", [NH, TC], F32)
    qT_all = idr("qT_all", [NCORE * DA, TC], BF16)
    kT_all = idr("kT_all", [NCORE * DA, TC], BF16)
    v_all = idr("v_all", [NTOK, DA], BF16)
    lf_all = idr("lf_all", [NCORE * NH, TC], F32)
    oT_h = idr("oT_h", [128, NTOK], BF16)
    o_all = idr("o_all", [NCORE * 128, NTOK], BF16)
    h1 = idr("h1_s", [TC, D], F32)
    h2 = idr("h2_s", [TC, D], F32)
    ccx = Sem(kb, "ccx")

    def gather(pairs):
        for (a, b) in pairs:
            nc.gpsimd.collective_compute("AllGather", ALU.bypass, replica_groups=rg, ins=[a[:, :]], outs=[b[:, :]]).then_inc(ccx.h, 1)
            ccx.v += 1
        for ek in kb.engs:
            kb._wait(ek, ccx, ccx.v)

    hcur = x
    for l in range(2):
        need(["w_in"])
        build_p1(TC, kb=kb, T={"h": hcur, "w_in_b": W["w_in"][l * D:(l + 1) * D, :], "gcol": P[f"gcol{l}"], "convw": P[f"convw{l}"], "bf": P[f"bf{l}"],
                               "ident": P["ident"], "zT": zT, "cu_tail": cu_tail, "b_head": b_head, "qT": qT_my, "kT": kT_my, "v": v_my, "lf": lf_my, "gT": gT})
        gather([(qT_my, qT_all), (kT_my, kT_all), (v_my, v_all), (lf_my, lf_all), (cu_tail, cu_all)])
        build_p2(S, B, kb=kb, T={"tri": P["tri"], "sel": P["sel"], "onesb": P["onesb"], "masks": P["masks"], "hmask": P["hmask"], "ident32": P["ident32"],
                                 "qT_all": qT_all, "kT_all": kT_all, "v_all": v_all, "lf_all": lf_all, "oT": oT_h})
        gather([(oT_h, o_all)])
        need(["wA", "wB", "wo"])
        build_p3(TC, kb=kb, T={"zT": zT, "gT": gT, "b_head": b_head.rearrange("(c p) t -> p c t", p=128), "convw": P[f"convw{l}"],
                               "wA_b": W["wA"][l * DC:(l + 1) * DC, :], "wB_b": W["wB"][l * DA:(l + 1) * DA, :], "wo_b": W["wo"][l * D:(l + 1) * D, :],
                               "h": hcur, "h1": h1, "hmask": P["hmask"], "pmask": P["pmask"], "cu_all": cu_all, "o_all": o_all})
        if l % 2 == 0:
            need(["dg", "du", "dd"])
            build_ffn(TC, False, kb=kb, T={"h": h1, "gcol": P[f"gcolf{l}"], "ident": P["ident"], "wg_b": W["dg"], "wu_b": W["du"], "wd_b": W["dd"], "hout": h2})
            hcur = h2
        else:
            need(["mg", "mu", "md"])
            build_ffn(TC, True, kb=kb, T={"h": h1, "gcol": P[f"gcolf{l}"], "ident": P["ident"], "wg_b": W["mg"], "wu_b": W["mu"], "wd_b": W["md"], "hout": out,
                                          "rw": P["rw"], "rb": P["rb"], "gfin": P["gfin"], "ident32": P["ident32"]})
    return kb.finish([cast, ccx] + list(ccs.values()))


def kernel_fused(x, mix_norm, w_in, b_forget, conv_w, w_conv_out, w_attn_out, w_o, ffn_norm,
           dense_w_gate, dense_w_up, dense_w_down, router_w, router_b,
           moe_w_gate, moe_w_up, moe_w_down, final_norm):
    f32 = np.float32
    bf = ml_dtypes.bfloat16
    B, S, _ = x.shape
    NTOK = B * S
    TC = NTOK // NCORE
    A = lambda a: np.ascontiguousarray(np.asarray(a, dtype=f32))
    wsrc = {
        "w_in": A(w_in).reshape(-1, PTOT), "wA": A(w_conv_out).reshape(-1, D), "wB": A(w_attn_out).reshape(-1, D),
        "wo": A(w_o).reshape(-1, D), "dg": A(dense_w_gate).reshape(-1, DFF), "du": A(dense_w_up).reshape(-1, DFF),
        "dd": A(dense_w_down).reshape(-1, D), "mg": A(moe_w_gate).reshape(-1, DFE), "mu": A(moe_w_up).reshape(-1, DFE),
        "md": A(moe_w_down).reshape(-1, D),
    }
    common = {"ident": np.eye(128, dtype=f32).astype(bf), "ident32": np.eye(128, dtype=f32)}
    common.update(p2_consts())
    for l in range(2):
        common[f"gcol{l}"] = np.ascontiguousarray(A(mix_norm[l]).reshape(16, 128).T)
        common[f"gcolf{l}"] = np.ascontiguousarray(A(ffn_norm[l]).reshape(16, 128).T)
        common[f"convw{l}"] = _col(np.ascontiguousarray(A(conv_w[l]).T))
        common[f"bf{l}"] = A(b_forget[l]).reshape(NH, 1)
    common["rw"] = np.ascontiguousarray(A(router_w[0]).reshape(16, 128, NE).transpose(1, 0, 2))
    common["rb"] = np.ascontiguousarray(np.broadcast_to(A(router_b[0]), (128, NE)))
    common["gfin"] = np.ascontiguousarray(np.broadcast_to(A(final_norm), (128, D)))
    xf = A(x).reshape(NTOK, D)
    cps = NCORE // B
    in_maps = []
    for c in range(NCORE):
        m = dict(common)
        m["x"] = xf[c * TC:(c + 1) * TC]
        hm = np.zeros((128, 8), f32)
        hm[:, c] = 1.0
        pm = np.zeros((128, 8), f32)
        if c % cps != 0:
            pm[:, c - 1] = 1.0
        m["hmask"] = hm
        m["pmask"] = pm
        for n, a in wsrc.items():
            r = a.shape[0] // NCORE
            m[n] = a[c * r:(c + 1) * r]
        in_maps.append(m)
    nc = _prog(("fused", S, B), lambda: build_fused(S, B))
    res = run_bass_kernel_spmd(nc, in_maps, core_ids=list(range(NCORE)))
    return np.concatenate([np.asarray(res.results[c]["out"]) for c in range(NCORE)], axis=0).reshape(B, S, D).astype(f32)


def build_mid(TC, moe, with_p1):
    kb = KB()
    E = lambda n, sh, dt: kb.dram(n, sh, dt, kind="ExternalInput")
    O = lambda n, sh, dt: kb.dram(n, sh, dt, kind="ExternalOutput")
    h1 = kb.dram("h1_i", [TC, D], F32)
    ident = E("ident", [128, 128], BF16)
    T3 = {"zT": E("zT", [DC, TC], BF16), "oT": E("oT", [DA, TC], BF16), "gT": E("gT", [2 * D, TC], BF16),
          "cu_prev": E("cu_prev", [128, 8, 2], F32), "b_head": E("b_head", [128, 8, 2], F32), "convw": E("convw", [128, 8, 3], F32),
          "wA_b": E("wA_b", [DC, D], BF16), "wB_b": E("wB_b", [DA, D], BF16), "wo_b": E("wo_b", [D, D], BF16),
          "h": E("h", [TC, D], F32), "h1": h1}
    build_p3(TC, kb=kb, T=T3)
    DFX = DFE if moe else DFF
    NEX = NE if moe else 1
    hout = O("hout", [TC, D], F32)
    TF = {"h": h1, "gcol": E("gcolf", [128, 16], F32), "ident": ident, "wg_b": E("wg_b", [NEX * D, DFX], BF16),
          "wu_b": E("wu_b", [NEX * D, DFX], BF16), "wd_b": E("wd_b", [NEX * DFX, D], BF16), "hout": hout}
    if moe:
        TF.update({"rw": E("rw", [128, 16, NE], F32), "rb": E("rb", [128, NE], F32), "gfin": E("gfin", [128, D], F32),
                   "ident32": E("ident32", [128, 128], F32)})
    build_ffn(TC, moe, kb=kb, T=TF)
    if with_p1:
        T1 = {"h": hout, "w_in_b": E("w_in_b", [D, PTOT], BF16), "gcol": E("gcol_n", [128, 16], F32), "convw": E("convw_n", [128, 8, 3], F32),
              "bf": E("bf_n", [8, 1], F32), "ident": ident,
              "zT": O("zT_n", [DC, TC], BF16), "cu_tail": O("cu_tail_n", [DC, 2], F32), "b_head": O("b_head_n", [DC, 2], F32),
              "qT": O("qT_n", [DA, TC], BF16), "kT": O("kT_n", [DA, TC], BF16), "v": O("v_n", [TC, DA], BF16),
              "lf": O("lf_n", [NH, TC], F32), "gT": O("gT_n", [2 * D, TC], BF16)}
        build_p1(TC, kb=kb, T=T1)
    return kb.finish()


def kernel_unfused6(x, mix_norm, w_in, b_forget, conv_w, w_conv_out, w_attn_out, w_o, ffn_norm,
                    dense_w_gate, dense_w_up, dense_w_down, router_w, router_b,
                    moe_w_gate, moe_w_up, moe_w_down, final_norm):
    f32 = np.float32
    bf = ml_dtypes.bfloat16
    B, S, _ = x.shape
    NTOK = B * S
    TC = NTOK // NCORE
    A = lambda a: np.ascontiguousarray(np.asarray(a, dtype=f32))
    wsrc = {
        "w_in": A(w_in).reshape(-1, PTOT), "wA": A(w_conv_out).reshape(-1, D), "wB": A(w_attn_out).reshape(-1, D),
        "wo": A(w_o).reshape(-1, D), "dg": A(dense_w_gate).reshape(-1, DFF), "du": A(dense_w_up).reshape(-1, DFF),
        "dd": A(dense_w_down).reshape(-1, D), "mg": A(moe_w_gate).reshape(-1, DFE), "mu": A(moe_w_up).reshape(-1, DFE),
        "md": A(moe_w_down).reshape(-1, D),
    }
    specs = [(n, a.shape[0] // NCORE, a.shape[1]) for n, a in wsrc.items()]
    nc0 = _prog("cast", lambda: build_cast(specs))
    r0 = _run(nc0, [{n: a[c * (a.shape[0] // NCORE):(c + 1) * (a.shape[0] // NCORE)] for n, a in wsrc.items()} for c in range(NCORE)])
    wb = {n: np.concatenate([np.asarray(r0[c][n + "_b"]) for c in range(NCORE)], axis=0) for n in wsrc}
    del wsrc, r0
    ident = np.eye(128, dtype=f32).astype(bf)
    consts2 = p2_consts()
    xs = [A(x).reshape(NTOK, D)[c * TC:(c + 1) * TC] for c in range(NCORE)]
    cps = NCORE // B
    gcol = [np.ascontiguousarray(A(mix_norm[l]).reshape(16, 128).T) for l in range(2)]
    gcolf = [np.ascontiguousarray(A(ffn_norm[l]).reshape(16, 128).T) for l in range(2)]
    convw = [_col(np.ascontiguousarray(A(conv_w[l]).T)) for l in range(2)]
    bfg = [A(b_forget[l]).reshape(NH, 1) for l in range(2)]
    nc1 = _prog("p1", lambda: build_p1(TC))
    nc2 = _prog("p2", lambda: build_p2(S, B))
    r1 = _run(nc1, [{"h": xs[c], "w_in_b": wb["w_in"][0:D], "gcol": gcol[0], "convw": convw[0], "bf": bfg[0], "ident": ident} for c in range(NCORE)])
    p1o = [{k: np.asarray(r1[c][k]) for k in ("zT", "cu_tail", "b_head", "qT", "kT", "v", "lf", "gT")} for c in range(NCORE)]
    hprev = xs
    out = None
    for l in range(2):
        in2 = []
        for hd in range(NH):
            sl = slice(hd * HD, (hd + 1) * HD)
            lf_h = np.concatenate([p1o[c]["lf"][hd] for c in range(NCORE)])
            m = {"qT": np.concatenate([p1o[c]["qT"][sl] for c in range(NCORE)], axis=1),
                 "kT": np.concatenate([p1o[c]["kT"][sl] for c in range(NCORE)], axis=1),
                 "v": np.ascontiguousarray(np.concatenate([p1o[c]["v"][:, sl] for c in range(NCORE)], axis=0)),
                 "lfcol": np.ascontiguousarray(lf_h.reshape(NTOK // 128, 128).T)}
            m.update(consts2)
            in2.append(m)
        r2 = _run(nc2, in2)
        del in2
        moe = (l % 2 == 1)
        with_p1 = (l == 0)
        ncm = _prog(("mid", moe, with_p1), lambda: build_mid(TC, moe, with_p1))
        inm = []
        for c in range(NCORE):
            oT_c = np.concatenate([np.asarray(r2[hd]["oT"])[:, c * TC:(c + 1) * TC] for hd in range(NH)], axis=0)
            cu_prev = np.zeros((DC, 2), f32) if c % cps == 0 else p1o[c - 1]["cu_tail"]
            m = {"zT": p1o[c]["zT"], "oT": np.ascontiguousarray(oT_c), "gT": p1o[c]["gT"], "cu_prev": _col(cu_prev), "b_head": _col(p1o[c]["b_head"]),
                 "convw": convw[l], "wA_b": wb["wA"][l * DC:(l + 1) * DC], "wB_b": wb["wB"][l * DA:(l + 1) * DA], "wo_b": wb["wo"][l * D:(l + 1) * D],
                 "h": hprev[c], "ident": ident, "gcolf": gcolf[l]}
            if not moe:
                m.update({"wg_b": wb["dg"], "wu_b": wb["du"], "wd_b": wb["dd"]})
            else:
                m.update({"wg_b": wb["mg"], "wu_b": wb["mu"], "wd_b": wb["md"],
                          "rw": np.ascontiguousarray(A(router_w[0]).reshape(16, 128, NE).transpose(1, 0, 2)),
                          "rb": np.ascontiguousarray(np.broadcast_to(A(router_b[0]), (128, NE))),
                          "gfin": np.ascontiguousarray(np.broadcast_to(A(final_norm), (128, D))), "ident32": np.eye(128, dtype=f32)})
            if with_p1:
                m.update({"w_in_b": wb["w_in"][(l + 1) * D:(l + 2) * D], "gcol_n": gcol[l + 1], "convw_n": convw[l + 1], "bf_n": bfg[l + 1]})
            inm.append(m)
        rm = _run(ncm, inm)
        del inm, r2
        hprev = [np.asarray(rm[c]["hout"]) for c in range(NCORE)]
        if with_p1:
            p1o = [{k: np.asarray(rm[c][k + "_n"]) for k in ("zT", "cu_tail", "b_head", "qT", "kT", "v", "lf", "gT")} for c in range(NCORE)]
    return np.concatenate(hprev, axis=0).reshape(B, S, D).astype(f32)


kernel = kernel_unfused6
```
